# Optimizing a Trainium2 kernel written in Bass

```python
import jax, jax.numpy as jnp
from jax import lax
import numpy as np

D_MODEL = 1024
BATCH = 4
SEQ = 8192
DEPTH = 1

PLE_DIM = 256
SB_HEADS = 8
SB_HEAD_DIM = 64
SB_WIDTH = SB_HEADS * SB_HEAD_DIM
SB_BLOCK = 128
ML_HEADS = 4
ML_HEAD_DIM = 128
ML_WIDTH = ML_HEADS * ML_HEAD_DIM
ML_CHUNK = 128
CONV_WIDTH = 4
PEER_HEADS = 8
PEER_KEYS = 128
PEER_EXPERTS = PEER_KEYS * PEER_KEYS
PEER_QDIM = 256
PEER_HALF = PEER_QDIM // 2
PEER_TOPK = 16
PEER_BLOCK = 128
ALPHA = (2.0 * DEPTH) ** 0.25
BETA = (8.0 * DEPTH) ** -0.25
LN_EPS = 1e-5
IN_SIZES = (SB_WIDTH, SB_WIDTH, SB_WIDTH, ML_WIDTH, ML_WIDTH, ML_WIDTH, ML_WIDTH, ML_HEADS, ML_HEADS, D_MODEL, D_MODEL)
IN_WIDTH = sum(IN_SIZES)

kernel_name = 'hybrid_stickbreak_mlstm_peer_deepnorm'


def layer_norm(x, g, b):
    xf = x.astype(jnp.float32)
    mu = jnp.mean(xf, axis=-1, keepdims=True)
    var = jnp.mean(jnp.square(xf - mu), axis=-1, keepdims=True)
    return ((xf - mu) * lax.rsqrt(var + LN_EPS) * g + b).astype(x.dtype)


def split_heads(t, n_heads):
    b, s, _ = t.shape
    return t.reshape(b, s, n_heads, -1).transpose(0, 2, 1, 3)


def merge_heads(t):
    b, h, s, d = t.shape
    return t.transpose(0, 2, 1, 3).reshape(b, s, h * d)


def causal_conv(x, w, b):
    k_w = w.shape[0]
    s = x.shape[1]
    xp = jnp.pad(x, ((0, 0), (k_w - 1, 0), (0, 0)))
    y = b
    for j in range(k_w):
        y = y + w[j] * xp[:, k_w - 1 - j:k_w - 1 - j + s]
    return y


def stick_breaking_attention(q, k, v):
    s_len, dh = q.shape[2], q.shape[3]
    scale = dh ** -0.5
    outs = []
    for blk in range(s_len // SB_BLOCK):
        t0 = blk * SB_BLOCK
        kl = t0 + SB_BLOCK
        z = jnp.einsum('bhtd,bhsd->bhts', q[:, :, t0:kl], k[:, :, :kl]).astype(jnp.float32) * scale
        t_idx = t0 + jnp.arange(SB_BLOCK)[:, None]
        s_idx = jnp.arange(kl)[None, :]
        causal = s_idx < t_idx
        log_beta = jax.nn.log_sigmoid(z)
        log_one_minus = jnp.where(causal, jax.nn.log_sigmoid(-z), 0.0)
        tail = lax.cumsum(log_one_minus, axis=3, reverse=True) - log_one_minus
        w = jnp.where(causal, jnp.exp(log_beta + tail), 0.0)
        outs.append(jnp.einsum('bhts,bhsd->bhtd', w.astype(v.dtype), v[:, :, :kl]))
    return jnp.concatenate(outs, axis=2)


def mlstm_chunkwise(q, k, v, log_i, log_f):
    b_, h_, s_len, d = q.shape
    L = ML_CHUNK
    nc = s_len // L
    f32 = jnp.float32
    q = q.astype(f32).reshape(b_, h_, nc, L, d)
    k = (k.astype(f32) * d ** -0.5).reshape(b_, h_, nc, L, d)
    v = v.astype(f32).reshape(b_, h_, nc, L, d)
    li = log_i.reshape(b_, h_, nc, L)
    bcum = jnp.cumsum(log_f.reshape(b_, h_, nc, L), axis=-1)
    b_last = bcum[..., -1]
    w_end = b_last[..., None] - bcum + li
    m_loc = jnp.max(w_end, axis=-1)
    e_end = jnp.exp(w_end - m_loc[..., None])
    c_loc = jnp.einsum('bhcsd,bhcse->bhcde', e_end[..., None] * v, k)
    n_loc = jnp.einsum('bhcs,bhcse->bhce', e_end, k)

    def step(carry, inp):
        c_st, n_st, m_st = carry
        cl, nl, ml, bl = inp
        m_new = jnp.maximum(bl + m_st, ml)
        a = jnp.exp(bl + m_st - m_new)
        g = jnp.exp(ml - m_new)
        c_new = a[..., None, None] * c_st + g[..., None, None] * cl
        n_new = a[..., None] * n_st + g[..., None] * nl
        return (c_new, n_new, m_new), (c_st, n_st, m_st)

    init = (jnp.zeros((b_, h_, d, d), f32), jnp.zeros((b_, h_, d), f32), jnp.zeros((b_, h_), f32))
    xs = (jnp.moveaxis(c_loc, 2, 0), jnp.moveaxis(n_loc, 2, 0), jnp.moveaxis(m_loc, 2, 0), jnp.moveaxis(b_last, 2, 0))
    _, (c_prev, n_prev, m_prev) = lax.scan(step, init, xs)
    c_prev = jnp.moveaxis(c_prev, 0, 2)
    n_prev = jnp.moveaxis(n_prev, 0, 2)
    m_prev = jnp.moveaxis(m_prev, 0, 2)
    d_log = bcum[..., :, None] - bcum[..., None, :] + li[..., None, :]
    tri = jnp.tril(jnp.ones((L, L), dtype=bool))
    d_log = jnp.where(tri, d_log, -jnp.inf)
    inter_log = bcum + m_prev[..., None]
    m_t = jnp.maximum(inter_log, jnp.max(d_log, axis=-1))
    a_t = jnp.exp(inter_log - m_t)
    s_w = jnp.einsum('bhctd,bhcsd->bhcts', q, k) * jnp.exp(d_log - m_t[..., None])
    num = a_t[..., None] * jnp.einsum('bhcde,bhcte->bhctd', c_prev, q) + jnp.einsum('bhcts,bhcsd->bhctd', s_w, v)
    den = a_t * jnp.einsum('bhce,bhcte->bhct', n_prev, q) + jnp.sum(s_w, axis=-1)
    h = num / jnp.maximum(jnp.abs(den), jnp.exp(-m_t))[..., None]
    return h.reshape(b_, h_, s_len, d)


def token_mixer(h, w_in, b_igate, b_fgate, conv_w, conv_b, w_branch_sb, w_branch_ml, w_out):
    proj = h @ w_in
    offs = []
    acc = 0
    for sz in IN_SIZES[:-1]:
        acc += sz
        offs.append(acc)
    sb_q, sb_k, sb_v, ml_q, ml_k, ml_v, ml_o, ml_i, ml_f, gate_sb, gate_ml = jnp.split(proj, offs, axis=-1)
    o_sb = merge_heads(stick_breaking_attention(split_heads(sb_q, SB_HEADS), split_heads(sb_k, SB_HEADS), split_heads(sb_v, SB_HEADS)))
    qk = jax.nn.silu(causal_conv(jnp.concatenate([ml_q, ml_k], axis=-1), conv_w, conv_b))
    ml_q, ml_k = jnp.split(qk, 2, axis=-1)
    log_i = (ml_i + b_igate).astype(jnp.float32).transpose(0, 2, 1)
    log_f = jax.nn.log_sigmoid((ml_f + b_fgate).astype(jnp.float32)).transpose(0, 2, 1)
    h_ml = mlstm_chunkwise(split_heads(ml_q, ML_HEADS), split_heads(ml_k, ML_HEADS), split_heads(ml_v, ML_HEADS), log_i, log_f)
    o_ml = jax.nn.sigmoid(ml_o) * merge_heads(h_ml).astype(h.dtype)
    y = jax.nn.sigmoid(gate_sb) * (o_sb @ w_branch_sb) + jax.nn.sigmoid(gate_ml) * (o_ml @ w_branch_ml)
    return y @ w_out


def peer_ffn(h, wq, k1, k2, u_tab, v_tab):
    b_, s_len, d = h.shape
    q = (h @ wq).astype(jnp.float32).reshape(b_, s_len, PEER_HEADS, 2, PEER_HALF)
    s1 = jnp.einsum('bshd,nd->bshn', q[..., 0, :], k1.astype(jnp.float32))
    s2 = jnp.einsum('bshd,nd->bshn', q[..., 1, :], k2.astype(jnp.float32))
    v1, i1 = lax.top_k(s1, PEER_TOPK)
    v2, i2 = lax.top_k(s2, PEER_TOPK)
    cand = (v1[..., :, None] + v2[..., None, :]).reshape(b_, s_len, PEER_HEADS, PEER_TOPK * PEER_TOPK)
    cidx = (i1[..., :, None] * PEER_KEYS + i2[..., None, :]).reshape(b_, s_len, PEER_HEADS, PEER_TOPK * PEER_TOPK)
    top_s, pos = lax.top_k(cand, PEER_TOPK)
    idx = jnp.take_along_axis(cidx, pos, axis=-1)
    gates = jax.nn.softmax(top_s, axis=-1).astype(h.dtype)
    nb = s_len // PEER_BLOCK

    def blockify(t):
        return jnp.moveaxis(t.reshape((b_, nb, PEER_BLOCK) + t.shape[2:]), 1, 0)

    def expert_block(args):
        hb, ib, gb = args
        ue = jnp.take(u_tab, ib, axis=0)
        act = jax.nn.gelu(jnp.einsum('btd,bthkd->bthk', hb, ue), approximate=False)
        ve = jnp.take(v_tab, ib, axis=0)
        return jnp.einsum('bthk,bthkd->btd', gb * act, ve)

    out = lax.map(expert_block, (blockify(h), blockify(idx), blockify(gates)))
    return jnp.moveaxis(out, 0, 1).reshape(b_, s_len, d)


def setup_inputs(seed: int = 0) -> dict:
    key = jax.random.key(seed)
    ks = jax.random.split(key, 24)
    f32 = jnp.float32
    nrm = lambda k, shape, s: jax.random.normal(k, shape, f32) * s
    dn = DEPTH
    b_f = jnp.broadcast_to(jnp.linspace(3.0, 6.0, ML_HEADS, dtype=f32), (dn, ML_HEADS)) + nrm(ks[5], (dn, ML_HEADS), 0.01)
    return {
        'x': nrm(ks[0], (BATCH, SEQ, D_MODEL), 1.0),
        'p': nrm(ks[1], (DEPTH, BATCH, SEQ, PLE_DIM), 1.0),
        'ln0_g': 1.0 + nrm(ks[2], (D_MODEL,), 0.01),
        'ln0_b': nrm(ks[3], (D_MODEL,), 0.01),
        'w_in': nrm(ks[4], (dn, D_MODEL, IN_WIDTH), D_MODEL ** -0.5),
        'b_igate': nrm(ks[6], (dn, ML_HEADS), 0.1),
        'b_fgate': b_f,
        'conv_w': nrm(ks[7], (dn, CONV_WIDTH, 2 * ML_WIDTH), CONV_WIDTH ** -0.5),
        'conv_b': nrm(ks[8], (dn, 2 * ML_WIDTH), 0.01),
        'w_branch_sb': nrm(ks[9], (dn, SB_WIDTH, D_MODEL), BETA * SB_WIDTH ** -0.5),
        'w_branch_ml': nrm(ks[10], (dn, ML_WIDTH, D_MODEL), BETA * ML_WIDTH ** -0.5),
        'w_out': nrm(ks[11], (dn, D_MODEL, D_MODEL), BETA * D_MODEL ** -0.5),
        'ln1_g': 1.0 + nrm(ks[12], (dn, D_MODEL), 0.01),
        'ln1_b': nrm(ks[13], (dn, D_MODEL), 0.01),
        'peer_wq': nrm(ks[14], (dn, D_MODEL, PEER_HEADS * PEER_QDIM), D_MODEL ** -0.5),
        'peer_k1': nrm(ks[15], (dn, PEER_KEYS, PEER_HALF), PEER_HALF ** -0.5),
        'peer_k2': nrm(ks[16], (dn, PEER_KEYS, PEER_HALF), PEER_HALF ** -0.5),
        'peer_u': nrm(ks[17], (dn, PEER_EXPERTS, D_MODEL), D_MODEL ** -0.5),
        'peer_v': nrm(ks[18], (dn, PEER_EXPERTS, D_MODEL), BETA * PEER_HEADS ** -0.5),
        'w_ple_gate': nrm(ks[19], (dn, D_MODEL, D_MODEL), D_MODEL ** -0.5),
        'w_ple': nrm(ks[20], (dn, PLE_DIM, D_MODEL), PLE_DIM ** -0.5),
        'ln2_g': 1.0 + nrm(ks[21], (dn, D_MODEL), 0.01),
        'ln2_b': nrm(ks[22], (dn, D_MODEL), 0.01),
    }


def reference(x, p, ln0_g, ln0_b, w_in, b_igate, b_fgate, conv_w, conv_b, w_branch_sb, w_branch_ml, w_out, ln1_g, ln1_b, peer_wq, peer_k1, peer_k2, peer_u, peer_v, w_ple_gate, w_ple, ln2_g, ln2_b):
    h = layer_norm(x, ln0_g, ln0_b)
    for i in range(DEPTH):
        mix = token_mixer(h, w_in[i], b_igate[i], b_fgate[i], conv_w[i], conv_b[i], w_branch_sb[i], w_branch_ml[i], w_out[i])
        h = layer_norm(ALPHA * h + mix, ln1_g[i], ln1_b[i])
        ffn = peer_ffn(h, peer_wq[i], peer_k1[i], peer_k2[i], peer_u[i], peer_v[i])
        ple = jax.nn.sigmoid(h @ w_ple_gate[i]) * (p[i] @ w_ple[i])
        h = layer_norm(ALPHA * h + ffn + ple, ln2_g[i], ln2_b[i])
    return h
```

```python
import numpy as np
from contextlib import ExitStack
import concourse.bass as bass
import concourse.mybir as mybir
from concourse.bass_utils import run_bass_kernel_spmd

F32 = mybir.dt.float32
BF16 = mybir.dt.bfloat16
I32 = mybir.dt.int32
U32 = mybir.dt.uint32
AF = mybir.ActivationFunctionType
ALU = mybir.AluOpType
AX = mybir.AxisListType

D = 1024
SEQ = 8192
NB = 64
NOWN = 32
ALPHA = 2.0 ** 0.25
EPS = 1e-5
ENGS = ['pe', 'act', 'dve', 'pool', 'sp']
import os
SERIALIZE = False
SER_P1, SER_P3, SER_P4A, SER_P4B = False, False, False, False


class Res:
    __slots__ = ('w', 'r')

    def __init__(self):
        self.w = None
        self.r = {}


class Tile:
    def __init__(self, t):
        self.t = t
        self.res = Res()
        self._sub = {}
        self.dkey = None
        self.dcount = 0

    def __getitem__(self, k):
        return self.t[k]

    def sub(self, k):
        if k not in self._sub:
            self._sub[k] = Res()
        return self._sub[k]


class Prog:
    def __init__(self, nc, es):
        self.nc = nc
        self.es = es
        self.ops = {e: [] for e in ENGS}
        self.cnt = {e: 0 for e in ENGS}
        self.dcnt = {e: 0 for e in ENGS}
        self.seen = {e: {} for e in ENGS}
        self.sem = {}
        for e in ENGS:
            self.sem[('c', e)] = es.enter_context(nc.semaphore('c_' + e))
        self.dtiles = []
        self.log = []
        self.serialize = SERIALIZE

    def sb(self, name, shape, dt, es=None):
        return Tile((es or self.es).enter_context(self.nc.sbuf_tensor(name, shape, dt)))

    def ps(self, name, shape, dt, es=None):
        return Tile((es or self.es).enter_context(self.nc.psum_tensor(name, shape, dt)))

    def barrier(self):
        cur = []
        for e in ENGS:
            if self.cnt[e]:
                cur.append((('c', e), self.cnt[e]))
        for t in self.dtiles:
            cur.append((t.dkey, 16 * t.dcount))
        for e in ENGS:
            waits = [(k, v) for k, v in cur if self.seen[e].get(k, 0) < v and not (e == 'pe' and k == ('c', 'pe'))]
            for k, v in waits:
                self.seen[e][k] = v
            self.ops[e].append((waits, None, None, 0))
            self.log.append((e, 'barrier', waits, None))

    def _deps(self, eng, reads, writes):
        waits = {}
        seen = self.seen[eng]

        def add(k, v):
            if seen.get(k, 0) >= v:
                return
            if waits.get(k, 0) < v:
                waits[k] = v

        for r in reads:
            if r.w is not None:
                add(*r.w)
        for w in writes:
            if w.w is not None:
                add(*w.w)
            for k, v in w.r.items():
                add(k, v)
        for k, v in waits.items():
            seen[k] = v
        return list(waits.items())

    def _record(self, ev, reads, writes):
        k, v = ev
        for r in reads:
            if r.r.get(k, 0) < v:
                r.r[k] = v
        for w in writes:
            w.w = ev
            w.r = {}

    def op(self, eng, fn, reads=(), writes=()):
        reads = [x.res if isinstance(x, Tile) else x for x in reads]
        writes = [x.res if isinstance(x, Tile) else x for x in writes]
        waits = self._deps(eng, reads, writes)
        self.cnt[eng] += 1
        ev = (('c', eng), self.cnt[eng])
        if eng == 'pe':
            self.seen[eng][ev[0]] = ev[1]
        self._record(ev, reads, writes)
        self.ops[eng].append((waits, fn, ev[0], 1))
        self.log.append((eng, 'op', waits, ev))
        if self.serialize:
            self.barrier()

    def dma(self, eng, fn, reads=(), writes=()):
        tiles = [x for x in list(writes) + list(reads) if isinstance(x, Tile)]
        owner = tiles[0]
        if owner.dkey is None:
            owner.dkey = ('t', len(self.dtiles))
            self.sem[owner.dkey] = self.es.enter_context(self.nc.semaphore('t%d' % len(self.dtiles)))
            self.dtiles.append(owner)
        reads = [x.res if isinstance(x, Tile) else x for x in reads]
        writes = [x.res if isinstance(x, Tile) else x for x in writes]
        waits = self._deps(eng, reads, writes)
        owner.dcount += 1
        ev = (owner.dkey, 16 * owner.dcount)
        self._record(ev, reads, writes)
        self.ops[eng].append((waits, fn, ev[0], 16))
        self.log.append((eng, 'dma', waits, ev))
        if self.serialize:
            self.barrier()

    def emit(self):
        nc = self.nc
        handles = {'pe': 'tensor', 'act': 'scalar', 'dve': 'vector', 'pool': 'gpsimd', 'sp': 'sync'}
        with nc.Block() as blk:
            for e in ENGS:
                ops = self.ops[e]
                final = None
                if e == 'sp':
                    final = [(t.dkey, 16 * t.dcount) for t in self.dtiles]

                def body(eng, ops=ops, final=final):
                    for waits, fn, key, inc in ops:
                        for k, v in waits:
                            eng.wait_ge(self.sem[k], v)
                        if fn is not None:
                            fn(eng).then_inc(self.sem[key], inc)
                    if final:
                        for k, v in final:
                            eng.wait_ge(self.sem[k], v)

                getattr(blk, handles[e])(body)


def _r(x):
    return [x] if not isinstance(x, (list, tuple)) else list(x)


def build_nc(NBLK=NB, with_mix=True, with_peer=True, debug=False, stop_after=None):
    T = NBLK * 128
    nc = bass.Bass("TRN2", target_bir_lowering=False)
    dr = lambda name, shape, dt=F32, kind="ExternalInput": nc.dram_tensor(name, shape, dt, kind=kind).ap()
    SCR = "ExternalOutput" if debug else "Internal"
    x_all = dr("x_all", [T, D])
    p_all = dr("p_all", [T, 256])
    ln_g = [dr(f"ln{i}_g", [D]) for i in range(3)]
    ln_b = [dr(f"ln{i}_b", [D]) for i in range(3)]
    WPIECES = ((0, 1024), (1024, 2048), (2048, 3072), (3072, 3592), (3592, 4616), (4616, 5640))
    w_in_p = [dr(f"w_in_p{i}", [D, c1 - c0]) for i, (c0, c1) in enumerate(WPIECES)]
    conv_wb = dr("conv_wb", [5, D])
    gate_b = dr("gate_b", [1, 8])
    w_bsb = dr("w_branch_sb", [512, D]); w_bml = dr("w_branch_ml", [512, D]); w_out = dr("w_out", [D, D])
    HNS = dr("hns", [T, D], F32, SCR)
    QTS = dr("qts", [8, 64, T], BF16, SCR)
    KTS = dr("kts", [8, 64, T], BF16, SCR)
    VS = dr("vs", [T, 512], BF16, SCR)
    OMLS = dr("omls", [T, 512], BF16, SCR)
    OSBT = dr("osbt", [4, 128, T], BF16, SCR)
    w_pg = dr("w_ple_gate", [D, D])
    w_ple = dr("w_ple", [256, D])
    peer_wq = dr("peer_wq", [D, 2048])
    peer_k1 = dr("peer_k1", [128, 128])
    peer_k2 = dr("peer_k2", [128, 128])
    peer_u = dr("peer_u", [16384, D])
    peer_v = dr("peer_v", [16384, D])
    out = dr("out", [T, D], F32, "ExternalOutput")
    H1S = dr("h1s", [T, D], F32, SCR)
    DBG = dr("dbg", [T, 4, 128], F32, "ExternalOutput") if debug else None

    with ExitStack() as es:
        P = Prog(nc, es)
        ident = P.sb("ident", [128, 128], BF16)
        identf = P.sb("identf", [128, 128], F32)
        epst = P.sb("epst", [128, 1], F32)
        for idt in (ident, identf):
            P.op('pool', lambda e, idt=idt: e.memset(idt[:], 1.0), [], [idt])
            P.op('pool', lambda e, idt=idt: e.affine_select(out=idt[:], in_=idt[:], pattern=[[-1, 128]],
                                                            compare_op=ALU.is_equal, fill=0.0, base=0,
                                                            channel_multiplier=1), [idt], [idt])
        P.op('pool', lambda e: e.memset(epst[:], EPS), [], [epst])
        onet = P.sb("onet", [128, 1], F32)
        P.op('pool', lambda e: e.memset(onet[:], 1.0), [], [onet])
        def ln_params(i, ph, tag):
            g = P.sb(f"{tag}_lng{i}", [128, D], F32, ph); b = P.sb(f"{tag}_lnb{i}", [128, D], F32, ph)
            P.dma('sp', lambda e: e.dma_start(out=g[:], in_=ln_g[i].partition_broadcast(128)), [], [g])
            P.dma('sp', lambda e: e.dma_start(out=b[:], in_=ln_b[i].partition_broadcast(128)), [], [b])
            return (g, b)
        pb = [P.ps(f"pb{i}", [128, 512], F32) for i in range(4)]

        def layer_norm(src, dst, k, st, mv, sd, rstd, xn):
            P.op('dve', lambda e: e.bn_stats(out=st[:, 0:6], in_=src[:, 0:512]), [src], [st])
            P.op('dve', lambda e: e.bn_stats(out=st[:, 6:12], in_=src[:, 512:1024]), [src, st], [st])
            P.op('dve', lambda e: e.bn_aggr(out=mv[:], in_=st[:]), [st], [mv])
            P.op('act', lambda e: e.activation(out=sd[:], in_=mv[:, 1:2], func=AF.Sqrt, bias=epst[:], scale=1.0),
                 [mv, epst], [sd])
            P.op('dve', lambda e: e.reciprocal(out=rstd[:], in_=sd[:]), [sd], [rstd])
            P.op('dve', lambda e: e.tensor_scalar(out=xn[:], in0=src[:], scalar1=mv[:, 0:1], scalar2=rstd[:],
                                                  op0=ALU.subtract, op1=ALU.mult), [src, mv, rstd], [xn])
            P.op('pool', lambda e: e.tensor_tensor(out=xn[:], in0=xn[:], in1=k[0][:], op=ALU.mult),
                 [xn, k[0]], [xn])
            P.op('pool', lambda e: e.tensor_tensor(out=dst[:], in0=xn[:], in1=k[1][:], op=ALU.add),
                 [xn, k[1]], [dst])

        def load_w(dst, src_ap, nchunk, c0=0, c1=None):
            for c in range(nchunk):
                sa = src_ap[c * 128:(c + 1) * 128, :] if c1 is None else src_ap[c * 128:(c + 1) * 128, c0:c1]
                P.dma('pool', lambda e, c=c, sa=sa: e.dma_start(out=dst[:, c, :], in_=sa, max_dma_last_dim=4096),
                      [], [dst])

        def transpose_to(src_b, nchunk, psb, dstT):
            pv = psb[:].bitcast(BF16)
            for c in range(nchunk):
                P.op('pe', lambda e, c=c: e.transpose(out=pv[:, c * 128:(c + 1) * 128],
                                                      in_=src_b[:, c * 128:(c + 1) * 128], identity=ident[:]),
                     [src_b, ident], [psb])
            P.op('dve', lambda e: e.tensor_copy(out=dstT[:].rearrange("p c t -> p (c t)"),
                                                in_=pv[:, 0:nchunk * 128]), [psb], [dstT])

        if with_mix:
            P.serialize = SER_P1
            def _phase0(ph):
                wA = P.sb("p1_wA", [128, 8, 3592], BF16, ph)
                for pi in range(4):
                    c0, c1 = WPIECES[pi]
                    for c in range(8):
                        P.dma('pool', lambda e, c=c, c0=c0, c1=c1, pi=pi: e.dma_start(
                            out=wA[:, c, c0:c1], in_=w_in_p[pi][c * 128:(c + 1) * 128, :], max_dma_last_dim=4096), [], [wA])
                L0 = ln_params(0, ph, "p1")
                cw = P.sb("p1_cw", [128, 8, 5], F32, ph)
                cw5 = P.sb("p1_cw5", [5, D], F32, ph)
                P.dma('sp', lambda e: e.dma_start(out=cw5[:], in_=conv_wb), [], [cw5])
                for c in range(8):
                    P.op('pe', lambda e, c=c: e.transpose(out=pb[0][:, c * 5:(c + 1) * 5], in_=cw5[0:5, c * 128:(c + 1) * 128],
                                                          identity=identf[0:5, 0:5]), [cw5, identf], [pb[0]])
                P.op('dve', lambda e: e.tensor_copy(out=cw[:].rearrange("p c j -> p (c j)"), in_=pb[0][:, 0:40]), [pb[0]], [cw])
                onesF = P.sb("p1_onesF", [128, 128], F32, ph)
                P.op('pool', lambda e: e.memset(onesF[:], 1.0), [], [onesF])
                gb1 = P.sb("p1_gb1", [1, 8], F32, ph)
                P.dma('sp', lambda e: e.dma_start(out=gb1[:], in_=gate_b), [], [gb1])
                gbb = P.sb("p1_gbb", [128, 8], F32, ph)
                P.op('pe', lambda e: e.matmul(pb[1][:, 0:8], lhsT=onesF[0:1, :], rhs=gb1[:], start=True, stop=True), [onesF, gb1], [pb[1]])
                P.op('dve', lambda e: e.tensor_copy(out=gbb[:], in_=pb[1][:, 0:8]), [pb[1]], [gbb])
                triLE = P.sb("p1_triLE", [128, 128], F32, ph)
                P.op('pool', lambda e: e.memset(triLE[:], 1.0), [], [triLE])
                P.op('pool', lambda e: e.affine_select(out=triLE[:], in_=triLE[:], pattern=[[1, 128]], compare_op=ALU.is_ge,
                                                       fill=0.0, base=0, channel_multiplier=-1), [triLE], [triLE])
                xt = [P.sb(f"p1_xt{i}", [128, D], F32, ph) for i in range(2)]
                st = P.sb("p1_st", [128, 12], F32, ph); mv = P.sb("p1_mv", [128, 2], F32, ph)
                sd = P.sb("p1_sd", [128, 1], F32, ph); rstd = P.sb("p1_rstd", [128, 1], F32, ph)
                xn = P.sb("p1_xn", [128, D], F32, ph)
                hn = [P.sb(f"p1_hn{i}", [128, D], F32, ph) for i in range(2)]
                hnb = P.sb("p1_hnb", [128, D], BF16, ph)
                hnT = P.sb("p1_hnT", [128, 8, 128], BF16, ph)
                qTb = [P.sb(f"p1_qTb{i}", [64, 8, 128], BF16, ph) for i in range(2)]
                kTb = [P.sb(f"p1_kTb{i}", [64, 8, 128], BF16, ph) for i in range(2)]
                xraw = P.sb("p1_xraw", [128, 8, 131], F32, ph)
                cacc = P.sb("p1_cacc", [128, 8, 128], F32, ph)
                ctmp = P.sb("p1_ctmp", [128, 8, 128], F32, ph)
                mqk = P.sb("p1_mqk", [128, 8, 128], BF16, ph)
                vb = [P.sb(f"p1_vb{i}", [128, 512], BF16, ph) for i in range(2)]
                mva = P.sb("p1_mva", [128, 4, 129], BF16, ph)
                sgo = P.sb("p1_sgo", [128, 512], F32, ph)
                ifr = P.sb("p1_ifr", [128, 8], F32, ph)
                li = P.sb("p1_li", [128, 4], F32, ph); fz = P.sb("p1_fz", [128, 4], F32, ph)
                l1 = P.sb("p1_l1", [128, 4], F32, ph); gtmp = P.sb("p1_gtmp", [128, 4], F32, ph)
                gs = P.sb("p1_gs", [128, 4], F32, ph); eq = P.sb("p1_eq", [128, 4], F32, ph); eb = P.sb("p1_eb", [128, 4], F32, ph)
                STt = P.sb("p1_ST", [128, 128], BF16, ph)
                ktil = P.sb("p1_ktil", [128, 128], BF16, ph)
                C32 = [P.sb(f"p1_C32_{h}", [128, 129], F32, ph) for h in range(4)]
                C16 = [P.sb(f"p1_C16_{h}", [128, 129], BF16, ph) for h in range(4)]
                tmpC = P.sb("p1_tmpC", [128, 129], F32, ph)
                dn = P.sb("p1_dn", [128, 1], F32, ph); scl = P.sb("p1_scl", [128, 1], F32, ph)
                oml = [P.sb(f"p1_oml{i}", [128, 512], BF16, ph) for i in range(2)]
                P.op('pool', lambda e: e.memset(xraw[:], 0.0), [], [xraw])
                P.op('pool', lambda e: e.memset(mva[:], 1.0), [], [mva])
                for h in range(4):
                    P.op('pool', lambda e, h=h: e.memset(C32[h][:], 0.0), [], [C32[h]])
                    P.op('pool', lambda e, h=h: e.memset(C16[h][:], 0.0), [], [C16[h]])
                LNSC = float(np.log(128.0 ** -0.5))
                lnsc = P.sb("p1_lnsc", [128, 1], F32, ph); onec = P.sb("p1_onec", [128, 1], F32, ph)
                P.op('pool', lambda e: e.memset(lnsc[:], LNSC), [], [lnsc])
                P.op('pool', lambda e: e.memset(onec[:], 1.0), [], [onec])

                for s in range(NBLK):
                    rows = slice(s * 128, (s + 1) * 128)
                    tcol = slice(s * 128, (s + 1) * 128)
                    xb = xt[s % 2]; hb = hn[s % 2]; qb = qTb[s % 2]; kb_ = kTb[s % 2]; vbb = vb[s % 2]; omb = oml[s % 2]
                    P.dma('sp', lambda e, xb=xb, rows=rows: e.dma_start(out=xb[:], in_=x_all[rows, :]), [], [xb])
                    layer_norm(xb, hb, L0, st, mv, sd, rstd, xn)
                    P.dma('sp', lambda e, hb=hb, rows=rows: e.dma_start(out=HNS[rows, :], in_=hb[:]), [hb], [])
                    P.op('act', lambda e, hb=hb: e.activation(out=hnb[:], in_=hb[:], func=AF.Copy), [hb], [hnb])
                    transpose_to(hnb, 8, pb[0], hnT)
                    for which, col0, dstb, scale, banks in ((0, 0, qb, 1.0, (0, 1)), (1, 512, kb_, 0.125, (2, 3))):
                        for h in range(8):
                            bk = pb[banks[h // 4]]; cs = slice((h % 4) * 128, (h % 4 + 1) * 128)
                            for c in range(8):
                                P.op('pe', lambda e, bk=bk, cs=cs, c=c, h=h, col0=col0: e.matmul(
                                    bk[0:64, cs], lhsT=wA[:, c, col0 + h * 64:col0 + (h + 1) * 64], rhs=hnT[:, c, :],
                                    start=(c == 0), stop=(c == 7)), [wA, hnT], [bk])
                        for hh in range(2):
                            P.op('act', lambda e, hh=hh, dstb=dstb, scale=scale, banks=banks: e.activation(
                                out=dstb[:, hh * 4:(hh + 1) * 4, :].rearrange("p h t -> p (h t)"), in_=pb[banks[hh]][0:64, :],
                                func=AF.Copy, scale=scale), [pb[banks[hh]]], [dstb])
                    P.dma('sp', lambda e, qb=qb, tcol=tcol: e.dma_start(out=QTS[:, :, tcol].rearrange("h d t -> d h t"), in_=qb[:]), [qb], [])
                    P.dma('sp', lambda e, kb_=kb_, tcol=tcol: e.dma_start(out=KTS[:, :, tcol].rearrange("h d t -> d h t"), in_=kb_[:]), [kb_], [])
                    for cc in range(8):
                        bk = pb[cc // 4]; cs = slice((cc % 4) * 128, (cc % 4 + 1) * 128)
                        for c in range(8):
                            P.op('pe', lambda e, bk=bk, cs=cs, c=c, cc=cc: e.matmul(
                                bk[:, cs], lhsT=wA[:, c, 1536 + cc * 128:1536 + (cc + 1) * 128], rhs=hnT[:, c, :],
                                start=(c == 0), stop=(c == 7)), [wA, hnT], [bk])
                    for hh in range(2):
                        P.op('act', lambda e, hh=hh: e.activation(out=xraw[:, hh * 4:(hh + 1) * 4, 3:131],
                                                                  in_=pb[hh][:].rearrange("p (c t) -> p c t", c=4),
                                                                  func=AF.Copy), [pb[hh]], [xraw])
                    for col0, bk, n in ((1024, pb[2], 512), (2560, pb[3], 512), (3072, pb[0], 512), (3584, pb[1], 8)):
                        for c in range(8):
                            P.op('pe', lambda e, bk=bk, c=c, col0=col0, n=n: e.matmul(
                                bk[:, 0:n], lhsT=hnT[:, c, :], rhs=wA[:, c, col0:col0 + n],
                                start=(c == 0), stop=(c == 7)), [wA, hnT], [bk])
                    P.op('act', lambda e, vbb=vbb: e.activation(out=vbb[:], in_=pb[2][:], func=AF.Copy), [pb[2]], [vbb])
                    P.dma('sp', lambda e, vbb=vbb, rows=rows: e.dma_start(out=VS[rows, :], in_=vbb[:]), [vbb], [])
                    P.op('act', lambda e: e.activation(out=mva[:, :, 0:128], in_=pb[3][:].rearrange("p (h d) -> p h d", h=4),
                                                       func=AF.Copy), [pb[3]], [mva])
                    P.op('act', lambda e: e.activation(out=sgo[:], in_=pb[0][:], func=AF.Sigmoid), [pb[0]], [sgo])
                    P.op('dve', lambda e: e.tensor_copy(out=ifr[:], in_=pb[1][:, 0:8]), [pb[1]], [ifr])
                    wb = lambda j: cw[:, :, j:j + 1].to_broadcast([128, 8, 128])
                    P.op('dve', lambda e: e.tensor_tensor(out=cacc[:], in0=xraw[:, :, 3:131], in1=wb(0), op=ALU.mult), [xraw, cw], [cacc])
                    P.op('dve', lambda e: e.tensor_tensor(out=cacc[:], in0=cacc[:], in1=cw[:, :, 4:5].to_broadcast([128, 8, 128]),
                                                          op=ALU.add), [cacc, cw], [cacc])
                    for j in range(1, 4):
                        P.op('dve', lambda e, j=j: e.tensor_tensor(out=ctmp[:], in0=xraw[:, :, 3 - j:131 - j], in1=wb(j), op=ALU.mult),
                             [xraw, cw], [ctmp])
                        P.op('dve', lambda e: e.tensor_tensor(out=cacc[:], in0=cacc[:], in1=ctmp[:], op=ALU.add), [cacc, ctmp], [cacc])
                    P.op('act', lambda e: e.activation(out=mqk[:], in_=cacc[:], func=AF.Silu), [cacc], [mqk])
                    P.op('dve', lambda e: e.tensor_copy(out=ctmp[:, :, 0:3], in_=xraw[:, :, 128:131]), [xraw], [ctmp])
                    P.op('dve', lambda e: e.tensor_copy(out=xraw[:, :, 0:3], in_=ctmp[:, :, 0:3]), [ctmp], [xraw])
                    P.op('dve', lambda e: e.tensor_tensor(out=li[:], in0=ifr[:, 0:4], in1=gbb[:, 0:4], op=ALU.add), [ifr, gbb], [li])
                    P.op('dve', lambda e: e.tensor_tensor(out=fz[:], in0=ifr[:, 4:8], in1=gbb[:, 4:8], op=ALU.add), [ifr, gbb], [fz])
                    P.op('act', lambda e: e.activation(out=fz[:], in_=fz[:], func=AF.Exp, scale=-1.0), [fz], [fz])
                    P.op('act', lambda e: e.activation(out=l1[:], in_=fz[:], func=AF.Ln, bias=onec[:], scale=1.0), [fz, onec], [l1])
                    P.op('pe', lambda e: e.matmul(pb[1][:, 16:20], lhsT=triLE[:], rhs=l1[:], start=True, stop=True), [triLE, l1], [pb[1]])
                    P.op('pe', lambda e: e.matmul(pb[1][:, 32:36], lhsT=onesF[:], rhs=l1[:], start=True, stop=True), [onesF, l1], [pb[1]])
                    P.op('dve', lambda e: e.tensor_tensor(out=gtmp[:], in0=li[:], in1=pb[1][:, 16:20], op=ALU.add), [li, pb[1]], [gtmp])
                    P.op('act', lambda e: e.activation(out=gs[:], in_=gtmp[:], func=AF.Exp, bias=lnsc[:], scale=1.0), [gtmp, lnsc], [gs])
                    P.op('act', lambda e: e.activation(out=eq[:], in_=pb[1][:, 16:20], func=AF.Exp, scale=-1.0), [pb[1]], [eq])
                    P.op('act', lambda e: e.activation(out=eb[:], in_=pb[1][:, 32:36], func=AF.Exp, scale=-1.0), [pb[1]], [eb])
                    for h in range(4):
                        qTh_ = mqk[:, h, :]; kTh_ = mqk[:, 4 + h, :]
                        P.op('pe', lambda e, h=h: e.matmul(pb[2][:, 0:128], lhsT=mqk[:, 4 + h, :], rhs=mqk[:, h, :], start=True, stop=True),
                             [mqk], [pb[2]])
                        P.op('dve', lambda e, h=h: e.scalar_tensor_tensor(out=STt[:], in0=pb[2][:, 0:128], scalar=gs[:, h:h + 1],
                                                                          in1=triLE[:], op0=ALU.mult, op1=ALU.mult),
                             [pb[2], gs, triLE], [STt])
                        pv3 = pb[3][:].bitcast(BF16)
                        P.op('pe', lambda e, h=h, pv3=pv3: e.transpose(out=pv3[:, 0:128], in_=mqk[:, 4 + h, :], identity=ident[:]),
                             [mqk, ident], [pb[3]])
                        P.op('act', lambda e, h=h, pv3=pv3: e.activation(out=ktil[:], in_=pv3[:, 0:128], func=AF.Copy, scale=gs[:, h:h + 1]),
                             [pb[3], gs], [ktil])
                        P.op('pe', lambda e, h=h: e.matmul(pb[0][:, 0:129], lhsT=STt[:], rhs=mva[:, h, :], start=True, stop=False),
                             [STt, mva], [pb[0]])
                        P.op('pe', lambda e, h=h: e.matmul(pb[0][:, 0:129], lhsT=mqk[:, h, :], rhs=C16[h][:], start=False, stop=True),
                             [mqk, C16[h]], [pb[0]])
                        P.op('pe', lambda e, h=h: e.matmul(pb[2][:, 256:385], lhsT=ktil[:], rhs=mva[:, h, :], start=True, stop=True),
                             [ktil, mva], [pb[2]])
                        P.op('act', lambda e, h=h: e.activation(out=tmpC[:], in_=C32[h][:], func=AF.Copy, scale=eb[:, h:h + 1]),
                             [C32[h], eb], [tmpC])
                        P.op('dve', lambda e, h=h: e.scalar_tensor_tensor(out=C32[h][:], in0=pb[2][:, 256:385], scalar=eb[:, h:h + 1],
                                                                          in1=tmpC[:], op0=ALU.mult, op1=ALU.add),
                             [pb[2], eb, tmpC], [C32[h]])
                        P.op('act', lambda e, h=h: e.activation(out=C16[h][:], in_=C32[h][:], func=AF.Copy), [C32[h]], [C16[h]])
                        P.op('act', lambda e, h=h: e.activation(out=dn[:], in_=pb[0][:, 128:129], func=AF.Abs, scale=eq[:, h:h + 1]),
                             [pb[0], eq], [dn])
                        P.op('dve', lambda e: e.tensor_single_scalar(out=dn[:], in_=dn[:], scalar=1.0, op=ALU.max), [dn], [dn])
                        P.op('dve', lambda e: e.reciprocal(out=dn[:], in_=dn[:]), [dn], [dn])
                        P.op('dve', lambda e, h=h: e.tensor_tensor(out=scl[:], in0=dn[:], in1=eq[:, h:h + 1], op=ALU.mult), [dn, eq], [scl])
                        P.op('dve', lambda e, h=h, omb=omb: e.scalar_tensor_tensor(
                            out=omb[:, h * 128:(h + 1) * 128], in0=pb[0][:, 0:128], scalar=scl[:], in1=sgo[:, h * 128:(h + 1) * 128],
                            op0=ALU.mult, op1=ALU.mult), [pb[0], scl, sgo], [omb])
                    P.dma('sp', lambda e, omb=omb, rows=rows: e.dma_start(out=OMLS[rows, :], in_=omb[:]), [omb], [])
                P.barrier()
            with ExitStack() as ph:
                _phase0(ph)
            if stop_after == 'p1':
                P.emit()
                return nc

            P.serialize = SER_P3
            def _phase1(ph):
                negtri = P.sb("p3_negtri", [128, 128], BF16, ph)
                triGE = P.sb("p3_triGE", [128, 128], BF16, ph)
                onesb = P.sb("p3_onesb", [128, 128], BF16, ph)
                zerob = P.sb("p3_zerob", [128, 512], BF16, ph)
                P.op('pool', lambda e: e.memset(onesb[:], 1.0), [], [onesb])
                P.op('pool', lambda e: e.memset(zerob[:], 0.0), [], [zerob])
                for tt, val in ((negtri, -30000.0), (triGE, 1.0)):
                    P.op('pool', lambda e, tt=tt, val=val: e.memset(tt[:], val), [], [tt])
                    P.op('pool', lambda e, tt=tt: e.affine_select(out=tt[:], in_=tt[:], pattern=[[-1, 128]], compare_op=ALU.is_ge,
                                                                  fill=0.0, base=0, channel_multiplier=1), [tt], [tt])
                kTh_t = [P.sb(f"p3_kT{i}", [64, T], BF16, ph) for i in range(2)]
                qTh_t = [P.sb(f"p3_qT{i}", [64, T], BF16, ph) for i in range(2)]
                vhp = P.sb("p3_vhp", [128, NBLK, 128], BF16, ph)
                e32 = [P.sb(f"p3_e32_{i}", [128, 512], F32, ph) for i in range(2)]
                sp16 = [P.sb(f"p3_sp16_{i}", [128, 512], BF16, ph) for i in range(2)]
                g32 = [P.sb(f"p3_g32_{i}", [128, 512], F32, ph) for i in range(2)]
                A16 = [P.sb(f"p3_A16_{i}", [128, 512], BF16, ph) for i in range(2)]
                R32 = P.sb("p3_R32", [128, 512], F32, ph)
                R16 = [P.sb(f"p3_R16_{i}", [128, 512], BF16, ph) for i in range(2)]
                obuf = [P.sb(f"p3_obuf{i}", [128, 512], BF16, ph) for i in range(2)]
                pz = [pb[0], pb[1]]; pc = [pb[2], pb[3]]
                po = [P.ps(f"p3_po{i}", [128, 512], F32, ph) for i in range(2)]
                stepn = 0; gcnt = 0
                for h in range(8):
                    hp, hq = h // 2, h % 2
                    kt = kTh_t[h % 2]; qt = qTh_t[h % 2]
                    P.dma('sp', lambda e, kt=kt, h=h: e.dma_start(out=kt[:], in_=KTS[h]), [], [kt])
                    P.dma('sp', lambda e, qt=qt, h=h: e.dma_start(out=qt[:], in_=QTS[h]), [], [qt])
                    if hq == 0:
                        P.dma('sp', lambda e, hp=hp: e.dma_start(
                            out=vhp[:], in_=VS[:, hp * 128:(hp + 1) * 128].rearrange("(kb s) c -> s kb c", s=128)), [], [vhp])
                    for G in range(NBLK // 4):
                        pob = po[gcnt % 2]; ob_ = obuf[gcnt % 2]; gcnt += 1
                        P.op('pool', lambda e: e.memset(R32[:], 0.0), [], [R32])
                        P.op('pe', lambda e, pob=pob: e.matmul(pob[:], lhsT=onesb[:], rhs=zerob[:], start=True, stop=False),
                             [onesb, zerob], [pob])
                        first = True
                        for kb in range(4 * G + 3, -1, -1):
                            off = max(0, kb - 4 * G) * 128
                            rng = slice(off, 512)
                            i2 = stepn % 2; stepn += 1
                            z = pz[i2]; cps = pc[i2]; ee = e32[i2]; ss = sp16[i2]; gg = g32[i2]; aa = A16[i2]
                            diag = kb >= 4 * G
                            P.op('pe', lambda e, z=z, rng=rng, kt=kt, qt=qt, kb=kb, G=G, off=off, diag=diag: e.matmul(
                                z[:, rng], lhsT=kt[:, kb * 128:(kb + 1) * 128], rhs=qt[:, G * 512 + off:(G + 1) * 512],
                                start=True, stop=(not diag)), [kt, qt], [z])
                            if diag:
                                P.op('pe', lambda e, z=z, off=off: e.matmul(z[:, off:off + 128], lhsT=ident[:], rhs=negtri[:],
                                                                            start=False, stop=True), [ident, negtri], [z])
                            P.op('act', lambda e, z=z, ee=ee, rng=rng: e.activation(out=ee[:, rng], in_=z[:, rng], func=AF.Exp), [z], [ee])
                            P.op('act', lambda e, ss=ss, ee=ee, rng=rng: e.activation(out=ss[:, rng], in_=ee[:, rng], func=AF.Ln,
                                                                                      bias=onet[:], scale=1.0), [ee, onet], [ss])
                            Rr = R16[(stepn) % 2]; Rw = R16[(stepn + 1) % 2]
                            P.op('pe', lambda e, cps=cps, ss=ss, rng=rng, first=first: e.matmul(
                                cps[:, rng], lhsT=triGE[:], rhs=ss[:, rng], start=True, stop=first), [triGE, ss], [cps])
                            if not first:
                                P.op('pe', lambda e, cps=cps, Rr=Rr, rng=rng: e.matmul(
                                    cps[:, rng], lhsT=onesb[:], rhs=Rr[:, rng], start=False, stop=True), [onesb, Rr], [cps])
                            P.op('act', lambda e, cps=cps, gg=gg, rng=rng: e.activation(out=gg[:, rng], in_=cps[:, rng], func=AF.Exp,
                                                                                        scale=-1.0), [cps], [gg])
                            P.op('dve', lambda e, aa=aa, ee=ee, gg=gg, rng=rng: e.tensor_tensor(out=aa[:, rng], in0=ee[:, rng],
                                                                                               in1=gg[:, rng], op=ALU.mult),
                                 [ee, gg], [aa])
                            P.op('pe', lambda e, pob=pob, aa=aa, rng=rng, kb=kb: e.matmul(
                                pob[:, rng], lhsT=vhp[:, kb, :], rhs=aa[:, rng], start=False, stop=(kb == 0)), [vhp, aa], [pob])
                            if kb > 0:
                                P.op('pool', lambda e, ss=ss, rng=rng: e.tensor_tensor(out=R32[:, rng], in0=R32[:, rng], in1=ss[:, rng],
                                                                                      op=ALU.add), [R32, ss], [R32])
                                P.op('pool', lambda e, Rw=Rw: e.tensor_copy(out=Rw[:], in_=R32[:]), [R32], [Rw])
                            first = False
                        prow = slice(64 * hq, 64 * hq + 64)
                        P.op('act', lambda e, pob=pob, ob_=ob_, prow=prow: e.activation(out=ob_[prow, :], in_=pob[prow, :], func=AF.Copy),
                             [pob], [ob_])
                        P.dma('sp', lambda e, ob_=ob_, prow=prow, hp=hp, G=G: e.dma_start(
                            out=OSBT[hp, prow, G * 512:(G + 1) * 512], in_=ob_[prow, :]), [ob_], [])
                P.barrier()
            with ExitStack() as ph:
                _phase1(ph)
            if stop_after == 'p3':
                P.emit()
                return nc

            P.serialize = SER_P4A
            def _phase2(ph):
                wG = P.sb("p4_wG", [128, 8, 2048], BF16, ph)
                for pi in (4, 5):
                    c0, c1 = WPIECES[pi]
                    for c in range(8):
                        P.dma('pool', lambda e, c=c, c0=c0, c1=c1, pi=pi: e.dma_start(
                            out=wG[:, c, c0 - 3592:c1 - 3592], in_=w_in_p[pi][c * 128:(c + 1) * 128, :], max_dma_last_dim=4096), [], [wG])
                wsb = P.sb("p4_wsb", [128, 4, D], BF16, ph); wml = P.sb("p4_wml", [128, 4, D], BF16, ph)
                wo = P.sb("p4_wo", [128, 8, D], BF16, ph)
                load_w(wsb, w_bsb, 4); load_w(wml, w_bml, 4); load_w(wo, w_out, 8)
                L1 = ln_params(1, ph, "p4")
                st = P.sb("p4_st", [128, 12], F32, ph); mv = P.sb("p4_mv", [128, 2], F32, ph)
                sd = P.sb("p4_sd", [128, 1], F32, ph); rstd = P.sb("p4_rstd", [128, 1], F32, ph)
                xn = P.sb("p4_xn", [128, D], F32, ph)
                hn = [P.sb(f"p4_hn{i}", [128, D], F32, ph) for i in range(2)]
                hnb = P.sb("p4_hnb", [128, D], BF16, ph)
                hnT = P.sb("p4_hnT", [128, 8, 128], BF16, ph)
                gT = P.sb("p4_gT", [128, 16, 128], BF16, ph)
                osbT = [P.sb(f"p4_osbT{i}", [128, 4, 128], BF16, ph) for i in range(2)]
                omlb = [P.sb(f"p4_oml{i}", [128, 512], BF16, ph) for i in range(2)]
                omlT = P.sb("p4_omlT", [128, 4, 128], BF16, ph)
                t1 = P.sb("p4_t1", [128, D], F32, ph); t2 = P.sb("p4_t2", [128, D], F32, ph)
                yT = P.sb("p4_yT", [128, 8, 128], BF16, ph)
                r1 = P.sb("p4_r1", [128, D], F32, ph)
                h1o = [P.sb(f"p4_h1{i}", [128, D], F32, ph) for i in range(2)]
                for s in range(NBLK):
                    rows = slice(s * 128, (s + 1) * 128)
                    hb = hn[s % 2]; ob_ = osbT[s % 2]; omb = omlb[s % 2]; h1b_ = h1o[s % 2]
                    P.dma('sp', lambda e, hb=hb, rows=rows: e.dma_start(out=hb[:], in_=HNS[rows, :]), [], [hb])
                    P.dma('sp', lambda e, ob_=ob_, rows=rows: e.dma_start(out=ob_[:], in_=OSBT[:, :, rows].rearrange("hp p t -> p hp t")),
                          [], [ob_])
                    P.dma('sp', lambda e, omb=omb, rows=rows: e.dma_start(out=omb[:], in_=OMLS[rows, :]), [], [omb])
                    P.op('act', lambda e, hb=hb: e.activation(out=hnb[:], in_=hb[:], func=AF.Copy), [hb], [hnb])
                    transpose_to(hnb, 8, pb[0], hnT)
                    transpose_to(omb, 4, pb[1], omlT)
                    for ch in range(16):
                        bk = pb[ch // 4]; cs = slice((ch % 4) * 128, (ch % 4 + 1) * 128)
                        for c in range(8):
                            P.op('pe', lambda e, bk=bk, cs=cs, c=c, ch=ch: e.matmul(
                                bk[:, cs], lhsT=wG[:, c, ch * 128:(ch + 1) * 128], rhs=hnT[:, c, :],
                                start=(c == 0), stop=(c == 7)), [wG, hnT], [bk])
                    for q4 in range(4):
                        P.op('act', lambda e, q4=q4: e.activation(out=gT[:, q4 * 4:(q4 + 1) * 4, :].rearrange("p c t -> p (c t)"),
                                                                  in_=pb[q4][:], func=AF.Sigmoid), [pb[q4]], [gT])
                    for dc in range(8):
                        cs = slice((dc % 4) * 128, (dc % 4 + 1) * 128)
                        for hp in range(4):
                            P.op('pe', lambda e, dc=dc, cs=cs, hp=hp, ob_=ob_: e.matmul(
                                pb[dc // 4][:, cs], lhsT=wsb[:, hp, dc * 128:(dc + 1) * 128], rhs=ob_[:, hp, :],
                                start=(hp == 0), stop=(hp == 3)), [wsb, ob_], [pb[dc // 4]])
                        for fc in range(4):
                            P.op('pe', lambda e, dc=dc, cs=cs, fc=fc: e.matmul(
                                pb[2 + dc // 4][:, cs], lhsT=wml[:, fc, dc * 128:(dc + 1) * 128], rhs=omlT[:, fc, :],
                                start=(fc == 0), stop=(fc == 3)), [wml, omlT], [pb[2 + dc // 4]])
                    for hf in range(2):
                        cs = slice(hf * 512, (hf + 1) * 512)
                        gsb_ = gT[:, hf * 4:(hf + 1) * 4, :].rearrange("p c t -> p (c t)")
                        gml_ = gT[:, 8 + hf * 4:8 + (hf + 1) * 4, :].rearrange("p c t -> p (c t)")
                        P.op('dve', lambda e, cs=cs, hf=hf, gsb_=gsb_: e.tensor_tensor(out=t1[:, cs], in0=gsb_, in1=pb[hf][:], op=ALU.mult),
                             [gT, pb[hf]], [t1])
                        P.op('dve', lambda e, cs=cs, hf=hf, gml_=gml_: e.tensor_tensor(out=t2[:, cs], in0=gml_, in1=pb[2 + hf][:], op=ALU.mult),
                             [gT, pb[2 + hf]], [t2])
                    P.op('dve', lambda e: e.tensor_tensor(out=yT[:].rearrange("p c t -> p (c t)"), in0=t1[:], in1=t2[:], op=ALU.add),
                         [t1, t2], [yT])
                    for hf in range(2):
                        cs = slice(hf * 512, (hf + 1) * 512)
                        for dc in range(8):
                            P.op('pe', lambda e, hf=hf, cs=cs, dc=dc: e.matmul(pb[hf][:], lhsT=yT[:, dc, :], rhs=wo[:, dc, cs],
                                                                             start=(dc == 0), stop=(dc == 7)), [yT, wo], [pb[hf]])
                        P.op('dve', lambda e, hf=hf, cs=cs, hb=hb: e.scalar_tensor_tensor(out=r1[:, cs], in0=hb[:, cs], scalar=ALPHA,
                                                                                        in1=pb[hf][:], op0=ALU.mult, op1=ALU.add),
                             [hb, pb[hf]], [r1])
                    layer_norm(r1, h1b_, L1, st, mv, sd, rstd, xn)
                    P.dma('sp', lambda e, h1b_=h1b_, rows=rows: e.dma_start(out=H1S[rows, :], in_=h1b_[:]), [h1b_], [])
                P.barrier()
            with ExitStack() as ph:
                _phase2(ph)
            if stop_after == 'p4a':
                P.emit()
                nc._plog = P.log
                return nc

        if not with_mix:
            def _phase3(ph):
                xt = [P.sb(f"a_xt{i}", [128, D], F32, ph) for i in range(2)]
                st = P.sb("a_st", [128, 12], F32, ph); mv = P.sb("a_mv", [128, 2], F32, ph)
                sd = P.sb("a_sd", [128, 1], F32, ph); rstd = P.sb("a_rstd", [128, 1], F32, ph)
                xn = P.sb("a_xn", [128, D], F32, ph); hn = P.sb("a_hn", [128, D], F32, ph)
                r1 = P.sb("a_r1", [128, D], F32, ph)
                h1o = [P.sb(f"a_h1{i}", [128, D], F32, ph) for i in range(2)]
                L0 = ln_params(0, ph, "a"); L1 = ln_params(1, ph, "a")
                for s in range(NBLK):
                    rows = slice(s * 128, (s + 1) * 128)
                    xb = xt[s % 2]; hb = h1o[s % 2]
                    P.dma('sp', lambda e, xb=xb, rows=rows: e.dma_start(out=xb[:], in_=x_all[rows, :]), [], [xb])
                    layer_norm(xb, hn, L0, st, mv, sd, rstd, xn)
                    P.op('act', lambda e: e.activation(out=r1[:], in_=hn[:], func=AF.Copy, scale=ALPHA), [hn], [r1])
                    layer_norm(r1, hb, L1, st, mv, sd, rstd, xn)
                    P.dma('sp', lambda e, hb=hb, rows=rows: e.dma_start(out=H1S[rows, :], in_=hb[:]), [hb], [])
                P.barrier()

            with ExitStack() as ph:
                _phase3(ph)
        P.serialize = SER_P4B
        def _phase4(ph):
            wpg = P.sb("b_wpg", [128, 8, D], BF16, ph)
            wple = P.sb("b_wple", [128, 2, D], BF16, ph)
            wq = P.sb("b_wq", [128, 8, 2048], BF16, ph)
            wql = P.sb("b_wql", [128, 8, 2048], BF16, ph)
            L2 = ln_params(2, ph, "b")
            kT = [P.sb(f"b_kT{i}", [128, 128], F32, ph) for i in range(2)]
            ktmp = P.sb("b_ktmp", [128, 128], F32, ph)
            kTh = [P.sb(f"b_kTh{i}", [128, 128], BF16, ph) for i in range(2)]
            kTl = [P.sb(f"b_kTl{i}", [128, 128], BF16, ph) for i in range(2)]
            iota16 = P.sb("b_iota16", [128, 16], F32, ph)
            load_w(wpg, w_pg, 8)
            load_w(wple, w_ple, 2)
            load_w(wq, peer_wq, 8)
            P.op('pool', lambda e: e.iota(iota16[:], pattern=[[1, 16]], base=0, channel_multiplier=0,
                                          allow_small_or_imprecise_dtypes=True), [], [iota16])
            for i, kk in enumerate((peer_k1, peer_k2)):
                P.dma('sp', lambda e, kk=kk: e.dma_start(out=ktmp[:], in_=kk), [], [ktmp])
                P.op('pe', lambda e: e.transpose(out=pb[0][:, 0:128], in_=ktmp[:], identity=identf[:]),
                     [ktmp, identf], [pb[0]])
                P.op('dve', lambda e, i=i: e.tensor_copy(out=kT[i][:], in_=pb[0][:, 0:128]), [pb[0]], [kT[i]])
                P.op('dve', lambda e, i=i: e.tensor_copy(out=kTh[i][:], in_=kT[i][:]), [kT[i]], [kTh[i]])
                P.op('dve', lambda e, i=i: e.tensor_tensor(out=kTl[i][:], in0=kT[i][:], in1=kTh[i][:], op=ALU.subtract),
                     [kT[i], kTh[i]], [kTl[i]])

            st = P.sb("b_st", [128, 12], F32, ph); mv = P.sb("b_mv", [128, 2], F32, ph)
            sd = P.sb("b_sd", [128, 1], F32, ph); rstd = P.sb("b_rstd", [128, 1], F32, ph)
            xn = P.sb("b_xn", [128, D], F32, ph)
            h1t = [P.sb(f"b_h1{i}", [128, D], F32, ph) for i in range(2)]
            ptl = [P.sb(f"b_pt{i}", [128, 256], F32, ph) for i in range(2)]
            ptb = P.sb("b_ptb", [128, 256], BF16, ph)
            h1b = P.sb("b_h1b", [128, D], BF16, ph)
            h1T = P.sb("b_h1T", [128, 8, 128], BF16, ph)
            h1l = P.sb("b_h1l", [128, D], BF16, ph)
            h1Tl = P.sb("b_h1Tl", [128, 8, 128], BF16, ph)
            qhi = P.sb("b_qhi", [128, 2048], BF16, ph)
            qlo = P.sb("b_qlo", [128, 2048], BF16, ph)
            pT = P.sb("b_pT", [128, 2, 128], BF16, ph)
            ple = P.sb("b_ple", [128, D], F32, ph)
            r2 = P.sb("b_r2", [128, D], F32, ph)
            ot = [P.sb(f"b_ot{i}", [128, D], F32, ph) for i in range(2)]
            bigA = P.sb("b_bigA", [128, 2048], F32, ph)
            bigB = P.sb("b_bigB", [128, 2048], F32, ph)
            bigC = P.sb("b_bigC", [128, 2048], F32, ph)
            bigD = P.sb("b_bigD", [128, 2048], F32, ph)
            V16 = P.sb("b_V16", [128, 16, 16], F32, ph)
            I16 = P.sb("b_I16", [128, 16, 16], U32, ph)
            I16f = P.sb("b_I16f", [128, 16, 16], F32, ph)
            i1s = P.sb("b_i1s", [128, 8, 16], F32, ph)
            tops = P.sb("b_tops", [128, 8, 16], F32, ph)
            posu = P.sb("b_posu", [128, 8, 16], U32, ph)
            pau = P.sb("b_pau", [128, 8, 16], U32, ph)
            pbu = P.sb("b_pbu", [128, 8, 16], U32, ph)
            paf = P.sb("b_paf", [128, 8, 16], F32, ph)
            pbf = P.sb("b_pbf", [128, 8, 16], F32, ph)
            e1 = P.sb("b_e1", [128, 8, 16], F32, ph)
            e2 = P.sb("b_e2", [128, 8, 16], F32, ph)
            idxf = P.sb("b_idxf", [128, 128], F32, ph)
            idxi = [P.sb(f"b_idxi{i}", [128, 128], I32, ph) for i in range(2)]
            gex = P.sb("b_gex", [128, 8, 16], F32, ph)
            gz = P.sb("b_gz", [128, 8], F32, ph)
            gates = P.sb("b_gates", [128, 8, 16], F32, ph)
            apre = P.sb("b_apre", [128, 128], F32, ph)
            wgt = P.sb("b_wgt", [128, 128], F32, ph)
            junk = P.sb("b_junk", [128, D], BF16, ph)
            NG = 4
            gb = [P.sb(f"b_gb{i}", [128, D], F32, ph) for i in range(NG)]
            gcount = [0]
            acc = P.ps("b_acc", [128, D], F32, ph)
            h1ps = P.ps("b_h1ps", [128, D], F32, ph)

            for c in range(8):
                P.dma('sp', lambda e, c=c: e.dma_start(out=bigA[:], in_=peer_wq[c * 128:(c + 1) * 128, :]), [], [bigA])
                P.op('dve', lambda e, c=c: e.tensor_tensor(out=wql[:, c, :], in0=bigA[:], in1=wq[:, c, :], op=ALU.subtract),
                     [bigA, wq], [wql])
            for s in range(NBLK):
                rows = slice(s * 128, (s + 1) * 128)
                h1 = h1t[s % 2]; pl = ptl[s % 2]; ob = ot[s % 2]; ixi = idxi[s % 2]
                P.dma('sp', lambda e, h1=h1, rows=rows: e.dma_start(out=h1[:], in_=H1S[rows, :]), [], [h1])
                P.dma('sp', lambda e, pl=pl, rows=rows: e.dma_start(out=pl[:], in_=p_all[rows, :]), [], [pl])
                P.op('act', lambda e, h1=h1: e.activation(out=h1b[:], in_=h1[:], func=AF.Copy), [h1], [h1b])
                transpose_to(h1b, 8, pb[0], h1T)
                if with_peer:
                    P.op('dve', lambda e, h1=h1: e.tensor_tensor(out=h1l[:], in0=h1[:], in1=h1b[:], op=ALU.subtract),
                         [h1, h1b], [h1l])
                    transpose_to(h1l, 8, pb[1], h1Tl)
                P.op('dve', lambda e, pl=pl: e.tensor_copy(out=ptb[:], in_=pl[:]), [pl], [ptb])
                transpose_to(ptb, 2, pb[1], pT)
                if with_peer:
                    P.op('act', lambda e, h1=h1: e.activation(out=h1ps[:], in_=h1[:], func=AF.Copy), [h1], [h1ps])
                    for ch in range(16):
                        bk = pb[ch // 4]; cs = slice((ch % 4) * 128, (ch % 4 + 1) * 128)
                        for pi, (wt, ht) in enumerate(((wq, h1T), (wql, h1T), (wq, h1Tl))):
                            for c in range(8):
                                P.op('pe', lambda e, bk=bk, cs=cs, c=c, ch=ch, wt=wt, ht=ht, pi=pi: e.matmul(
                                    bk[:, cs], lhsT=wt[:, c, ch * 128:(ch + 1) * 128], rhs=ht[:, c, :],
                                    start=(c == 0 and pi == 0), stop=(c == 7 and pi == 2)), [wt, ht], [bk])
                    for q4 in range(4):
                        P.op('act', lambda e, q4=q4: e.activation(out=qhi[:, q4 * 512:(q4 + 1) * 512], in_=pb[q4][:],
                                                                  func=AF.Copy), [pb[q4]], [qhi])
                        P.op('dve', lambda e, q4=q4: e.tensor_tensor(out=qlo[:, q4 * 512:(q4 + 1) * 512], in0=pb[q4][:],
                                                                     in1=qhi[:, q4 * 512:(q4 + 1) * 512], op=ALU.subtract),
                             [pb[q4], qhi], [qlo])
                    for ch in range(16):
                        bk = pb[ch // 4]; cs = slice((ch % 4) * 128, (ch % 4 + 1) * 128)
                        for pi, (qt, kt) in enumerate(((qhi, kTh), (qlo, kTh), (qhi, kTl))):
                            P.op('pe', lambda e, bk=bk, cs=cs, ch=ch, qt=qt, kt=kt, pi=pi: e.matmul(
                                bk[:, cs], lhsT=qt[:, ch * 128:(ch + 1) * 128], rhs=kt[ch % 2][:],
                                start=(pi == 0), stop=(pi == 2)), [qt, kt[ch % 2]], [bk])
                    for q4 in range(4):
                        P.op('act', lambda e, q4=q4: e.activation(out=bigB[:, q4 * 512:(q4 + 1) * 512], in_=pb[q4][:],
                                                                  func=AF.Copy), [pb[q4]], [bigB])
                    for ch in range(16):
                        seg = slice(ch * 128, (ch + 1) * 128)
                        P.op('dve', lambda e, ch=ch, seg=seg: e.max(out=V16[:, ch, 0:8], in_=bigB[:, seg]), [bigB], [V16])
                        P.op('dve', lambda e, ch=ch, seg=seg: e.match_replace(out=bigC[:, seg], in_to_replace=V16[:, ch, 0:8],
                                                                              in_values=bigB[:, seg], imm_value=-1e30),
                             [bigB, V16], [bigC])
                        P.op('dve', lambda e, ch=ch, seg=seg: e.max(out=V16[:, ch, 8:16], in_=bigC[:, seg]), [bigC], [V16])
                        P.op('dve', lambda e, ch=ch, seg=seg: e.max_index(out=I16[:, ch, 0:8], in_max=V16[:, ch, 0:8],
                                                                          in_values=bigB[:, seg]), [bigB, V16], [I16])
                        P.op('dve', lambda e, ch=ch, seg=seg: e.max_index(out=I16[:, ch, 8:16], in_max=V16[:, ch, 8:16],
                                                                          in_values=bigB[:, seg]), [bigB, V16], [I16])
                    P.op('dve', lambda e: e.tensor_copy(out=I16f[:], in_=I16[:]), [I16], [I16f])
                    Vv = V16[:].rearrange("p (h two) k -> p h two k", two=2)
                    Iv = I16f[:].rearrange("p (h two) k -> p h two k", two=2)
                    P.op('dve', lambda e: e.tensor_scalar(out=i1s[:], in0=Iv[:, :, 0, :], scalar1=128.0, scalar2=None,
                                                          op0=ALU.mult), [I16f], [i1s])
                    c4 = lambda t: t[:].rearrange("p (h a b) -> p h a b", h=8, a=16)
                    bc_a = lambda ap: ap.unsqueeze(3).to_broadcast([128, 8, 16, 16])
                    bc_b = lambda ap: ap.unsqueeze(2).to_broadcast([128, 8, 16, 16])
                    P.op('dve', lambda e: e.tensor_tensor(out=c4(bigA), in0=bc_a(Vv[:, :, 0, :]), in1=bc_b(Vv[:, :, 1, :]),
                                                          op=ALU.add), [V16], [bigA])
                    P.op('dve', lambda e: e.tensor_tensor(out=c4(bigD), in0=bc_a(i1s[:]), in1=bc_b(Iv[:, :, 1, :]),
                                                          op=ALU.add), [i1s, I16f], [bigD])
                    for h in range(8):
                        seg = slice(h * 256, (h + 1) * 256)
                        P.op('dve', lambda e, h=h, seg=seg: e.max(out=tops[:, h, 0:8], in_=bigA[:, seg]), [bigA], [tops])
                        P.op('dve', lambda e, h=h, seg=seg: e.match_replace(out=bigC[:, seg], in_to_replace=tops[:, h, 0:8],
                                                                            in_values=bigA[:, seg], imm_value=-1e30),
                             [bigA, tops], [bigC])
                        P.op('dve', lambda e, h=h, seg=seg: e.max(out=tops[:, h, 8:16], in_=bigC[:, seg]), [bigC], [tops])
                        P.op('dve', lambda e, h=h, seg=seg: e.max_index(out=posu[:, h, 0:8], in_max=tops[:, h, 0:8],
                                                                        in_values=bigA[:, seg]), [bigA, tops], [posu])
                        P.op('dve', lambda e, h=h, seg=seg: e.max_index(out=posu[:, h, 8:16], in_max=tops[:, h, 8:16],
                                                                        in_values=bigA[:, seg]), [bigA, tops], [posu])
                    P.op('dve', lambda e: e.tensor_single_scalar(out=pau[:], in_=posu[:], scalar=4,
                                                                 op=ALU.logical_shift_right), [posu], [pau])
                    P.op('dve', lambda e: e.tensor_single_scalar(out=pbu[:], in_=posu[:], scalar=15,
                                                                 op=ALU.bitwise_and), [posu], [pbu])
                    P.op('dve', lambda e: e.tensor_copy(out=paf[:], in_=pau[:]), [pau], [paf])
                    P.op('dve', lambda e: e.tensor_copy(out=pbf[:], in_=pbu[:]), [pbu], [pbf])
                    io4 = iota16[:].unsqueeze(1).unsqueeze(1).to_broadcast([128, 8, 16, 16])
                    for (pf, src_ap, res_t, rd) in ((paf, i1s[:], e1, [i1s]), (pbf, Iv[:, :, 1, :], e2, [I16f])):
                        P.op('dve', lambda e, pf=pf: e.tensor_tensor(out=c4(bigB), in0=bc_a(pf[:]), in1=io4, op=ALU.is_equal),
                             [pf, iota16], [bigB])
                        P.op('dve', lambda e, src_ap=src_ap: e.tensor_tensor(out=c4(bigC), in0=c4(bigB), in1=bc_b(src_ap),
                                                                             op=ALU.mult), [bigB] + rd, [bigC])
                        P.op('dve', lambda e, res_t=res_t: e.tensor_reduce(out=res_t[:], in_=c4(bigC), axis=AX.X, op=ALU.add),
                             [bigC], [res_t])
                    P.op('dve', lambda e: e.tensor_tensor(out=idxf[:].rearrange("p (h k) -> p h k", h=8), in0=e1[:], in1=e2[:],
                                                          op=ALU.add), [e1, e2], [idxf])
                    P.op('dve', lambda e, ixi=ixi: e.tensor_copy(out=ixi[:], in_=idxf[:]), [idxf], [ixi])
                    P.op('dve', lambda e: e.tensor_tensor(out=gex[:], in0=tops[:],
                                                          in1=tops[:, :, 0:1].to_broadcast([128, 8, 16]), op=ALU.subtract),
                         [tops], [gex])
                    P.op('act', lambda e: e.activation(out=gex[:], in_=gex[:], func=AF.Exp), [gex], [gex])
                    P.op('dve', lambda e: e.tensor_reduce(out=gz[:], in_=gex[:], axis=AX.X, op=ALU.add), [gex], [gz])
                    P.op('dve', lambda e: e.reciprocal(out=gz[:], in_=gz[:]), [gz], [gz])
                    P.op('dve', lambda e: e.tensor_tensor(out=gates[:], in0=gex[:],
                                                          in1=gz[:].unsqueeze(2).to_broadcast([128, 8, 16]), op=ALU.mult),
                         [gex, gz], [gates])
                    for hk in range(128):
                        g = gb[gcount[0] % NG]; gcount[0] += 1
                        P.dma('pool', lambda e, g=g, hk=hk, ixi=ixi: e.indirect_dma_start(
                            out=g[:], out_offset=None, in_=peer_u,
                            in_offset=bass.IndirectOffsetOnAxis(ap=ixi[:, hk:hk + 1], axis=0)), [ixi], [g])
                        P.op('dve', lambda e, g=g, hk=hk: e.scalar_tensor_tensor(
                            out=junk[:], in0=g[:], scalar=1.0, in1=h1ps[:], op0=ALU.mult, op1=ALU.mult,
                            accum_out=apre[:, hk:hk + 1]), [g, h1ps], [junk, apre])
                    if debug:
                        P.dma('sp', lambda e, rows=rows: e.dma_start(out=DBG[rows, 0, :], in_=idxf[:]), [idxf], [])
                        P.dma('sp', lambda e, rows=rows: e.dma_start(out=DBG[rows, 1, :], in_=apre[:]), [apre], [])
                        P.dma('sp', lambda e, rows=rows: e.dma_start(out=DBG[rows, 2, :], in_=gates[:].rearrange("p h k -> p (h k)")), [gates], [])
                    P.op('act', lambda e: e.activation(out=apre[:], in_=apre[:], func=AF.Gelu), [apre], [apre])
                    if debug:
                        P.dma('sp', lambda e, rows=rows: e.dma_start(out=DBG[rows, 3, :], in_=apre[:]), [apre], [])
                    P.op('dve', lambda e: e.tensor_tensor(out=wgt[:], in0=apre[:],
                                                          in1=gates[:].rearrange("p h k -> p (h k)"), op=ALU.mult),
                         [apre, gates], [wgt])
                    for hk in range(128):
                        g = gb[gcount[0] % NG]; gcount[0] += 1
                        P.dma('pool', lambda e, g=g, hk=hk, ixi=ixi: e.indirect_dma_start(
                            out=g[:], out_offset=None, in_=peer_v,
                            in_offset=bass.IndirectOffsetOnAxis(ap=ixi[:, hk:hk + 1], axis=0)), [ixi], [g])
                        if hk == 0:
                            P.op('dve', lambda e, g=g: e.tensor_scalar(
                                out=acc[:], in0=g[:], scalar1=wgt[:, 0:1], scalar2=None, op0=ALU.mult),
                                [g, wgt], [acc])
                        else:
                            P.op('dve', lambda e, g=g, hk=hk: e.scalar_tensor_tensor(
                                out=acc[:], in0=g[:], scalar=wgt[:, hk:hk + 1], in1=acc[:],
                                op0=ALU.mult, op1=ALU.add), [g, wgt, acc], [acc])
                for hf in range(2):
                    cs = slice(hf * 512, (hf + 1) * 512)
                    for c in range(8):
                        P.op('pe', lambda e, c=c, cs=cs, hf=hf: e.matmul(pb[hf][:], lhsT=h1T[:, c, :], rhs=wpg[:, c, cs],
                                                                         start=(c == 0), stop=(c == 7)),
                             [h1T, wpg], [pb[hf]])
                    for c in range(2):
                        P.op('pe', lambda e, c=c, cs=cs, hf=hf: e.matmul(pb[2 + hf][:], lhsT=pT[:, c, :], rhs=wple[:, c, cs],
                                                                         start=(c == 0), stop=(c == 1)),
                             [pT, wple], [pb[2 + hf]])
                    P.op('act', lambda e, cs=cs, hf=hf: e.activation(out=xn[:, cs], in_=pb[hf][:], func=AF.Sigmoid),
                         [pb[hf]], [xn])
                    P.op('dve', lambda e, cs=cs, hf=hf: e.tensor_tensor(out=ple[:, cs], in0=xn[:, cs], in1=pb[2 + hf][:],
                                                                        op=ALU.mult), [xn, pb[2 + hf]], [ple])
                P.op('dve', lambda e, h1=h1: e.scalar_tensor_tensor(out=r2[:], in0=h1[:], scalar=ALPHA, in1=ple[:],
                                                                    op0=ALU.mult, op1=ALU.add), [h1, ple], [r2])
                if with_peer:
                    P.op('dve', lambda e: e.tensor_tensor(out=r2[:], in0=r2[:], in1=acc[:], op=ALU.add), [r2, acc], [r2])
                layer_norm(r2, ob, L2, st, mv, sd, rstd, xn)
                P.dma('sp', lambda e, ob=ob, rows=rows: e.dma_start(out=out[rows, :], in_=ob[:]), [ob], [])
        with ExitStack() as ph:
            _phase4(ph)
        P.emit()
    return nc


_NC = {}
W_NAMES = ['w_ple_gate', 'w_ple', 'peer_wq', 'peer_k1', 'peer_k2', 'peer_u', 'peer_v',
           'w_branch_sb', 'w_branch_ml', 'w_out']


def kernel(_nblk=NB, _with_mix=True, _with_peer=True, _debug=False, _stop=None, **inp):
    key = (_nblk, _with_mix, _with_peer, _debug, _stop)
    if key not in _NC:
        _NC[key] = build_nc(_nblk, _with_mix, _with_peer, _debug, _stop)
    nc = _NC[key]
    T = _nblk * 128
    x = np.asarray(inp['x'], dtype=np.float32)
    p = np.asarray(inp['p'], dtype=np.float32)[0]
    shared = {n: np.ascontiguousarray(np.asarray(inp[n], dtype=np.float32)[0]) for n in W_NAMES}
    w_in_h = np.asarray(inp['w_in'], dtype=np.float32)[0]
    for i, (c0, c1) in enumerate(((0, 1024), (1024, 2048), (2048, 3072), (3072, 3592), (3592, 4616), (4616, 5640))):
        shared[f'w_in_p{i}'] = np.ascontiguousarray(w_in_h[:, c0:c1])
    shared['conv_wb'] = np.ascontiguousarray(np.concatenate([np.asarray(inp['conv_w'], dtype=np.float32)[0],
                                                             np.asarray(inp['conv_b'], dtype=np.float32)], axis=0))
    shared['gate_b'] = np.ascontiguousarray(np.concatenate([np.asarray(inp['b_igate'], dtype=np.float32)[0],
                                                            np.asarray(inp['b_fgate'], dtype=np.float32)[0]])[None, :])
    shared['ln0_g'] = np.ascontiguousarray(inp['ln0_g'], dtype=np.float32)
    shared['ln0_b'] = np.ascontiguousarray(inp['ln0_b'], dtype=np.float32)
    for i in (1, 2):
        shared[f'ln{i}_g'] = np.ascontiguousarray(inp[f'ln{i}_g'][0], dtype=np.float32)
        shared[f'ln{i}_b'] = np.ascontiguousarray(inp[f'ln{i}_b'][0], dtype=np.float32)
    in_maps = []
    for core in range(8):
        b = core // 2
        m = dict(shared)
        m["x_all"] = np.ascontiguousarray(x[b, :T])
        m["p_all"] = np.ascontiguousarray(p[b, :T])
        in_maps.append(m)
    res = run_bass_kernel_spmd(nc, in_maps, core_ids=list(range(8)))
    outp = np.empty((4, T, D), dtype=np.float32)
    for b in range(4):
        outp[b] = res.results[2 * b]["out"]
    if _debug:
        return outp, [res.results[2 * b] for b in range(4)]
    return outp
```

```python
import numpy as np
from contextlib import ExitStack
import concourse.bass as bass
import concourse.mybir as mybir
from concourse.bass_utils import run_bass_kernel_spmd

F32 = mybir.dt.float32
BF16 = mybir.dt.bfloat16
I32 = mybir.dt.int32
U32 = mybir.dt.uint32
AF = mybir.ActivationFunctionType
ALU = mybir.AluOpType
AX = mybir.AxisListType

D = 1024
SEQ = 8192
NB = 64
NOWN = 32
ALPHA = 2.0 ** 0.25
EPS = 1e-5
ENGS = ['pe', 'act', 'dve', 'pool', 'sp']
import os
SERIALIZE = False
SER_P1, SER_P3, SER_P4A, SER_P4B = False, False, False, False


class Res:
    __slots__ = ('w', 'r')

    def __init__(self):
        self.w = None
        self.r = {}


class Tile:
    def __init__(self, t):
        self.t = t
        self.res = Res()
        self._sub = {}
        self.dkey = None
        self.dcount = 0

    def __getitem__(self, k):
        return self.t[k]

    def sub(self, k):
        if k not in self._sub:
            self._sub[k] = Res()
        return self._sub[k]


class Prog:
    def __init__(self, nc, es):
        self.nc = nc
        self.es = es
        self.ops = {e: [] for e in ENGS}
        self.cnt = {e: 0 for e in ENGS}
        self.dcnt = {e: 0 for e in ENGS}
        self.seen = {e: {} for e in ENGS}
        self.sem = {}
        for e in ENGS:
            self.sem[('c', e)] = es.enter_context(nc.semaphore('c_' + e))
        self.dtiles = []
        self.log = []
        self.serialize = SERIALIZE

    def sb(self, name, shape, dt, es=None):
        return Tile((es or self.es).enter_context(self.nc.sbuf_tensor(name, shape, dt)))

    def ps(self, name, shape, dt, es=None):
        return Tile((es or self.es).enter_context(self.nc.psum_tensor(name, shape, dt)))

    def barrier(self):
        cur = []
        for e in ENGS:
            if self.cnt[e]:
                cur.append((('c', e), self.cnt[e]))
        for t in self.dtiles:
            cur.append((t.dkey, 16 * t.dcount))
        for e in ENGS:
            waits = [(k, v) for k, v in cur if self.seen[e].get(k, 0) < v and not (e == 'pe' and k == ('c', 'pe'))]
            for k, v in waits:
                self.seen[e][k] = v
            self.ops[e].append((waits, None, None, 0))
            self.log.append((e, 'barrier', waits, None))

    def _deps(self, eng, reads, writes):
        waits = {}
        seen = self.seen[eng]

        def add(k, v):
            if seen.get(k, 0) >= v:
                return
            if waits.get(k, 0) < v:
                waits[k] = v

        for r in reads:
            if r.w is not None:
                add(*r.w)
        for w in writes:
            if w.w is not None:
                add(*w.w)
            for k, v in w.r.items():
                add(k, v)
        for k, v in waits.items():
            seen[k] = v
        return list(waits.items())

    def _record(self, ev, reads, writes):
        k, v = ev
        for r in reads:
            if r.r.get(k, 0) < v:
                r.r[k] = v
        for w in writes:
            w.w = ev
            w.r = {}

    def op(self, eng, fn, reads=(), writes=()):
        reads = [x.res if isinstance(x, Tile) else x for x in reads]
        writes = [x.res if isinstance(x, Tile) else x for x in writes]
        waits = self._deps(eng, reads, writes)
        self.cnt[eng] += 1
        ev = (('c', eng), self.cnt[eng])
        if eng == 'pe':
            self.seen[eng][ev[0]] = ev[1]
        self._record(ev, reads, writes)
        self.ops[eng].append((waits, fn, ev[0], 1))
        self.log.append((eng, 'op', waits, ev))
        if self.serialize:
            self.barrier()

    def dma(self, eng, fn, reads=(), writes=()):
        tiles = [x for x in list(writes) + list(reads) if isinstance(x, Tile)]
        owner = tiles[0]
        if owner.dkey is None:
            owner.dkey = ('t', len(self.dtiles))
            self.sem[owner.dkey] = self.es.enter_context(self.nc.semaphore('t%d' % len(self.dtiles)))
            self.dtiles.append(owner)
        reads = [x.res if isinstance(x, Tile) else x for x in reads]
        writes = [x.res if isinstance(x, Tile) else x for x in writes]
        waits = self._deps(eng, reads, writes)
        owner.dcount += 1
        ev = (owner.dkey, 16 * owner.dcount)
        self._record(ev, reads, writes)
        self.ops[eng].append((waits, fn, ev[0], 16))
        self.log.append((eng, 'dma', waits, ev))
        if self.serialize:
            self.barrier()

    def emit(self):
        nc = self.nc
        handles = {'pe': 'tensor', 'act': 'scalar', 'dve': 'vector', 'pool': 'gpsimd', 'sp': 'sync'}
        with nc.Block() as blk:
            for e in ENGS:
                ops = self.ops[e]
                final = None
                if e == 'sp':
                    final = [(t.dkey, 16 * t.dcount) for t in self.dtiles]

                def body(eng, ops=ops, final=final):
                    for waits, fn, key, inc in ops:
                        for k, v in waits:
                            eng.wait_ge(self.sem[k], v)
                        if fn is not None:
                            fn(eng).then_inc(self.sem[key], inc)
                    if final:
                        for k, v in final:
                            eng.wait_ge(self.sem[k], v)

                getattr(blk, handles[e])(body)


def _r(x):
    return [x] if not isinstance(x, (list, tuple)) else list(x)


def build_nc(NBLK=NB, with_mix=True, with_peer=True, debug=False, stop_after=None):
    T = NBLK * 128
    nc = bass.Bass("TRN2", target_bir_lowering=False)
    dr = lambda name, shape, dt=F32, kind="ExternalInput": nc.dram_tensor(name, shape, dt, kind=kind).ap()
    SCR = "ExternalOutput" if debug else "Internal"
    NO = NBLK // 2
    TO = NO * 128
    x_all = dr("x_all", [T, D])
    x_own = dr("x_own", [TO, D])
    p_own = dr("p_own", [TO, 256])
    sel_in = dr("sel", [128, 2])
    ln_g = [dr(f"ln{i}_g", [D]) for i in range(3)]
    ln_b = [dr(f"ln{i}_b", [D]) for i in range(3)]
    WPIECES = ((0, 1024), (1024, 2048), (2048, 3072), (3072, 3592), (3592, 4616), (4616, 5640))
    w_in_p = [dr(f"w_in_p{i}", [D, c1 - c0]) for i, (c0, c1) in enumerate(WPIECES)]
    conv_wb = dr("conv_wb", [5, D])
    gate_b = dr("gate_b", [1, 8])
    w_bsb = dr("w_branch_sb", [512, D]); w_bml = dr("w_branch_ml", [512, D]); w_out = dr("w_out", [D, D])
    QTS = dr("qts", [8, 64, T], BF16, SCR)
    KTS = dr("kts", [8, 64, T], BF16, SCR)
    VS = dr("vs", [T, 512], BF16, SCR)
    OMLS = dr("omls", [T, 512], BF16, SCR)
    OSBT = dr("osbt", [4, 128, T], BF16, SCR)
    w_pg = dr("w_ple_gate", [D, D])
    w_ple = dr("w_ple", [256, D])
    peer_wq = dr("peer_wq", [D, 2048])
    peer_k1 = dr("peer_k1", [128, 128])
    peer_k2 = dr("peer_k2", [128, 128])
    peer_u = dr("peer_u", [16384, D])
    peer_v = dr("peer_v", [16384, D])
    out = dr("out", [TO, D], F32, "ExternalOutput")
    H1S = dr("h1s", [TO, D], F32, SCR)
    DBG = dr("dbg", [TO, 4, 128], F32, "ExternalOutput") if debug else None

    with ExitStack() as es:
        P = Prog(nc, es)
        ident = P.sb("ident", [128, 128], BF16)
        identf = P.sb("identf", [128, 128], F32)
        epst = P.sb("epst", [128, 1], F32)
        for idt in (ident, identf):
            P.op('pool', lambda e, idt=idt: e.memset(idt[:], 1.0), [], [idt])
            P.op('pool', lambda e, idt=idt: e.affine_select(out=idt[:], in_=idt[:], pattern=[[-1, 128]],
                                                            compare_op=ALU.is_equal, fill=0.0, base=0,
                                                            channel_multiplier=1), [idt], [idt])
        P.op('pool', lambda e: e.memset(epst[:], EPS), [], [epst])
        onet = P.sb("onet", [128, 1], F32)
        P.op('pool', lambda e: e.memset(onet[:], 1.0), [], [onet])
        def ln_params(i, ph, tag):
            g = P.sb(f"{tag}_lng{i}", [128, D], F32, ph); b = P.sb(f"{tag}_lnb{i}", [128, D], F32, ph)
            P.dma('sp', lambda e: e.dma_start(out=g[:], in_=ln_g[i].partition_broadcast(128)), [], [g])
            P.dma('sp', lambda e: e.dma_start(out=b[:], in_=ln_b[i].partition_broadcast(128)), [], [b])
            return (g, b)
        pb = [P.ps(f"pb{i}", [128, 512], F32) for i in range(4)]

        def layer_norm(src, dst, k, st, mv, sd, rstd, xn):
            P.op('dve', lambda e: e.bn_stats(out=st[:, 0:6], in_=src[:, 0:512]), [src], [st])
            P.op('dve', lambda e: e.bn_stats(out=st[:, 6:12], in_=src[:, 512:1024]), [src, st], [st])
            P.op('dve', lambda e: e.bn_aggr(out=mv[:], in_=st[:]), [st], [mv])
            P.op('act', lambda e: e.activation(out=sd[:], in_=mv[:, 1:2], func=AF.Sqrt, bias=epst[:], scale=1.0),
                 [mv, epst], [sd])
            P.op('dve', lambda e: e.reciprocal(out=rstd[:], in_=sd[:]), [sd], [rstd])
            P.op('dve', lambda e: e.tensor_scalar(out=xn[:], in0=src[:], scalar1=mv[:, 0:1], scalar2=rstd[:],
                                                  op0=ALU.subtract, op1=ALU.mult), [src, mv, rstd], [xn])
            P.op('pool', lambda e: e.tensor_tensor(out=xn[:], in0=xn[:], in1=k[0][:], op=ALU.mult),
                 [xn, k[0]], [xn])
            P.op('pool', lambda e: e.tensor_tensor(out=dst[:], in0=xn[:], in1=k[1][:], op=ALU.add),
                 [xn, k[1]], [dst])

        def load_w(dst, src_ap, nchunk, c0=0, c1=None):
            for c in range(nchunk):
                sa = src_ap[c * 128:(c + 1) * 128, :] if c1 is None else src_ap[c * 128:(c + 1) * 128, c0:c1]
                P.dma('pool', lambda e, c=c, sa=sa: e.dma_start(out=dst[:, c, :], in_=sa, max_dma_last_dim=4096),
                      [], [dst])

        def transpose_to(src_b, nchunk, psb, dstT):
            pv = psb[:].bitcast(BF16)
            for c in range(nchunk):
                P.op('pe', lambda e, c=c: e.transpose(out=pv[:, c * 128:(c + 1) * 128],
                                                      in_=src_b[:, c * 128:(c + 1) * 128], identity=ident[:]),
                     [src_b, ident], [psb])
            P.op('dve', lambda e: e.tensor_copy(out=dstT[:].rearrange("p c t -> p (c t)"),
                                                in_=pv[:, 0:nchunk * 128]), [psb], [dstT])

        if with_mix:
            P.serialize = SER_P1
            def _phase0(ph):
                wA = P.sb("p1_wA", [128, 8, 3592], BF16, ph)
                for pi in range(4):
                    c0, c1 = WPIECES[pi]
                    for c in range(8):
                        P.dma('pool', lambda e, c=c, c0=c0, c1=c1, pi=pi: e.dma_start(
                            out=wA[:, c, c0:c1], in_=w_in_p[pi][c * 128:(c + 1) * 128, :], max_dma_last_dim=4096), [], [wA])
                L0 = ln_params(0, ph, "p1")
                cw = P.sb("p1_cw", [128, 8, 5], F32, ph)
                cw5 = P.sb("p1_cw5", [5, D], F32, ph)
                P.dma('sp', lambda e: e.dma_start(out=cw5[:], in_=conv_wb), [], [cw5])
                for c in range(8):
                    P.op('pe', lambda e, c=c: e.transpose(out=pb[0][:, c * 5:(c + 1) * 5], in_=cw5[0:5, c * 128:(c + 1) * 128],
                                                          identity=identf[0:5, 0:5]), [cw5, identf], [pb[0]])
                P.op('dve', lambda e: e.tensor_copy(out=cw[:].rearrange("p c j -> p (c j)"), in_=pb[0][:, 0:40]), [pb[0]], [cw])
                onesF = P.sb("p1_onesF", [128, 128], F32, ph)
                P.op('pool', lambda e: e.memset(onesF[:], 1.0), [], [onesF])
                gb1 = P.sb("p1_gb1", [1, 8], F32, ph)
                P.dma('sp', lambda e: e.dma_start(out=gb1[:], in_=gate_b), [], [gb1])
                gbb = P.sb("p1_gbb", [128, 8], F32, ph)
                P.op('pe', lambda e: e.matmul(pb[1][:, 0:8], lhsT=onesF[0:1, :], rhs=gb1[:], start=True, stop=True), [onesF, gb1], [pb[1]])
                P.op('dve', lambda e: e.tensor_copy(out=gbb[:], in_=pb[1][:, 0:8]), [pb[1]], [gbb])
                triLE = P.sb("p1_triLE", [128, 128], F32, ph)
                P.op('pool', lambda e: e.memset(triLE[:], 1.0), [], [triLE])
                P.op('pool', lambda e: e.affine_select(out=triLE[:], in_=triLE[:], pattern=[[1, 128]], compare_op=ALU.is_ge,
                                                       fill=0.0, base=0, channel_multiplier=-1), [triLE], [triLE])
                xt = [P.sb(f"p1_xt{i}", [128, D], F32, ph) for i in range(2)]
                st = P.sb("p1_st", [128, 12], F32, ph); mv = P.sb("p1_mv", [128, 2], F32, ph)
                sd = P.sb("p1_sd", [128, 1], F32, ph); rstd = P.sb("p1_rstd", [128, 1], F32, ph)
                xn = P.sb("p1_xn", [128, D], F32, ph)
                hn = [P.sb(f"p1_hn{i}", [128, D], F32, ph) for i in range(2)]
                hnb = P.sb("p1_hnb", [128, D], BF16, ph)
                hnT = P.sb("p1_hnT", [128, 8, 128], BF16, ph)
                qTb = [P.sb(f"p1_qTb{i}", [64, 8, 128], BF16, ph) for i in range(2)]
                kTb = [P.sb(f"p1_kTb{i}", [64, 8, 128], BF16, ph) for i in range(2)]
                xraw = P.sb("p1_xraw", [128, 8, 131], F32, ph)
                cacc = P.sb("p1_cacc", [128, 8, 128], F32, ph)
                ctmp = P.sb("p1_ctmp", [128, 8, 128], F32, ph)
                mqk = P.sb("p1_mqk", [128, 8, 128], BF16, ph)
                vb = [P.sb(f"p1_vb{i}", [128, 512], BF16, ph) for i in range(2)]
                mva = P.sb("p1_mva", [128, 4, 129], BF16, ph)
                sgo = P.sb("p1_sgo", [128, 512], F32, ph)
                ifr = P.sb("p1_ifr", [128, 8], F32, ph)
                li = P.sb("p1_li", [128, 4], F32, ph); fz = P.sb("p1_fz", [128, 4], F32, ph)
                l1 = P.sb("p1_l1", [128, 4], F32, ph); gtmp = P.sb("p1_gtmp", [128, 4], F32, ph)
                gs = P.sb("p1_gs", [128, 4], F32, ph); eq = P.sb("p1_eq", [128, 4], F32, ph); eb = P.sb("p1_eb", [128, 4], F32, ph)
                STt = P.sb("p1_ST", [128, 128], BF16, ph)
                ktil = P.sb("p1_ktil", [128, 128], BF16, ph)
                C32 = [P.sb(f"p1_C32_{h}", [128, 129], F32, ph) for h in range(4)]
                C16 = [P.sb(f"p1_C16_{h}", [128, 129], BF16, ph) for h in range(4)]
                tmpC = P.sb("p1_tmpC", [128, 129], F32, ph)
                dn = P.sb("p1_dn", [128, 1], F32, ph); scl = P.sb("p1_scl", [128, 1], F32, ph)
                oml = [P.sb(f"p1_oml{i}", [128, 512], BF16, ph) for i in range(2)]
                P.op('pool', lambda e: e.memset(xraw[:], 0.0), [], [xraw])
                P.op('pool', lambda e: e.memset(mva[:], 1.0), [], [mva])
                for h in range(4):
                    P.op('pool', lambda e, h=h: e.memset(C32[h][:], 0.0), [], [C32[h]])
                    P.op('pool', lambda e, h=h: e.memset(C16[h][:], 0.0), [], [C16[h]])
                LNSC = float(np.log(128.0 ** -0.5))
                lnsc = P.sb("p1_lnsc", [128, 1], F32, ph); onec = P.sb("p1_onec", [128, 1], F32, ph)
                P.op('pool', lambda e: e.memset(lnsc[:], LNSC), [], [lnsc])
                P.op('pool', lambda e: e.memset(onec[:], 1.0), [], [onec])

                for s in range(NBLK):
                    rows = slice(s * 128, (s + 1) * 128)
                    tcol = slice(s * 128, (s + 1) * 128)
                    xb = xt[s % 2]; hb = hn[s % 2]; qb = qTb[s % 2]; kb_ = kTb[s % 2]; vbb = vb[s % 2]; omb = oml[s % 2]
                    P.dma('sp', lambda e, xb=xb, rows=rows: e.dma_start(out=xb[:], in_=x_all[rows, :]), [], [xb])
                    layer_norm(xb, hb, L0, st, mv, sd, rstd, xn)
                    P.op('act', lambda e, hb=hb: e.activation(out=hnb[:], in_=hb[:], func=AF.Copy), [hb], [hnb])
                    transpose_to(hnb, 8, pb[0], hnT)
                    for which, col0, dstb, scale, banks in ((0, 0, qb, 1.0, (0, 1)), (1, 512, kb_, 0.125, (2, 3))):
                        for h in range(8):
                            bk = pb[banks[h // 4]]; cs = slice((h % 4) * 128, (h % 4 + 1) * 128)
                            for c in range(8):
                                P.op('pe', lambda e, bk=bk, cs=cs, c=c, h=h, col0=col0: e.matmul(
                                    bk[0:64, cs], lhsT=wA[:, c, col0 + h * 64:col0 + (h + 1) * 64], rhs=hnT[:, c, :],
                                    start=(c == 0), stop=(c == 7)), [wA, hnT], [bk])
                        for hh in range(2):
                            P.op('act', lambda e, hh=hh, dstb=dstb, scale=scale, banks=banks: e.activation(
                                out=dstb[:, hh * 4:(hh + 1) * 4, :].rearrange("p h t -> p (h t)"), in_=pb[banks[hh]][0:64, :],
                                func=AF.Copy, scale=scale), [pb[banks[hh]]], [dstb])
                    P.dma('sp', lambda e, qb=qb, tcol=tcol: e.dma_start(out=QTS[:, :, tcol].rearrange("h d t -> d h t"), in_=qb[:]), [qb], [])
                    P.dma('sp', lambda e, kb_=kb_, tcol=tcol: e.dma_start(out=KTS[:, :, tcol].rearrange("h d t -> d h t"), in_=kb_[:]), [kb_], [])
                    for cc in range(8):
                        bk = pb[cc // 4]; cs = slice((cc % 4) * 128, (cc % 4 + 1) * 128)
                        for c in range(8):
                            P.op('pe', lambda e, bk=bk, cs=cs, c=c, cc=cc: e.matmul(
                                bk[:, cs], lhsT=wA[:, c, 1536 + cc * 128:1536 + (cc + 1) * 128], rhs=hnT[:, c, :],
                                start=(c == 0), stop=(c == 7)), [wA, hnT], [bk])
                    for hh in range(2):
                        P.op('act', lambda e, hh=hh: e.activation(out=xraw[:, hh * 4:(hh + 1) * 4, 3:131],
                                                                  in_=pb[hh][:].rearrange("p (c t) -> p c t", c=4),
                                                                  func=AF.Copy), [pb[hh]], [xraw])
                    for col0, bk, n in ((1024, pb[2], 512), (2560, pb[3], 512), (3072, pb[0], 512), (3584, pb[1], 8)):
                        for c in range(8):
                            P.op('pe', lambda e, bk=bk, c=c, col0=col0, n=n: e.matmul(
                                bk[:, 0:n], lhsT=hnT[:, c, :], rhs=wA[:, c, col0:col0 + n],
                                start=(c == 0), stop=(c == 7)), [wA, hnT], [bk])
                    P.op('act', lambda e, vbb=vbb: e.activation(out=vbb[:], in_=pb[2][:], func=AF.Copy), [pb[2]], [vbb])
                    P.dma('sp', lambda e, vbb=vbb, rows=rows: e.dma_start(out=VS[rows, :], in_=vbb[:]), [vbb], [])
                    P.op('act', lambda e: e.activation(out=mva[:, :, 0:128], in_=pb[3][:].rearrange("p (h d) -> p h d", h=4),
                                                       func=AF.Copy), [pb[3]], [mva])
                    P.op('act', lambda e: e.activation(out=sgo[:], in_=pb[0][:], func=AF.Sigmoid), [pb[0]], [sgo])
                    P.op('dve', lambda e: e.tensor_copy(out=ifr[:], in_=pb[1][:, 0:8]), [pb[1]], [ifr])
                    wb = lambda j: cw[:, :, j:j + 1].to_broadcast([128, 8, 128])
                    P.op('dve', lambda e: e.tensor_tensor(out=cacc[:], in0=xraw[:, :, 3:131], in1=wb(0), op=ALU.mult), [xraw, cw], [cacc])
                    P.op('dve', lambda e: e.tensor_tensor(out=cacc[:], in0=cacc[:], in1=cw[:, :, 4:5].to_broadcast([128, 8, 128]),
                                                          op=ALU.add), [cacc, cw], [cacc])
                    for j in range(1, 4):
                        P.op('dve', lambda e, j=j: e.tensor_tensor(out=ctmp[:], in0=xraw[:, :, 3 - j:131 - j], in1=wb(j), op=ALU.mult),
                             [xraw, cw], [ctmp])
                        P.op('dve', lambda e: e.tensor_tensor(out=cacc[:], in0=cacc[:], in1=ctmp[:], op=ALU.add), [cacc, ctmp], [cacc])
                    P.op('act', lambda e: e.activation(out=mqk[:], in_=cacc[:], func=AF.Silu), [cacc], [mqk])
                    P.op('dve', lambda e: e.tensor_copy(out=ctmp[:, :, 0:3], in_=xraw[:, :, 128:131]), [xraw], [ctmp])
                    P.op('dve', lambda e: e.tensor_copy(out=xraw[:, :, 0:3], in_=ctmp[:, :, 0:3]), [ctmp], [xraw])
                    P.op('dve', lambda e: e.tensor_tensor(out=li[:], in0=ifr[:, 0:4], in1=gbb[:, 0:4], op=ALU.add), [ifr, gbb], [li])
                    P.op('dve', lambda e: e.tensor_tensor(out=fz[:], in0=ifr[:, 4:8], in1=gbb[:, 4:8], op=ALU.add), [ifr, gbb], [fz])
                    P.op('act', lambda e: e.activation(out=fz[:], in_=fz[:], func=AF.Exp, scale=-1.0), [fz], [fz])
                    P.op('act', lambda e: e.activation(out=l1[:], in_=fz[:], func=AF.Ln, bias=onec[:], scale=1.0), [fz, onec], [l1])
                    P.op('pe', lambda e: e.matmul(pb[1][:, 16:20], lhsT=triLE[:], rhs=l1[:], start=True, stop=True), [triLE, l1], [pb[1]])
                    P.op('pe', lambda e: e.matmul(pb[1][:, 32:36], lhsT=onesF[:], rhs=l1[:], start=True, stop=True), [onesF, l1], [pb[1]])
                    P.op('dve', lambda e: e.tensor_tensor(out=gtmp[:], in0=li[:], in1=pb[1][:, 16:20], op=ALU.add), [li, pb[1]], [gtmp])
                    P.op('act', lambda e: e.activation(out=gs[:], in_=gtmp[:], func=AF.Exp, bias=lnsc[:], scale=1.0), [gtmp, lnsc], [gs])
                    P.op('act', lambda e: e.activation(out=eq[:], in_=pb[1][:, 16:20], func=AF.Exp, scale=-1.0), [pb[1]], [eq])
                    P.op('act', lambda e: e.activation(out=eb[:], in_=pb[1][:, 32:36], func=AF.Exp, scale=-1.0), [pb[1]], [eb])
                    for h in range(4):
                        qTh_ = mqk[:, h, :]; kTh_ = mqk[:, 4 + h, :]
                        P.op('pe', lambda e, h=h: e.matmul(pb[2][:, 0:128], lhsT=mqk[:, 4 + h, :], rhs=mqk[:, h, :], start=True, stop=True),
                             [mqk], [pb[2]])
                        P.op('dve', lambda e, h=h: e.scalar_tensor_tensor(out=STt[:], in0=pb[2][:, 0:128], scalar=gs[:, h:h + 1],
                                                                          in1=triLE[:], op0=ALU.mult, op1=ALU.mult),
                             [pb[2], gs, triLE], [STt])
                        pv3 = pb[3][:].bitcast(BF16)
                        P.op('pe', lambda e, h=h, pv3=pv3: e.transpose(out=pv3[:, 0:128], in_=mqk[:, 4 + h, :], identity=ident[:]),
                             [mqk, ident], [pb[3]])
                        P.op('act', lambda e, h=h, pv3=pv3: e.activation(out=ktil[:], in_=pv3[:, 0:128], func=AF.Copy, scale=gs[:, h:h + 1]),
                             [pb[3], gs], [ktil])
                        P.op('pe', lambda e, h=h: e.matmul(pb[0][:, 0:129], lhsT=STt[:], rhs=mva[:, h, :], start=True, stop=False),
                             [STt, mva], [pb[0]])
                        P.op('pe', lambda e, h=h: e.matmul(pb[0][:, 0:129], lhsT=mqk[:, h, :], rhs=C16[h][:], start=False, stop=True),
                             [mqk, C16[h]], [pb[0]])
                        P.op('pe', lambda e, h=h: e.matmul(pb[2][:, 256:385], lhsT=ktil[:], rhs=mva[:, h, :], start=True, stop=True),
                             [ktil, mva], [pb[2]])
                        P.op('act', lambda e, h=h: e.activation(out=tmpC[:], in_=C32[h][:], func=AF.Copy, scale=eb[:, h:h + 1]),
                             [C32[h], eb], [tmpC])
                        P.op('dve', lambda e, h=h: e.scalar_tensor_tensor(out=C32[h][:], in0=pb[2][:, 256:385], scalar=eb[:, h:h + 1],
                                                                          in1=tmpC[:], op0=ALU.mult, op1=ALU.add),
                             [pb[2], eb, tmpC], [C32[h]])
                        P.op('act', lambda e, h=h: e.activation(out=C16[h][:], in_=C32[h][:], func=AF.Copy), [C32[h]], [C16[h]])
                        P.op('act', lambda e, h=h: e.activation(out=dn[:], in_=pb[0][:, 128:129], func=AF.Abs, scale=eq[:, h:h + 1]),
                             [pb[0], eq], [dn])
                        P.op('dve', lambda e: e.tensor_single_scalar(out=dn[:], in_=dn[:], scalar=1.0, op=ALU.max), [dn], [dn])
                        P.op('dve', lambda e: e.reciprocal(out=dn[:], in_=dn[:]), [dn], [dn])
                        P.op('dve', lambda e, h=h: e.tensor_tensor(out=scl[:], in0=dn[:], in1=eq[:, h:h + 1], op=ALU.mult), [dn, eq], [scl])
                        P.op('dve', lambda e, h=h, omb=omb: e.scalar_tensor_tensor(
                            out=omb[:, h * 128:(h + 1) * 128], in0=pb[0][:, 0:128], scalar=scl[:], in1=sgo[:, h * 128:(h + 1) * 128],
                            op0=ALU.mult, op1=ALU.mult), [pb[0], scl, sgo], [omb])
                    P.dma('sp', lambda e, omb=omb, rows=rows: e.dma_start(out=OMLS[rows, :], in_=omb[:]), [omb], [])
                P.barrier()
            with ExitStack() as ph:
                _phase0(ph)
            if stop_after == 'p1':
                P.emit()
                return nc

            P.serialize = SER_P3
            def _phase1(ph):
                negtri = P.sb("p3_negtri", [128, 128], BF16, ph)
                triGE = P.sb("p3_triGE", [128, 128], BF16, ph)
                onesb = P.sb("p3_onesb", [128, 128], BF16, ph)
                zerob = P.sb("p3_zerob", [128, 512], BF16, ph)
                P.op('pool', lambda e: e.memset(onesb[:], 1.0), [], [onesb])
                P.op('pool', lambda e: e.memset(zerob[:], 0.0), [], [zerob])
                for tt, val in ((negtri, -30000.0), (triGE, 1.0)):
                    P.op('pool', lambda e, tt=tt, val=val: e.memset(tt[:], val), [], [tt])
                    P.op('pool', lambda e, tt=tt: e.affine_select(out=tt[:], in_=tt[:], pattern=[[-1, 128]], compare_op=ALU.is_ge,
                                                                  fill=0.0, base=0, channel_multiplier=1), [tt], [tt])
                kTh_t = [P.sb(f"p3_kT{i}", [64, T], BF16, ph) for i in range(2)]
                qTh_t = [P.sb(f"p3_qT{i}", [64, T], BF16, ph) for i in range(2)]
                vhp = P.sb("p3_vhp", [128, NBLK, 128], BF16, ph)
                e32 = [P.sb(f"p3_e32_{i}", [128, 512], F32, ph) for i in range(2)]
                sp16 = [P.sb(f"p3_sp16_{i}", [128, 512], BF16, ph) for i in range(2)]
                g32 = [P.sb(f"p3_g32_{i}", [128, 512], F32, ph) for i in range(2)]
                A16 = [P.sb(f"p3_A16_{i}", [128, 512], BF16, ph) for i in range(2)]
                R32 = P.sb("p3_R32", [128, 512], F32, ph)
                R16 = [P.sb(f"p3_R16_{i}", [128, 512], BF16, ph) for i in range(2)]
                obuf = [P.sb(f"p3_obuf{i}", [128, 512], BF16, ph) for i in range(2)]
                pz = [pb[0], pb[1]]; pc = [pb[2], pb[3]]
                po = [P.ps(f"p3_po{i}", [128, 512], F32, ph) for i in range(2)]
                stepn = 0; gcnt = 0
                for h in range(8):
                    hp, hq = h // 2, h % 2
                    kt = kTh_t[h % 2]; qt = qTh_t[h % 2]
                    P.dma('sp', lambda e, kt=kt, h=h: e.dma_start(out=kt[:], in_=KTS[h]), [], [kt])
                    P.dma('sp', lambda e, qt=qt, h=h: e.dma_start(out=qt[:], in_=QTS[h]), [], [qt])
                    if hq == 0:
                        P.dma('sp', lambda e, hp=hp: e.dma_start(
                            out=vhp[:], in_=VS[:, hp * 128:(hp + 1) * 128].rearrange("(kb s) c -> s kb c", s=128)), [], [vhp])
                    for G in range(NBLK // 4):
                        pob = po[gcnt % 2]; ob_ = obuf[gcnt % 2]; gcnt += 1
                        P.op('pool', lambda e: e.memset(R32[:], 0.0), [], [R32])
                        P.op('pe', lambda e, pob=pob: e.matmul(pob[:], lhsT=onesb[:], rhs=zerob[:], start=True, stop=False),
                             [onesb, zerob], [pob])
                        first = True
                        for kb in range(4 * G + 3, -1, -1):
                            off = max(0, kb - 4 * G) * 128
                            rng = slice(off, 512)
                            i2 = stepn % 2; stepn += 1
                            z = pz[i2]; cps = pc[i2]; ee = e32[i2]; ss = sp16[i2]; gg = g32[i2]; aa = A16[i2]
                            diag = kb >= 4 * G
                            P.op('pe', lambda e, z=z, rng=rng, kt=kt, qt=qt, kb=kb, G=G, off=off, diag=diag: e.matmul(
                                z[:, rng], lhsT=kt[:, kb * 128:(kb + 1) * 128], rhs=qt[:, G * 512 + off:(G + 1) * 512],
                                start=True, stop=(not diag)), [kt, qt], [z])
                            if diag:
                                P.op('pe', lambda e, z=z, off=off: e.matmul(z[:, off:off + 128], lhsT=ident[:], rhs=negtri[:],
                                                                            start=False, stop=True), [ident, negtri], [z])
                            P.op('act', lambda e, z=z, ee=ee, rng=rng: e.activation(out=ee[:, rng], in_=z[:, rng], func=AF.Exp), [z], [ee])
                            P.op('act', lambda e, ss=ss, ee=ee, rng=rng: e.activation(out=ss[:, rng], in_=ee[:, rng], func=AF.Ln,
                                                                                      bias=onet[:], scale=1.0), [ee, onet], [ss])
                            Rr = R16[(stepn) % 2]; Rw = R16[(stepn + 1) % 2]
                            P.op('pe', lambda e, cps=cps, ss=ss, rng=rng, first=first: e.matmul(
                                cps[:, rng], lhsT=triGE[:], rhs=ss[:, rng], start=True, stop=first), [triGE, ss], [cps])
                            if not first:
                                P.op('pe', lambda e, cps=cps, Rr=Rr, rng=rng: e.matmul(
                                    cps[:, rng], lhsT=onesb[:], rhs=Rr[:, rng], start=False, stop=True), [onesb, Rr], [cps])
                            P.op('act', lambda e, cps=cps, gg=gg, rng=rng: e.activation(out=gg[:, rng], in_=cps[:, rng], func=AF.Exp,
                                                                                        scale=-1.0), [cps], [gg])
                            P.op('dve', lambda e, aa=aa, ee=ee, gg=gg, rng=rng: e.tensor_tensor(out=aa[:, rng], in0=ee[:, rng],
                                                                                               in1=gg[:, rng], op=ALU.mult),
                                 [ee, gg], [aa])
                            P.op('pe', lambda e, pob=pob, aa=aa, rng=rng, kb=kb: e.matmul(
                                pob[:, rng], lhsT=vhp[:, kb, :], rhs=aa[:, rng], start=False, stop=(kb == 0)), [vhp, aa], [pob])
                            if kb > 0:
                                P.op('pool', lambda e, ss=ss, rng=rng: e.tensor_tensor(out=R32[:, rng], in0=R32[:, rng], in1=ss[:, rng],
                                                                                      op=ALU.add), [R32, ss], [R32])
                                P.op('pool', lambda e, Rw=Rw: e.tensor_copy(out=Rw[:], in_=R32[:]), [R32], [Rw])
                            first = False
                        prow = slice(64 * hq, 64 * hq + 64)
                        P.op('act', lambda e, pob=pob, ob_=ob_, prow=prow: e.activation(out=ob_[prow, :], in_=pob[prow, :], func=AF.Copy),
                             [pob], [ob_])
                        P.dma('sp', lambda e, ob_=ob_, prow=prow, hp=hp, G=G: e.dma_start(
                            out=OSBT[hp, prow, G * 512:(G + 1) * 512], in_=ob_[prow, :]), [ob_], [])
                P.barrier()
            with ExitStack() as ph:
                _phase1(ph)
            if stop_after == 'p3':
                P.emit()
                return nc

            P.serialize = SER_P4A
            def _phase2(ph):
                wG = P.sb("p4_wG", [128, 8, 2048], BF16, ph)
                for pi in (4, 5):
                    c0, c1 = WPIECES[pi]
                    for c in range(8):
                        P.dma('pool', lambda e, c=c, c0=c0, c1=c1, pi=pi: e.dma_start(
                            out=wG[:, c, c0 - 3592:c1 - 3592], in_=w_in_p[pi][c * 128:(c + 1) * 128, :], max_dma_last_dim=4096), [], [wG])
                wsb = P.sb("p4_wsb", [128, 4, D], BF16, ph); wml = P.sb("p4_wml", [128, 4, D], BF16, ph)
                wo = P.sb("p4_wo", [128, 8, D], BF16, ph)
                load_w(wsb, w_bsb, 4); load_w(wml, w_bml, 4); load_w(wo, w_out, 8)
                L1 = ln_params(1, ph, "p4")
                st = P.sb("p4_st", [128, 12], F32, ph); mv = P.sb("p4_mv", [128, 2], F32, ph)
                sd = P.sb("p4_sd", [128, 1], F32, ph); rstd = P.sb("p4_rstd", [128, 1], F32, ph)
                xn = P.sb("p4_xn", [128, D], F32, ph)
                hn = [P.sb(f"p4_hn{i}", [128, D], F32, ph) for i in range(2)]
                hnb = P.sb("p4_hnb", [128, D], BF16, ph)
                hnT = P.sb("p4_hnT", [128, 8, 128], BF16, ph)
                gT = P.sb("p4_gT", [128, 16, 128], BF16, ph)
                osbT = [P.sb(f"p4_osbT{i}", [128, 4, 128], BF16, ph) for i in range(2)]
                omlb = [P.sb(f"p4_oml{i}", [128, 512], BF16, ph) for i in range(2)]
                omlT = P.sb("p4_omlT", [128, 4, 128], BF16, ph)
                t1 = P.sb("p4_t1", [128, D], F32, ph); t2 = P.sb("p4_t2", [128, D], F32, ph)
                yT = P.sb("p4_yT", [128, 8, 128], BF16, ph)
                r1 = P.sb("p4_r1", [128, D], F32, ph)
                h1o = [P.sb(f"p4_h1{i}", [128, D], F32, ph) for i in range(2)]
                L0 = ln_params(0, ph, "p4")
                xt = [P.sb(f"p4_xt{i}", [128, D], F32, ph) for i in range(2)]
                osbB = [P.sb(f"p4_osbB{i}", [128, 4, 128], BF16, ph) for i in range(2)]
                omlB = [P.sb(f"p4_omlB{i}", [128, 512], BF16, ph) for i in range(2)]
                selt = P.sb("p4_sel", [128, 2], F32, ph)
                P.dma('sp', lambda e: e.dma_start(out=selt[:], in_=sel_in), [], [selt])
                for s in range(NO):
                    rows = slice(s * 128, (s + 1) * 128)
                    rowsA = rows; rowsB = slice((NO + s) * 128, (NO + s + 1) * 128)
                    hb = hn[s % 2]; ob_ = osbT[s % 2]; omb = omlb[s % 2]; h1b_ = h1o[s % 2]
                    xb = xt[s % 2]; obB = osbB[s % 2]; omB = omlB[s % 2]
                    P.dma('sp', lambda e, xb=xb, rows=rows: e.dma_start(out=xb[:], in_=x_own[rows, :]), [], [xb])
                    layer_norm(xb, hb, L0, st, mv, sd, rstd, xn)
                    P.dma('sp', lambda e, ob_=ob_, rowsA=rowsA: e.dma_start(out=ob_[:], in_=OSBT[:, :, rowsA].rearrange("hp p t -> p hp t")),
                          [], [ob_])
                    P.dma('sp', lambda e, obB=obB, rowsB=rowsB: e.dma_start(out=obB[:], in_=OSBT[:, :, rowsB].rearrange("hp p t -> p hp t")),
                          [], [obB])
                    P.dma('sp', lambda e, omb=omb, rowsA=rowsA: e.dma_start(out=omb[:], in_=OMLS[rowsA, :]), [], [omb])
                    P.dma('sp', lambda e, omB=omB, rowsB=rowsB: e.dma_start(out=omB[:], in_=OMLS[rowsB, :]), [], [omB])
                    for (ta, tb_, pat) in ((ob_, obB, "p c t -> p (c t)"), (omb, omB, None)):
                        va = ta[:].rearrange(pat) if pat else ta[:]
                        vb_ = tb_[:].rearrange(pat) if pat else tb_[:]
                        P.op('dve', lambda e, va=va: e.tensor_scalar(out=va, in0=va, scalar1=selt[:, 0:1], scalar2=None, op0=ALU.mult),
                             [ta, selt], [ta])
                        P.op('dve', lambda e, va=va, vb_=vb_: e.scalar_tensor_tensor(out=va, in0=vb_, scalar=selt[:, 1:2], in1=va,
                                                                                  op0=ALU.mult, op1=ALU.add), [tb_, selt, ta], [ta])
                    P.op('act', lambda e, hb=hb: e.activation(out=hnb[:], in_=hb[:], func=AF.Copy), [hb], [hnb])
                    transpose_to(hnb, 8, pb[0], hnT)
                    transpose_to(omb, 4, pb[1], omlT)
                    for ch in range(16):
                        bk = pb[ch // 4]; cs = slice((ch % 4) * 128, (ch % 4 + 1) * 128)
                        for c in range(8):
                            P.op('pe', lambda e, bk=bk, cs=cs, c=c, ch=ch: e.matmul(
                                bk[:, cs], lhsT=wG[:, c, ch * 128:(ch + 1) * 128], rhs=hnT[:, c, :],
                                start=(c == 0), stop=(c == 7)), [wG, hnT], [bk])
                    for q4 in range(4):
                        P.op('act', lambda e, q4=q4: e.activation(out=gT[:, q4 * 4:(q4 + 1) * 4, :].rearrange("p c t -> p (c t)"),
                                                                  in_=pb[q4][:], func=AF.Sigmoid), [pb[q4]], [gT])
                    for dc in range(8):
                        cs = slice((dc % 4) * 128, (dc % 4 + 1) * 128)
                        for hp in range(4):
                            P.op('pe', lambda e, dc=dc, cs=cs, hp=hp, ob_=ob_: e.matmul(
                                pb[dc // 4][:, cs], lhsT=wsb[:, hp, dc * 128:(dc + 1) * 128], rhs=ob_[:, hp, :],
                                start=(hp == 0), stop=(hp == 3)), [wsb, ob_], [pb[dc // 4]])
                        for fc in range(4):
                            P.op('pe', lambda e, dc=dc, cs=cs, fc=fc: e.matmul(
                                pb[2 + dc // 4][:, cs], lhsT=wml[:, fc, dc * 128:(dc + 1) * 128], rhs=omlT[:, fc, :],
                                start=(fc == 0), stop=(fc == 3)), [wml, omlT], [pb[2 + dc // 4]])
                    for hf in range(2):
                        cs = slice(hf * 512, (hf + 1) * 512)
                        gsb_ = gT[:, hf * 4:(hf + 1) * 4, :].rearrange("p c t -> p (c t)")
                        gml_ = gT[:, 8 + hf * 4:8 + (hf + 1) * 4, :].rearrange("p c t -> p (c t)")
                        P.op('dve', lambda e, cs=cs, hf=hf, gsb_=gsb_: e.tensor_tensor(out=t1[:, cs], in0=gsb_, in1=pb[hf][:], op=ALU.mult),
                             [gT, pb[hf]], [t1])
                        P.op('dve', lambda e, cs=cs, hf=hf, gml_=gml_: e.tensor_tensor(out=t2[:, cs], in0=gml_, in1=pb[2 + hf][:], op=ALU.mult),
                             [gT, pb[2 + hf]], [t2])
                    P.op('dve', lambda e: e.tensor_tensor(out=yT[:].rearrange("p c t -> p (c t)"), in0=t1[:], in1=t2[:], op=ALU.add),
                         [t1, t2], [yT])
                    for hf in range(2):
                        cs = slice(hf * 512, (hf + 1) * 512)
                        for dc in range(8):
                            P.op('pe', lambda e, hf=hf, cs=cs, dc=dc: e.matmul(pb[hf][:], lhsT=yT[:, dc, :], rhs=wo[:, dc, cs],
                                                                             start=(dc == 0), stop=(dc == 7)), [yT, wo], [pb[hf]])
                        P.op('dve', lambda e, hf=hf, cs=cs, hb=hb: e.scalar_tensor_tensor(out=r1[:, cs], in0=hb[:, cs], scalar=ALPHA,
                                                                                        in1=pb[hf][:], op0=ALU.mult, op1=ALU.add),
                             [hb, pb[hf]], [r1])
                    layer_norm(r1, h1b_, L1, st, mv, sd, rstd, xn)
                    P.dma('sp', lambda e, h1b_=h1b_, rows=rows: e.dma_start(out=H1S[rows, :], in_=h1b_[:]), [h1b_], [])
                P.barrier()
            with ExitStack() as ph:
                _phase2(ph)
            if stop_after == 'p4a':
                P.emit()
                nc._plog = P.log
                return nc

        if not with_mix:
            def _phase3(ph):
                xt = [P.sb(f"a_xt{i}", [128, D], F32, ph) for i in range(2)]
                st = P.sb("a_st", [128, 12], F32, ph); mv = P.sb("a_mv", [128, 2], F32, ph)
                sd = P.sb("a_sd", [128, 1], F32, ph); rstd = P.sb("a_rstd", [128, 1], F32, ph)
                xn = P.sb("a_xn", [128, D], F32, ph); hn = P.sb("a_hn", [128, D], F32, ph)
                r1 = P.sb("a_r1", [128, D], F32, ph)
                h1o = [P.sb(f"a_h1{i}", [128, D], F32, ph) for i in range(2)]
                L0 = ln_params(0, ph, "a"); L1 = ln_params(1, ph, "a")
                for s in range(NO):
                    rows = slice(s * 128, (s + 1) * 128)
                    xb = xt[s % 2]; hb = h1o[s % 2]
                    P.dma('sp', lambda e, xb=xb, rows=rows: e.dma_start(out=xb[:], in_=x_own[rows, :]), [], [xb])
                    layer_norm(xb, hn, L0, st, mv, sd, rstd, xn)
                    P.op('act', lambda e: e.activation(out=r1[:], in_=hn[:], func=AF.Copy, scale=ALPHA), [hn], [r1])
                    layer_norm(r1, hb, L1, st, mv, sd, rstd, xn)
                    P.dma('sp', lambda e, hb=hb, rows=rows: e.dma_start(out=H1S[rows, :], in_=hb[:]), [hb], [])
                P.barrier()

            with ExitStack() as ph:
                _phase3(ph)
        P.serialize = SER_P4B
        def _phase4(ph):
            wpg = P.sb("b_wpg", [128, 8, D], BF16, ph)
            wple = P.sb("b_wple", [128, 2, D], BF16, ph)
            wq = P.sb("b_wq", [128, 8, 2048], BF16, ph)
            wql = P.sb("b_wql", [128, 8, 2048], BF16, ph)
            L2 = ln_params(2, ph, "b")
            kT = [P.sb(f"b_kT{i}", [128, 128], F32, ph) for i in range(2)]
            ktmp = P.sb("b_ktmp", [128, 128], F32, ph)
            kTh = [P.sb(f"b_kTh{i}", [128, 128], BF16, ph) for i in range(2)]
            kTl = [P.sb(f"b_kTl{i}", [128, 128], BF16, ph) for i in range(2)]
            iota16 = P.sb("b_iota16", [128, 16], F32, ph)
            load_w(wpg, w_pg, 8)
            load_w(wple, w_ple, 2)
            load_w(wq, peer_wq, 8)
            P.op('pool', lambda e: e.iota(iota16[:], pattern=[[1, 16]], base=0, channel_multiplier=0,
                                          allow_small_or_imprecise_dtypes=True), [], [iota16])
            for i, kk in enumerate((peer_k1, peer_k2)):
                P.dma('sp', lambda e, kk=kk: e.dma_start(out=ktmp[:], in_=kk), [], [ktmp])
                P.op('pe', lambda e: e.transpose(out=pb[0][:, 0:128], in_=ktmp[:], identity=identf[:]),
                     [ktmp, identf], [pb[0]])
                P.op('dve', lambda e, i=i: e.tensor_copy(out=kT[i][:], in_=pb[0][:, 0:128]), [pb[0]], [kT[i]])
                P.op('dve', lambda e, i=i: e.tensor_copy(out=kTh[i][:], in_=kT[i][:]), [kT[i]], [kTh[i]])
                P.op('dve', lambda e, i=i: e.tensor_tensor(out=kTl[i][:], in0=kT[i][:], in1=kTh[i][:], op=ALU.subtract),
                     [kT[i], kTh[i]], [kTl[i]])

            st = P.sb("b_st", [128, 12], F32, ph); mv = P.sb("b_mv", [128, 2], F32, ph)
            sd = P.sb("b_sd", [128, 1], F32, ph); rstd = P.sb("b_rstd", [128, 1], F32, ph)
            xn = P.sb("b_xn", [128, D], F32, ph)
            h1t = [P.sb(f"b_h1{i}", [128, D], F32, ph) for i in range(2)]
            ptl = [P.sb(f"b_pt{i}", [128, 256], F32, ph) for i in range(2)]
            ptb = P.sb("b_ptb", [128, 256], BF16, ph)
            h1b = P.sb("b_h1b", [128, D], BF16, ph)
            h1T = P.sb("b_h1T", [128, 8, 128], BF16, ph)
            h1l = P.sb("b_h1l", [128, D], BF16, ph)
            h1Tl = P.sb("b_h1Tl", [128, 8, 128], BF16, ph)
            qhi = P.sb("b_qhi", [128, 2048], BF16, ph)
            qlo = P.sb("b_qlo", [128, 2048], BF16, ph)
            pT = P.sb("b_pT", [128, 2, 128], BF16, ph)
            ple = P.sb("b_ple", [128, D], F32, ph)
            r2 = P.sb("b_r2", [128, D], F32, ph)
            ot = [P.sb(f"b_ot{i}", [128, D], F32, ph) for i in range(2)]
            bigA = P.sb("b_bigA", [128, 2048], F32, ph)
            bigB = P.sb("b_bigB", [128, 2048], F32, ph)
            bigC = P.sb("b_bigC", [128, 2048], F32, ph)
            bigD = P.sb("b_bigD", [128, 2048], F32, ph)
            V16 = P.sb("b_V16", [128, 16, 16], F32, ph)
            I16 = P.sb("b_I16", [128, 16, 16], U32, ph)
            I16f = P.sb("b_I16f", [128, 16, 16], F32, ph)
            i1s = P.sb("b_i1s", [128, 8, 16], F32, ph)
            tops = P.sb("b_tops", [128, 8, 16], F32, ph)
            posu = P.sb("b_posu", [128, 8, 16], U32, ph)
            pau = P.sb("b_pau", [128, 8, 16], U32, ph)
            pbu = P.sb("b_pbu", [128, 8, 16], U32, ph)
            paf = P.sb("b_paf", [128, 8, 16], F32, ph)
            pbf = P.sb("b_pbf", [128, 8, 16], F32, ph)
            e1 = P.sb("b_e1", [128, 8, 16], F32, ph)
            e2 = P.sb("b_e2", [128, 8, 16], F32, ph)
            idxf = P.sb("b_idxf", [128, 128], F32, ph)
            idxi = [P.sb(f"b_idxi{i}", [128, 128], I32, ph) for i in range(2)]
            gex = P.sb("b_gex", [128, 8, 16], F32, ph)
            gz = P.sb("b_gz", [128, 8], F32, ph)
            gates = P.sb("b_gates", [128, 8, 16], F32, ph)
            apre = P.sb("b_apre", [128, 128], F32, ph)
            wgt = P.sb("b_wgt", [128, 128], F32, ph)
            junk = P.sb("b_junk", [128, D], BF16, ph)
            NG = 4
            gb = [P.sb(f"b_gb{i}", [128, D], F32, ph) for i in range(NG)]
            gcount = [0]
            acc = P.ps("b_acc", [128, D], F32, ph)
            h1ps = P.ps("b_h1ps", [128, D], F32, ph)

            for c in range(8):
                P.dma('sp', lambda e, c=c: e.dma_start(out=bigA[:], in_=peer_wq[c * 128:(c + 1) * 128, :]), [], [bigA])
                P.op('dve', lambda e, c=c: e.tensor_tensor(out=wql[:, c, :], in0=bigA[:], in1=wq[:, c, :], op=ALU.subtract),
                     [bigA, wq], [wql])
            for s in range(NO):
                rows = slice(s * 128, (s + 1) * 128)
                h1 = h1t[s % 2]; pl = ptl[s % 2]; ob = ot[s % 2]; ixi = idxi[s % 2]
                P.dma('sp', lambda e, h1=h1, rows=rows: e.dma_start(out=h1[:], in_=H1S[rows, :]), [], [h1])
                P.dma('sp', lambda e, pl=pl, rows=rows: e.dma_start(out=pl[:], in_=p_own[rows, :]), [], [pl])
                P.op('act', lambda e, h1=h1: e.activation(out=h1b[:], in_=h1[:], func=AF.Copy), [h1], [h1b])
                transpose_to(h1b, 8, pb[0], h1T)
                if with_peer:
                    P.op('dve', lambda e, h1=h1: e.tensor_tensor(out=h1l[:], in0=h1[:], in1=h1b[:], op=ALU.subtract),
                         [h1, h1b], [h1l])
                    transpose_to(h1l, 8, pb[1], h1Tl)
                P.op('dve', lambda e, pl=pl: e.tensor_copy(out=ptb[:], in_=pl[:]), [pl], [ptb])
                transpose_to(ptb, 2, pb[1], pT)
                if with_peer:
                    P.op('act', lambda e, h1=h1: e.activation(out=h1ps[:], in_=h1[:], func=AF.Copy), [h1], [h1ps])
                    for ch in range(16):
                        bk = pb[ch // 4]; cs = slice((ch % 4) * 128, (ch % 4 + 1) * 128)
                        for pi, (wt, ht) in enumerate(((wq, h1T), (wql, h1T), (wq, h1Tl))):
                            for c in range(8):
                                P.op('pe', lambda e, bk=bk, cs=cs, c=c, ch=ch, wt=wt, ht=ht, pi=pi: e.matmul(
                                    bk[:, cs], lhsT=wt[:, c, ch * 128:(ch + 1) * 128], rhs=ht[:, c, :],
                                    start=(c == 0 and pi == 0), stop=(c == 7 and pi == 2)), [wt, ht], [bk])
                    for q4 in range(4):
                        P.op('act', lambda e, q4=q4: e.activation(out=qhi[:, q4 * 512:(q4 + 1) * 512], in_=pb[q4][:],
                                                                  func=AF.Copy), [pb[q4]], [qhi])
                        P.op('dve', lambda e, q4=q4: e.tensor_tensor(out=qlo[:, q4 * 512:(q4 + 1) * 512], in0=pb[q4][:],
                                                                     in1=qhi[:, q4 * 512:(q4 + 1) * 512], op=ALU.subtract),
                             [pb[q4], qhi], [qlo])
                    for ch in range(16):
                        bk = pb[ch // 4]; cs = slice((ch % 4) * 128, (ch % 4 + 1) * 128)
                        for pi, (qt, kt) in enumerate(((qhi, kTh), (qlo, kTh), (qhi, kTl))):
                            P.op('pe', lambda e, bk=bk, cs=cs, ch=ch, qt=qt, kt=kt, pi=pi: e.matmul(
                                bk[:, cs], lhsT=qt[:, ch * 128:(ch + 1) * 128], rhs=kt[ch % 2][:],
                                start=(pi == 0), stop=(pi == 2)), [qt, kt[ch % 2]], [bk])
                    for q4 in range(4):
                        P.op('act', lambda e, q4=q4: e.activation(out=bigB[:, q4 * 512:(q4 + 1) * 512], in_=pb[q4][:],
                                                                  func=AF.Copy), [pb[q4]], [bigB])
                    for ch in range(16):
                        seg = slice(ch * 128, (ch + 1) * 128)
                        P.op('dve', lambda e, ch=ch, seg=seg: e.max(out=V16[:, ch, 0:8], in_=bigB[:, seg]), [bigB], [V16])
                        P.op('dve', lambda e, ch=ch, seg=seg: e.match_replace(out=bigC[:, seg], in_to_replace=V16[:, ch, 0:8],
                                                                              in_values=bigB[:, seg], imm_value=-1e30),
                             [bigB, V16], [bigC])
                        P.op('dve', lambda e, ch=ch, seg=seg: e.max(out=V16[:, ch, 8:16], in_=bigC[:, seg]), [bigC], [V16])
                        P.op('dve', lambda e, ch=ch, seg=seg: e.max_index(out=I16[:, ch, 0:8], in_max=V16[:, ch, 0:8],
                                                                          in_values=bigB[:, seg]), [bigB, V16], [I16])
                        P.op('dve', lambda e, ch=ch, seg=seg: e.max_index(out=I16[:, ch, 8:16], in_max=V16[:, ch, 8:16],
                                                                          in_values=bigB[:, seg]), [bigB, V16], [I16])
                    P.op('dve', lambda e: e.tensor_copy(out=I16f[:], in_=I16[:]), [I16], [I16f])
                    Vv = V16[:].rearrange("p (h two) k -> p h two k", two=2)
                    Iv = I16f[:].rearrange("p (h two) k -> p h two k", two=2)
                    P.op('dve', lambda e: e.tensor_scalar(out=i1s[:], in0=Iv[:, :, 0, :], scalar1=128.0, scalar2=None,
                                                          op0=ALU.mult), [I16f], [i1s])
                    c4 = lambda t: t[:].rearrange("p (h a b) -> p h a b", h=8, a=16)
                    bc_a = lambda ap: ap.unsqueeze(3).to_broadcast([128, 8, 16, 16])
                    bc_b = lambda ap: ap.unsqueeze(2).to_broadcast([128, 8, 16, 16])
                    P.op('dve', lambda e: e.tensor_tensor(out=c4(bigA), in0=bc_a(Vv[:, :, 0, :]), in1=bc_b(Vv[:, :, 1, :]),
                                                          op=ALU.add), [V16], [bigA])
                    P.op('dve', lambda e: e.tensor_tensor(out=c4(bigD), in0=bc_a(i1s[:]), in1=bc_b(Iv[:, :, 1, :]),
                                                          op=ALU.add), [i1s, I16f], [bigD])
                    for h in range(8):
                        seg = slice(h * 256, (h + 1) * 256)
                        P.op('dve', lambda e, h=h, seg=seg: e.max(out=tops[:, h, 0:8], in_=bigA[:, seg]), [bigA], [tops])
                        P.op('dve', lambda e, h=h, seg=seg: e.match_replace(out=bigC[:, seg], in_to_replace=tops[:, h, 0:8],
                                                                            in_values=bigA[:, seg], imm_value=-1e30),
                             [bigA, tops], [bigC])
                        P.op('dve', lambda e, h=h, seg=seg: e.max(out=tops[:, h, 8:16], in_=bigC[:, seg]), [bigC], [tops])
                        P.op('dve', lambda e, h=h, seg=seg: e.max_index(out=posu[:, h, 0:8], in_max=tops[:, h, 0:8],
                                                                        in_values=bigA[:, seg]), [bigA, tops], [posu])
                        P.op('dve', lambda e, h=h, seg=seg: e.max_index(out=posu[:, h, 8:16], in_max=tops[:, h, 8:16],
                                                                        in_values=bigA[:, seg]), [bigA, tops], [posu])
                    P.op('dve', lambda e: e.tensor_single_scalar(out=pau[:], in_=posu[:], scalar=4,
                                                                 op=ALU.logical_shift_right), [posu], [pau])
                    P.op('dve', lambda e: e.tensor_single_scalar(out=pbu[:], in_=posu[:], scalar=15,
                                                                 op=ALU.bitwise_and), [posu], [pbu])
                    P.op('dve', lambda e: e.tensor_copy(out=paf[:], in_=pau[:]), [pau], [paf])
                    P.op('dve', lambda e: e.tensor_copy(out=pbf[:], in_=pbu[:]), [pbu], [pbf])
                    io4 = iota16[:].unsqueeze(1).unsqueeze(1).to_broadcast([128, 8, 16, 16])
                    for (pf, src_ap, res_t, rd) in ((paf, i1s[:], e1, [i1s]), (pbf, Iv[:, :, 1, :], e2, [I16f])):
                        P.op('dve', lambda e, pf=pf: e.tensor_tensor(out=c4(bigB), in0=bc_a(pf[:]), in1=io4, op=ALU.is_equal),
                             [pf, iota16], [bigB])
                        P.op('dve', lambda e, src_ap=src_ap: e.tensor_tensor(out=c4(bigC), in0=c4(bigB), in1=bc_b(src_ap),
                                                                             op=ALU.mult), [bigB] + rd, [bigC])
                        P.op('dve', lambda e, res_t=res_t: e.tensor_reduce(out=res_t[:], in_=c4(bigC), axis=AX.X, op=ALU.add),
                             [bigC], [res_t])
                    P.op('dve', lambda e: e.tensor_tensor(out=idxf[:].rearrange("p (h k) -> p h k", h=8), in0=e1[:], in1=e2[:],
                                                          op=ALU.add), [e1, e2], [idxf])
                    P.op('dve', lambda e, ixi=ixi: e.tensor_copy(out=ixi[:], in_=idxf[:]), [idxf], [ixi])
                    P.op('dve', lambda e: e.tensor_tensor(out=gex[:], in0=tops[:],
                                                          in1=tops[:, :, 0:1].to_broadcast([128, 8, 16]), op=ALU.subtract),
                         [tops], [gex])
                    P.op('act', lambda e: e.activation(out=gex[:], in_=gex[:], func=AF.Exp), [gex], [gex])
                    P.op('dve', lambda e: e.tensor_reduce(out=gz[:], in_=gex[:], axis=AX.X, op=ALU.add), [gex], [gz])
                    P.op('dve', lambda e: e.reciprocal(out=gz[:], in_=gz[:]), [gz], [gz])
                    P.op('dve', lambda e: e.tensor_tensor(out=gates[:], in0=gex[:],
                                                          in1=gz[:].unsqueeze(2).to_broadcast([128, 8, 16]), op=ALU.mult),
                         [gex, gz], [gates])
                    for hk in range(128):
                        g = gb[gcount[0] % NG]; gcount[0] += 1
                        P.dma('pool', lambda e, g=g, hk=hk, ixi=ixi: e.indirect_dma_start(
                            out=g[:], out_offset=None, in_=peer_u,
                            in_offset=bass.IndirectOffsetOnAxis(ap=ixi[:, hk:hk + 1], axis=0)), [ixi], [g])
                        P.op('dve', lambda e, g=g, hk=hk: e.scalar_tensor_tensor(
                            out=junk[:], in0=g[:], scalar=1.0, in1=h1ps[:], op0=ALU.mult, op1=ALU.mult,
                            accum_out=apre[:, hk:hk + 1]), [g, h1ps], [junk, apre])
                    if debug:
                        P.dma('sp', lambda e, rows=rows: e.dma_start(out=DBG[rows, 0, :], in_=idxf[:]), [idxf], [])
                        P.dma('sp', lambda e, rows=rows: e.dma_start(out=DBG[rows, 1, :], in_=apre[:]), [apre], [])
                        P.dma('sp', lambda e, rows=rows: e.dma_start(out=DBG[rows, 2, :], in_=gates[:].rearrange("p h k -> p (h k)")), [gates], [])
                    P.op('act', lambda e: e.activation(out=apre[:], in_=apre[:], func=AF.Gelu), [apre], [apre])
                    if debug:
                        P.dma('sp', lambda e, rows=rows: e.dma_start(out=DBG[rows, 3, :], in_=apre[:]), [apre], [])
                    P.op('dve', lambda e: e.tensor_tensor(out=wgt[:], in0=apre[:],
                                                          in1=gates[:].rearrange("p h k -> p (h k)"), op=ALU.mult),
                         [apre, gates], [wgt])
                    for hk in range(128):
                        g = gb[gcount[0] % NG]; gcount[0] += 1
                        P.dma('pool', lambda e, g=g, hk=hk, ixi=ixi: e.indirect_dma_start(
                            out=g[:], out_offset=None, in_=peer_v,
                            in_offset=bass.IndirectOffsetOnAxis(ap=ixi[:, hk:hk + 1], axis=0)), [ixi], [g])
                        if hk == 0:
                            P.op('dve', lambda e, g=g: e.tensor_scalar(
                                out=acc[:], in0=g[:], scalar1=wgt[:, 0:1], scalar2=None, op0=ALU.mult),
                                [g, wgt], [acc])
                        else:
                            P.op('dve', lambda e, g=g, hk=hk: e.scalar_tensor_tensor(
                                out=acc[:], in0=g[:], scalar=wgt[:, hk:hk + 1], in1=acc[:],
                                op0=ALU.mult, op1=ALU.add), [g, wgt, acc], [acc])
                for hf in range(2):
                    cs = slice(hf * 512, (hf + 1) * 512)
                    for c in range(8):
                        P.op('pe', lambda e, c=c, cs=cs, hf=hf: e.matmul(pb[hf][:], lhsT=h1T[:, c, :], rhs=wpg[:, c, cs],
                                                                         start=(c == 0), stop=(c == 7)),
                             [h1T, wpg], [pb[hf]])
                    for c in range(2):
                        P.op('pe', lambda e, c=c, cs=cs, hf=hf: e.matmul(pb[2 + hf][:], lhsT=pT[:, c, :], rhs=wple[:, c, cs],
                                                                         start=(c == 0), stop=(c == 1)),
                             [pT, wple], [pb[2 + hf]])
                    P.op('act', lambda e, cs=cs, hf=hf: e.activation(out=xn[:, cs], in_=pb[hf][:], func=AF.Sigmoid),
                         [pb[hf]], [xn])
                    P.op('dve', lambda e, cs=cs, hf=hf: e.tensor_tensor(out=ple[:, cs], in0=xn[:, cs], in1=pb[2 + hf][:],
                                                                        op=ALU.mult), [xn, pb[2 + hf]], [ple])
                P.op('dve', lambda e, h1=h1: e.scalar_tensor_tensor(out=r2[:], in0=h1[:], scalar=ALPHA, in1=ple[:],
                                                                    op0=ALU.mult, op1=ALU.add), [h1, ple], [r2])
                if with_peer:
                    P.op('dve', lambda e: e.tensor_tensor(out=r2[:], in0=r2[:], in1=acc[:], op=ALU.add), [r2, acc], [r2])
                layer_norm(r2, ob, L2, st, mv, sd, rstd, xn)
                P.dma('sp', lambda e, ob=ob, rows=rows: e.dma_start(out=out[rows, :], in_=ob[:]), [ob], [])
        with ExitStack() as ph:
            _phase4(ph)
        P.emit()
    return nc


_NC = {}
W_NAMES = ['w_ple_gate', 'w_ple', 'peer_wq', 'peer_k1', 'peer_k2', 'peer_u', 'peer_v',
           'w_branch_sb', 'w_branch_ml', 'w_out']


def kernel(_nblk=NB, _with_mix=True, _with_peer=True, _debug=False, _stop=None, **inp):
    key = (_nblk, _with_mix, _with_peer, _debug, _stop)
    if key not in _NC:
        _NC[key] = build_nc(_nblk, _with_mix, _with_peer, _debug, _stop)
    nc = _NC[key]
    T = _nblk * 128
    x = np.asarray(inp['x'], dtype=np.float32)
    p = np.asarray(inp['p'], dtype=np.float32)[0]
    shared = {n: np.ascontiguousarray(np.asarray(inp[n], dtype=np.float32)[0]) for n in W_NAMES}
    w_in_h = np.asarray(inp['w_in'], dtype=np.float32)[0]
    for i, (c0, c1) in enumerate(((0, 1024), (1024, 2048), (2048, 3072), (3072, 3592), (3592, 4616), (4616, 5640))):
        shared[f'w_in_p{i}'] = np.ascontiguousarray(w_in_h[:, c0:c1])
    shared['conv_wb'] = np.ascontiguousarray(np.concatenate([np.asarray(inp['conv_w'], dtype=np.float32)[0],
                                                             np.asarray(inp['conv_b'], dtype=np.float32)], axis=0))
    shared['gate_b'] = np.ascontiguousarray(np.concatenate([np.asarray(inp['b_igate'], dtype=np.float32)[0],
                                                            np.asarray(inp['b_fgate'], dtype=np.float32)[0]])[None, :])
    shared['ln0_g'] = np.ascontiguousarray(inp['ln0_g'], dtype=np.float32)
    shared['ln0_b'] = np.ascontiguousarray(inp['ln0_b'], dtype=np.float32)
    for i in (1, 2):
        shared[f'ln{i}_g'] = np.ascontiguousarray(inp[f'ln{i}_g'][0], dtype=np.float32)
        shared[f'ln{i}_b'] = np.ascontiguousarray(inp[f'ln{i}_b'][0], dtype=np.float32)
    in_maps = []
    TO = T // 2
    for core in range(8):
        b, j = core // 2, core % 2
        m = dict(shared)
        m["x_all"] = np.ascontiguousarray(x[b, :T])
        m["x_own"] = np.ascontiguousarray(x[b, j * TO:(j + 1) * TO])
        m["p_own"] = np.ascontiguousarray(p[b, j * TO:(j + 1) * TO])
        sel = np.zeros((128, 2), dtype=np.float32); sel[:, j] = 1.0
        m["sel"] = sel
        in_maps.append(m)
    res = run_bass_kernel_spmd(nc, in_maps, core_ids=list(range(8)))
    outp = np.empty((4, T, D), dtype=np.float32)
    for core in range(8):
        b, j = core // 2, core % 2
        outp[b, j * TO:(j + 1) * TO] = res.results[core]["out"]
    if _debug:
        return outp, [res.results[2 * b] for b in range(4)]
    return outp
```

```python
import numpy as np
from contextlib import ExitStack
import concourse.bass as bass
import concourse.mybir as mybir
from concourse.bass_utils import run_bass_kernel_spmd

F32 = mybir.dt.float32
BF16 = mybir.dt.bfloat16
I32 = mybir.dt.int32
U32 = mybir.dt.uint32
AF = mybir.ActivationFunctionType
ALU = mybir.AluOpType
AX = mybir.AxisListType

D = 1024
SEQ = 8192
NB = 64
NOWN = 32
ALPHA = 2.0 ** 0.25
EPS = 1e-5
ENGS = ['pe', 'act', 'dve', 'pool', 'sp']
import os
SERIALIZE = False
SER_P1, SER_P3, SER_P4A, SER_P4B = False, False, False, False


class Res:
    __slots__ = ('w', 'r')

    def __init__(self):
        self.w = None
        self.r = {}


class Tile:
    def __init__(self, t):
        self.t = t
        self.res = Res()
        self._sub = {}
        self.dkey = None
        self.dcount = 0

    def __getitem__(self, k):
        return self.t[k]

    def sub(self, k):
        if k not in self._sub:
            self._sub[k] = Res()
        return self._sub[k]


class Prog:
    def __init__(self, nc, es):
        self.nc = nc
        self.es = es
        self.ops = {e: [] for e in ENGS}
        self.cnt = {e: 0 for e in ENGS}
        self.dcnt = {e: 0 for e in ENGS}
        self.seen = {e: {} for e in ENGS}
        self.sem = {}
        for e in ENGS:
            self.sem[('c', e)] = es.enter_context(nc.semaphore('c_' + e))
        self.dtiles = []
        self.log = []
        self.serialize = SERIALIZE

    def sb(self, name, shape, dt, es=None):
        return Tile((es or self.es).enter_context(self.nc.sbuf_tensor(name, shape, dt)))

    def ps(self, name, shape, dt, es=None):
        return Tile((es or self.es).enter_context(self.nc.psum_tensor(name, shape, dt)))

    def barrier(self):
        cur = []
        for e in ENGS:
            if self.cnt[e]:
                cur.append((('c', e), self.cnt[e]))
        for t in self.dtiles:
            cur.append((t.dkey, 16 * t.dcount))
        for e in ENGS:
            waits = [(k, v) for k, v in cur if self.seen[e].get(k, 0) < v and not (e == 'pe' and k == ('c', 'pe'))]
            for k, v in waits:
                self.seen[e][k] = v
            self.ops[e].append((waits, None, None, 0))
            self.log.append((e, 'barrier', waits, None))

    def _deps(self, eng, reads, writes):
        waits = {}
        seen = self.seen[eng]

        def add(k, v):
            if seen.get(k, 0) >= v:
                return
            if waits.get(k, 0) < v:
                waits[k] = v

        for r in reads:
            if r.w is not None:
                add(*r.w)
        for w in writes:
            if w.w is not None:
                add(*w.w)
            for k, v in w.r.items():
                add(k, v)
        for k, v in waits.items():
            seen[k] = v
        return list(waits.items())

    def _record(self, ev, reads, writes):
        k, v = ev
        for r in reads:
            if r.r.get(k, 0) < v:
                r.r[k] = v
        for w in writes:
            w.w = ev
            w.r = {}

    def op(self, eng, fn, reads=(), writes=()):
        reads = [x.res if isinstance(x, Tile) else x for x in reads]
        writes = [x.res if isinstance(x, Tile) else x for x in writes]
        waits = self._deps(eng, reads, writes)
        self.cnt[eng] += 1
        ev = (('c', eng), self.cnt[eng])
        if eng == 'pe':
            self.seen[eng][ev[0]] = ev[1]
        self._record(ev, reads, writes)
        self.ops[eng].append((waits, fn, ev[0], 1))
        self.log.append((eng, 'op', waits, ev))
        if self.serialize:
            self.barrier()

    def dma(self, eng, fn, reads=(), writes=()):
        tiles = [x for x in list(writes) + list(reads) if isinstance(x, Tile)]
        owner = tiles[0]
        if owner.dkey is None:
            owner.dkey = ('t', len(self.dtiles))
            self.sem[owner.dkey] = self.es.enter_context(self.nc.semaphore('t%d' % len(self.dtiles)))
            self.dtiles.append(owner)
        reads = [x.res if isinstance(x, Tile) else x for x in reads]
        writes = [x.res if isinstance(x, Tile) else x for x in writes]
        waits = self._deps(eng, reads, writes)
        owner.dcount += 1
        ev = (owner.dkey, 16 * owner.dcount)
        self._record(ev, reads, writes)
        self.ops[eng].append((waits, fn, ev[0], 16))
        self.log.append((eng, 'dma', waits, ev))
        if self.serialize:
            self.barrier()

    def emit(self):
        nc = self.nc
        handles = {'pe': 'tensor', 'act': 'scalar', 'dve': 'vector', 'pool': 'gpsimd', 'sp': 'sync'}
        with nc.Block() as blk:
            for e in ENGS:
                ops = self.ops[e]
                final = None
                if e == 'sp':
                    final = [(t.dkey, 16 * t.dcount) for t in self.dtiles]

                def body(eng, ops=ops, final=final):
                    for waits, fn, key, inc in ops:
                        for k, v in waits:
                            eng.wait_ge(self.sem[k], v)
                        if fn is not None:
                            fn(eng).then_inc(self.sem[key], inc)
                    if final:
                        for k, v in final:
                            eng.wait_ge(self.sem[k], v)

                getattr(blk, handles[e])(body)


def _r(x):
    return [x] if not isinstance(x, (list, tuple)) else list(x)


def build_nc(NBLK=NB, with_mix=True, with_peer=True, debug=False, stop_after=None):
    T = NBLK * 128
    nc = bass.Bass("TRN2", target_bir_lowering=False)
    dr = lambda name, shape, dt=F32, kind="ExternalInput": nc.dram_tensor(name, shape, dt, kind=kind).ap()
    SCR = "ExternalOutput" if debug else "Internal"
    NO = NBLK // 2
    TO = NO * 128
    x_all = dr("x_all", [T, D])
    x_own = dr("x_own", [TO, D])
    p_own = dr("p_own", [TO, 256])
    sel_in = dr("sel", [128, 2])
    ln_g = [dr(f"ln{i}_g", [D]) for i in range(3)]
    ln_b = [dr(f"ln{i}_b", [D]) for i in range(3)]
    WPIECES = ((0, 1024), (1024, 2048), (2048, 3072), (3072, 3592), (3592, 4616), (4616, 5640))
    w_in_p = [dr(f"w_in_p{i}", [D, c1 - c0]) for i, (c0, c1) in enumerate(WPIECES)]
    conv_wb = dr("conv_wb", [5, D])
    gate_b = dr("gate_b", [1, 8])
    w_bsb = dr("w_branch_sb", [512, D]); w_bml = dr("w_branch_ml", [512, D]); w_out = dr("w_out", [D, D])
    QTS = dr("qts", [8, 64, T], BF16, SCR)
    KTS = dr("kts", [8, 64, T], BF16, SCR)
    VS = dr("vs", [T, 512], BF16, SCR)
    OMLS = dr("omls", [T, 512], BF16, SCR)
    OSBT = dr("osbt", [4, 128, T], BF16, SCR)
    w_pg = dr("w_ple_gate", [D, D])
    w_ple = dr("w_ple", [256, D])
    peer_wq = dr("peer_wq", [D, 2048])
    peer_k1 = dr("peer_k1", [128, 128])
    peer_k2 = dr("peer_k2", [128, 128])
    peer_u = dr("peer_u", [16384, D])
    peer_v = dr("peer_v", [16384, D])
    out = dr("out", [TO, D], F32, "ExternalOutput")
    H1S = dr("h1s", [TO, D], F32, SCR)
    DBG = dr("dbg", [TO, 4, 128], F32, "ExternalOutput") if debug else None

    with ExitStack() as es:
        P = Prog(nc, es)
        ident = P.sb("ident", [128, 128], BF16)
        identf = P.sb("identf", [128, 128], F32)
        epst = P.sb("epst", [128, 1], F32)
        for idt in (ident, identf):
            P.op('pool', lambda e, idt=idt: e.memset(idt[:], 1.0), [], [idt])
            P.op('pool', lambda e, idt=idt: e.affine_select(out=idt[:], in_=idt[:], pattern=[[-1, 128]],
                                                            compare_op=ALU.is_equal, fill=0.0, base=0,
                                                            channel_multiplier=1), [idt], [idt])
        P.op('pool', lambda e: e.memset(epst[:], EPS), [], [epst])
        onet = P.sb("onet", [128, 1], F32)
        P.op('pool', lambda e: e.memset(onet[:], 1.0), [], [onet])
        def ln_params(i, ph, tag):
            g = P.sb(f"{tag}_lng{i}", [128, D], F32, ph); b = P.sb(f"{tag}_lnb{i}", [128, D], F32, ph)
            P.dma('sp', lambda e: e.dma_start(out=g[:], in_=ln_g[i].partition_broadcast(128)), [], [g])
            P.dma('sp', lambda e: e.dma_start(out=b[:], in_=ln_b[i].partition_broadcast(128)), [], [b])
            return (g, b)
        pb = [P.ps(f"pb{i}", [128, 512], F32) for i in range(4)]

        def layer_norm(src, dst, k, st, mv, sd, rstd, xn):
            P.op('dve', lambda e: e.bn_stats(out=st[:, 0:6], in_=src[:, 0:512]), [src], [st])
            P.op('dve', lambda e: e.bn_stats(out=st[:, 6:12], in_=src[:, 512:1024]), [src, st], [st])
            P.op('dve', lambda e: e.bn_aggr(out=mv[:], in_=st[:]), [st], [mv])
            P.op('act', lambda e: e.activation(out=sd[:], in_=mv[:, 1:2], func=AF.Sqrt, bias=epst[:], scale=1.0),
                 [mv, epst], [sd])
            P.op('dve', lambda e: e.reciprocal(out=rstd[:], in_=sd[:]), [sd], [rstd])
            P.op('dve', lambda e: e.tensor_scalar(out=xn[:], in0=src[:], scalar1=mv[:, 0:1], scalar2=rstd[:],
                                                  op0=ALU.subtract, op1=ALU.mult), [src, mv, rstd], [xn])
            P.op('pool', lambda e: e.tensor_tensor(out=xn[:], in0=xn[:], in1=k[0][:], op=ALU.mult),
                 [xn, k[0]], [xn])
            P.op('pool', lambda e: e.tensor_tensor(out=dst[:], in0=xn[:], in1=k[1][:], op=ALU.add),
                 [xn, k[1]], [dst])

        def load_w(dst, src_ap, nchunk, c0=0, c1=None):
            for c in range(nchunk):
                sa = src_ap[c * 128:(c + 1) * 128, :] if c1 is None else src_ap[c * 128:(c + 1) * 128, c0:c1]
                P.dma('pool', lambda e, c=c, sa=sa: e.dma_start(out=dst[:, c, :], in_=sa, max_dma_last_dim=4096),
                      [], [dst])

        def transpose_to(src_b, nchunk, psb, dstT):
            pv = psb[:].bitcast(BF16)
            for c in range(nchunk):
                P.op('pe', lambda e, c=c: e.transpose(out=pv[:, c * 128:(c + 1) * 128],
                                                      in_=src_b[:, c * 128:(c + 1) * 128], identity=ident[:]),
                     [src_b, ident], [psb])
            P.op('dve', lambda e: e.tensor_copy(out=dstT[:].rearrange("p c t -> p (c t)"),
                                                in_=pv[:, 0:nchunk * 128]), [psb], [dstT])

        if with_mix:
            P.serialize = SER_P1
            def _phase0(ph):
                wA = P.sb("p1_wA", [128, 8, 3592], BF16, ph)
                for pi in range(4):
                    c0, c1 = WPIECES[pi]
                    for c in range(8):
                        P.dma('pool', lambda e, c=c, c0=c0, c1=c1, pi=pi: e.dma_start(
                            out=wA[:, c, c0:c1], in_=w_in_p[pi][c * 128:(c + 1) * 128, :], max_dma_last_dim=4096), [], [wA])
                L0 = ln_params(0, ph, "p1")
                cw = P.sb("p1_cw", [128, 8, 5], F32, ph)
                cw5 = P.sb("p1_cw5", [5, D], F32, ph)
                P.dma('sp', lambda e: e.dma_start(out=cw5[:], in_=conv_wb), [], [cw5])
                for c in range(8):
                    P.op('pe', lambda e, c=c: e.transpose(out=pb[0][:, c * 5:(c + 1) * 5], in_=cw5[0:5, c * 128:(c + 1) * 128],
                                                          identity=identf[0:5, 0:5]), [cw5, identf], [pb[0]])
                P.op('dve', lambda e: e.tensor_copy(out=cw[:].rearrange("p c j -> p (c j)"), in_=pb[0][:, 0:40]), [pb[0]], [cw])
                onesF = P.sb("p1_onesF", [128, 128], F32, ph)
                P.op('pool', lambda e: e.memset(onesF[:], 1.0), [], [onesF])
                gb1 = P.sb("p1_gb1", [1, 8], F32, ph)
                P.dma('sp', lambda e: e.dma_start(out=gb1[:], in_=gate_b), [], [gb1])
                gbb = P.sb("p1_gbb", [128, 8], F32, ph)
                P.op('pe', lambda e: e.matmul(pb[1][:, 0:8], lhsT=onesF[0:1, :], rhs=gb1[:], start=True, stop=True), [onesF, gb1], [pb[1]])
                P.op('dve', lambda e: e.tensor_copy(out=gbb[:], in_=pb[1][:, 0:8]), [pb[1]], [gbb])
                triLE = P.sb("p1_triLE", [128, 128], F32, ph)
                P.op('pool', lambda e: e.memset(triLE[:], 1.0), [], [triLE])
                P.op('pool', lambda e: e.affine_select(out=triLE[:], in_=triLE[:], pattern=[[1, 128]], compare_op=ALU.is_ge,
                                                       fill=0.0, base=0, channel_multiplier=-1), [triLE], [triLE])
                xt = [P.sb(f"p1_xt{i}", [128, D], F32, ph) for i in range(2)]
                st = P.sb("p1_st", [128, 12], F32, ph); mv = P.sb("p1_mv", [128, 2], F32, ph)
                sd = P.sb("p1_sd", [128, 1], F32, ph); rstd = P.sb("p1_rstd", [128, 1], F32, ph)
                xn = P.sb("p1_xn", [128, D], F32, ph)
                hn = [P.sb(f"p1_hn{i}", [128, D], F32, ph) for i in range(2)]
                hnb = P.sb("p1_hnb", [128, D], BF16, ph)
                hnT = P.sb("p1_hnT", [128, 8, 128], BF16, ph)
                qTb = [P.sb(f"p1_qTb{i}", [64, 8, 128], BF16, ph) for i in range(2)]
                kTb = [P.sb(f"p1_kTb{i}", [64, 8, 128], BF16, ph) for i in range(2)]
                xraw = P.sb("p1_xraw", [128, 8, 131], F32, ph)
                cacc = P.sb("p1_cacc", [128, 8, 128], F32, ph)
                ctmp = P.sb("p1_ctmp", [128, 8, 128], F32, ph)
                mqk = P.sb("p1_mqk", [128, 8, 128], BF16, ph)
                vb = [P.sb(f"p1_vb{i}", [128, 512], BF16, ph) for i in range(2)]
                mva = P.sb("p1_mva", [128, 4, 129], BF16, ph)
                sgo = P.sb("p1_sgo", [128, 512], F32, ph)
                ifr = P.sb("p1_ifr", [128, 8], F32, ph)
                li = P.sb("p1_li", [128, 4], F32, ph); fz = P.sb("p1_fz", [128, 4], F32, ph)
                l1 = P.sb("p1_l1", [128, 4], F32, ph); gtmp = P.sb("p1_gtmp", [128, 4], F32, ph)
                gs = P.sb("p1_gs", [128, 4], F32, ph); eq = P.sb("p1_eq", [128, 4], F32, ph); eb = P.sb("p1_eb", [128, 4], F32, ph)
                STt = P.sb("p1_ST", [128, 128], BF16, ph)
                ktil = P.sb("p1_ktil", [128, 128], BF16, ph)
                C32 = [P.sb(f"p1_C32_{h}", [128, 129], F32, ph) for h in range(4)]
                C16 = [P.sb(f"p1_C16_{h}", [128, 129], BF16, ph) for h in range(4)]
                tmpC = P.sb("p1_tmpC", [128, 129], F32, ph)
                dn = P.sb("p1_dn", [128, 1], F32, ph); scl = P.sb("p1_scl", [128, 1], F32, ph)
                oml = [P.sb(f"p1_oml{i}", [128, 512], BF16, ph) for i in range(2)]
                P.op('pool', lambda e: e.memset(xraw[:], 0.0), [], [xraw])
                P.op('pool', lambda e: e.memset(mva[:], 1.0), [], [mva])
                for h in range(4):
                    P.op('pool', lambda e, h=h: e.memset(C32[h][:], 0.0), [], [C32[h]])
                    P.op('pool', lambda e, h=h: e.memset(C16[h][:], 0.0), [], [C16[h]])
                LNSC = float(np.log(128.0 ** -0.5))
                lnsc = P.sb("p1_lnsc", [128, 1], F32, ph); onec = P.sb("p1_onec", [128, 1], F32, ph)
                P.op('pool', lambda e: e.memset(lnsc[:], LNSC), [], [lnsc])
                P.op('pool', lambda e: e.memset(onec[:], 1.0), [], [onec])

                for s in range(NBLK):
                    rows = slice(s * 128, (s + 1) * 128)
                    tcol = slice(s * 128, (s + 1) * 128)
                    xb = xt[s % 2]; hb = hn[s % 2]; qb = qTb[s % 2]; kb_ = kTb[s % 2]; vbb = vb[s % 2]; omb = oml[s % 2]
                    P.dma('sp', lambda e, xb=xb, rows=rows: e.dma_start(out=xb[:], in_=x_all[rows, :]), [], [xb])
                    layer_norm(xb, hb, L0, st, mv, sd, rstd, xn)
                    P.op('act', lambda e, hb=hb: e.activation(out=hnb[:], in_=hb[:], func=AF.Copy), [hb], [hnb])
                    transpose_to(hnb, 8, pb[0], hnT)
                    for which, col0, dstb, scale, banks in ((0, 0, qb, 1.0, (0, 1)), (1, 512, kb_, 0.125, (2, 3))):
                        for h in range(8):
                            bk = pb[banks[h // 4]]; cs = slice((h % 4) * 128, (h % 4 + 1) * 128)
                            for c in range(8):
                                P.op('pe', lambda e, bk=bk, cs=cs, c=c, h=h, col0=col0: e.matmul(
                                    bk[0:64, cs], lhsT=wA[:, c, col0 + h * 64:col0 + (h + 1) * 64], rhs=hnT[:, c, :],
                                    start=(c == 0), stop=(c == 7)), [wA, hnT], [bk])
                        for hh in range(2):
                            P.op('act', lambda e, hh=hh, dstb=dstb, scale=scale, banks=banks: e.activation(
                                out=dstb[:, hh * 4:(hh + 1) * 4, :].rearrange("p h t -> p (h t)"), in_=pb[banks[hh]][0:64, :],
                                func=AF.Copy, scale=scale), [pb[banks[hh]]], [dstb])
                    P.dma('sp', lambda e, qb=qb, tcol=tcol: e.dma_start(out=QTS[:, :, tcol].rearrange("h d t -> d h t"), in_=qb[:]), [qb], [])
                    P.dma('sp', lambda e, kb_=kb_, tcol=tcol: e.dma_start(out=KTS[:, :, tcol].rearrange("h d t -> d h t"), in_=kb_[:]), [kb_], [])
                    for cc in range(8):
                        bk = pb[cc // 4]; cs = slice((cc % 4) * 128, (cc % 4 + 1) * 128)
                        for c in range(8):
                            P.op('pe', lambda e, bk=bk, cs=cs, c=c, cc=cc: e.matmul(
                                bk[:, cs], lhsT=wA[:, c, 1536 + cc * 128:1536 + (cc + 1) * 128], rhs=hnT[:, c, :],
                                start=(c == 0), stop=(c == 7)), [wA, hnT], [bk])
                    for hh in range(2):
                        P.op('act', lambda e, hh=hh: e.activation(out=xraw[:, hh * 4:(hh + 1) * 4, 3:131],
                                                                  in_=pb[hh][:].rearrange("p (c t) -> p c t", c=4),
                                                                  func=AF.Copy), [pb[hh]], [xraw])
                    for col0, bk, n in ((1024, pb[2], 512), (2560, pb[3], 512), (3072, pb[0], 512), (3584, pb[1], 8)):
                        for c in range(8):
                            P.op('pe', lambda e, bk=bk, c=c, col0=col0, n=n: e.matmul(
                                bk[:, 0:n], lhsT=hnT[:, c, :], rhs=wA[:, c, col0:col0 + n],
                                start=(c == 0), stop=(c == 7)), [wA, hnT], [bk])
                    P.op('act', lambda e, vbb=vbb: e.activation(out=vbb[:], in_=pb[2][:], func=AF.Copy), [pb[2]], [vbb])
                    P.dma('sp', lambda e, vbb=vbb, rows=rows: e.dma_start(out=VS[rows, :], in_=vbb[:]), [vbb], [])
                    P.op('act', lambda e: e.activation(out=mva[:, :, 0:128], in_=pb[3][:].rearrange("p (h d) -> p h d", h=4),
                                                       func=AF.Copy), [pb[3]], [mva])
                    P.op('act', lambda e: e.activation(out=sgo[:], in_=pb[0][:], func=AF.Sigmoid), [pb[0]], [sgo])
                    P.op('dve', lambda e: e.tensor_copy(out=ifr[:], in_=pb[1][:, 0:8]), [pb[1]], [ifr])
                    wb = lambda j: cw[:, :, j:j + 1].to_broadcast([128, 8, 128])
                    P.op('dve', lambda e: e.tensor_tensor(out=cacc[:], in0=xraw[:, :, 3:131], in1=wb(0), op=ALU.mult), [xraw, cw], [cacc])
                    P.op('dve', lambda e: e.tensor_tensor(out=cacc[:], in0=cacc[:], in1=cw[:, :, 4:5].to_broadcast([128, 8, 128]),
                                                          op=ALU.add), [cacc, cw], [cacc])
                    for j in range(1, 4):
                        P.op('dve', lambda e, j=j: e.tensor_tensor(out=ctmp[:], in0=xraw[:, :, 3 - j:131 - j], in1=wb(j), op=ALU.mult),
                             [xraw, cw], [ctmp])
                        P.op('dve', lambda e: e.tensor_tensor(out=cacc[:], in0=cacc[:], in1=ctmp[:], op=ALU.add), [cacc, ctmp], [cacc])
                    P.op('act', lambda e: e.activation(out=mqk[:], in_=cacc[:], func=AF.Silu), [cacc], [mqk])
                    P.op('dve', lambda e: e.tensor_copy(out=ctmp[:, :, 0:3], in_=xraw[:, :, 128:131]), [xraw], [ctmp])
                    P.op('dve', lambda e: e.tensor_copy(out=xraw[:, :, 0:3], in_=ctmp[:, :, 0:3]), [ctmp], [xraw])
                    P.op('dve', lambda e: e.tensor_tensor(out=li[:], in0=ifr[:, 0:4], in1=gbb[:, 0:4], op=ALU.add), [ifr, gbb], [li])
                    P.op('dve', lambda e: e.tensor_tensor(out=fz[:], in0=ifr[:, 4:8], in1=gbb[:, 4:8], op=ALU.add), [ifr, gbb], [fz])
                    P.op('act', lambda e: e.activation(out=fz[:], in_=fz[:], func=AF.Exp, scale=-1.0), [fz], [fz])
                    P.op('act', lambda e: e.activation(out=l1[:], in_=fz[:], func=AF.Ln, bias=onec[:], scale=1.0), [fz, onec], [l1])
                    P.op('pe', lambda e: e.matmul(pb[1][:, 16:20], lhsT=triLE[:], rhs=l1[:], start=True, stop=True), [triLE, l1], [pb[1]])
                    P.op('pe', lambda e: e.matmul(pb[1][:, 32:36], lhsT=onesF[:], rhs=l1[:], start=True, stop=True), [onesF, l1], [pb[1]])
                    P.op('dve', lambda e: e.tensor_tensor(out=gtmp[:], in0=li[:], in1=pb[1][:, 16:20], op=ALU.add), [li, pb[1]], [gtmp])
                    P.op('act', lambda e: e.activation(out=gs[:], in_=gtmp[:], func=AF.Exp, bias=lnsc[:], scale=1.0), [gtmp, lnsc], [gs])
                    P.op('act', lambda e: e.activation(out=eq[:], in_=pb[1][:, 16:20], func=AF.Exp, scale=-1.0), [pb[1]], [eq])
                    P.op('act', lambda e: e.activation(out=eb[:], in_=pb[1][:, 32:36], func=AF.Exp, scale=-1.0), [pb[1]], [eb])
                    for h in range(4):
                        qTh_ = mqk[:, h, :]; kTh_ = mqk[:, 4 + h, :]
                        P.op('pe', lambda e, h=h: e.matmul(pb[2][:, 0:128], lhsT=mqk[:, 4 + h, :], rhs=mqk[:, h, :], start=True, stop=True),
                             [mqk], [pb[2]])
                        P.op('dve', lambda e, h=h: e.scalar_tensor_tensor(out=STt[:], in0=pb[2][:, 0:128], scalar=gs[:, h:h + 1],
                                                                          in1=triLE[:], op0=ALU.mult, op1=ALU.mult),
                             [pb[2], gs, triLE], [STt])
                        pv3 = pb[3][:].bitcast(BF16)
                        P.op('pe', lambda e, h=h, pv3=pv3: e.transpose(out=pv3[:, 0:128], in_=mqk[:, 4 + h, :], identity=ident[:]),
                             [mqk, ident], [pb[3]])
                        P.op('act', lambda e, h=h, pv3=pv3: e.activation(out=ktil[:], in_=pv3[:, 0:128], func=AF.Copy, scale=gs[:, h:h + 1]),
                             [pb[3], gs], [ktil])
                        P.op('pe', lambda e, h=h: e.matmul(pb[0][:, 0:129], lhsT=STt[:], rhs=mva[:, h, :], start=True, stop=False),
                             [STt, mva], [pb[0]])
                        P.op('pe', lambda e, h=h: e.matmul(pb[0][:, 0:129], lhsT=mqk[:, h, :], rhs=C16[h][:], start=False, stop=True),
                             [mqk, C16[h]], [pb[0]])
                        P.op('pe', lambda e, h=h: e.matmul(pb[2][:, 256:385], lhsT=ktil[:], rhs=mva[:, h, :], start=True, stop=True),
                             [ktil, mva], [pb[2]])
                        P.op('act', lambda e, h=h: e.activation(out=tmpC[:], in_=C32[h][:], func=AF.Copy, scale=eb[:, h:h + 1]),
                             [C32[h], eb], [tmpC])
                        P.op('dve', lambda e, h=h: e.scalar_tensor_tensor(out=C32[h][:], in0=pb[2][:, 256:385], scalar=eb[:, h:h + 1],
                                                                          in1=tmpC[:], op0=ALU.mult, op1=ALU.add),
                             [pb[2], eb, tmpC], [C32[h]])
                        P.op('act', lambda e, h=h: e.activation(out=C16[h][:], in_=C32[h][:], func=AF.Copy), [C32[h]], [C16[h]])
                        P.op('act', lambda e, h=h: e.activation(out=dn[:], in_=pb[0][:, 128:129], func=AF.Abs, scale=eq[:, h:h + 1]),
                             [pb[0], eq], [dn])
                        P.op('dve', lambda e: e.tensor_single_scalar(out=dn[:], in_=dn[:], scalar=1.0, op=ALU.max), [dn], [dn])
                        P.op('dve', lambda e: e.reciprocal(out=dn[:], in_=dn[:]), [dn], [dn])
                        P.op('dve', lambda e, h=h: e.tensor_tensor(out=scl[:], in0=dn[:], in1=eq[:, h:h + 1], op=ALU.mult), [dn, eq], [scl])
                        P.op('dve', lambda e, h=h, omb=omb: e.scalar_tensor_tensor(
                            out=omb[:, h * 128:(h + 1) * 128], in0=pb[0][:, 0:128], scalar=scl[:], in1=sgo[:, h * 128:(h + 1) * 128],
                            op0=ALU.mult, op1=ALU.mult), [pb[0], scl, sgo], [omb])
                    P.dma('sp', lambda e, omb=omb, rows=rows: e.dma_start(out=OMLS[rows, :], in_=omb[:]), [omb], [])
                P.barrier()
            with ExitStack() as ph:
                _phase0(ph)
            if stop_after == 'p1':
                P.emit()
                return nc

            P.serialize = SER_P3
            def _phase1(ph):
                negtri = P.sb("p3_negtri", [128, 128], BF16, ph)
                triGE = P.sb("p3_triGE", [128, 128], BF16, ph)
                onesb = P.sb("p3_onesb", [128, 128], BF16, ph)
                zerob = P.sb("p3_zerob", [128, 512], BF16, ph)
                P.op('pool', lambda e: e.memset(onesb[:], 1.0), [], [onesb])
                P.op('pool', lambda e: e.memset(zerob[:], 0.0), [], [zerob])
                for tt, val in ((negtri, -30000.0), (triGE, 1.0)):
                    P.op('pool', lambda e, tt=tt, val=val: e.memset(tt[:], val), [], [tt])
                    P.op('pool', lambda e, tt=tt: e.affine_select(out=tt[:], in_=tt[:], pattern=[[-1, 128]], compare_op=ALU.is_ge,
                                                                  fill=0.0, base=0, channel_multiplier=1), [tt], [tt])
                kTh_t = [P.sb(f"p3_kT{i}", [64, T], BF16, ph) for i in range(2)]
                qTh_t = [P.sb(f"p3_qT{i}", [64, T], BF16, ph) for i in range(2)]
                vhp = P.sb("p3_vhp", [128, NBLK, 128], BF16, ph)
                e32 = [P.sb(f"p3_e32_{i}", [128, 512], F32, ph) for i in range(2)]
                sp16 = [P.sb(f"p3_sp16_{i}", [128, 512], BF16, ph) for i in range(2)]
                g32 = [P.sb(f"p3_g32_{i}", [128, 512], F32, ph) for i in range(2)]
                A16 = [P.sb(f"p3_A16_{i}", [128, 512], BF16, ph) for i in range(2)]
                R32 = P.sb("p3_R32", [128, 512], F32, ph)
                R16 = [P.sb(f"p3_R16_{i}", [128, 512], BF16, ph) for i in range(2)]
                obuf = [P.sb(f"p3_obuf{i}", [128, 512], BF16, ph) for i in range(2)]
                pz = [pb[0], pb[1]]; pc = [pb[2], pb[3]]
                po = [P.ps(f"p3_po{i}", [128, 512], F32, ph) for i in range(2)]
                stepn = 0; gcnt = 0
                for h in range(8):
                    hp, hq = h // 2, h % 2
                    kt = kTh_t[h % 2]; qt = qTh_t[h % 2]
                    P.dma('sp', lambda e, kt=kt, h=h: e.dma_start(out=kt[:], in_=KTS[h]), [], [kt])
                    P.dma('sp', lambda e, qt=qt, h=h: e.dma_start(out=qt[:], in_=QTS[h]), [], [qt])
                    if hq == 0:
                        P.dma('sp', lambda e, hp=hp: e.dma_start(
                            out=vhp[:], in_=VS[:, hp * 128:(hp + 1) * 128].rearrange("(kb s) c -> s kb c", s=128)), [], [vhp])
                    for G in range(NBLK // 4):
                        pob = po[gcnt % 2]; ob_ = obuf[gcnt % 2]; gcnt += 1
                        P.op('pool', lambda e: e.memset(R32[:], 0.0), [], [R32])
                        P.op('pe', lambda e, pob=pob: e.matmul(pob[:], lhsT=onesb[:], rhs=zerob[:], start=True, stop=False),
                             [onesb, zerob], [pob])
                        kbs = list(range(4 * G + 3, -1, -1))
                        nst = len(kbs)

                        def geom(i):
                            kb = kbs[i]
                            off = max(0, kb - 4 * G) * 128
                            return kb, off, slice(off, 512)

                        def front(i):
                            kb, off, rng = geom(i)
                            z = pz[i % 2]; ee = e32[i % 2]; ss = sp16[i % 2]
                            diag = kb >= 4 * G
                            P.op('pe', lambda e, z=z, rng=rng, kb=kb, off=off, diag=diag, kt=kt, qt=qt, G=G: e.matmul(
                                z[:, rng], lhsT=kt[:, kb * 128:(kb + 1) * 128], rhs=qt[:, G * 512 + off:(G + 1) * 512],
                                start=True, stop=(not diag)), [kt, qt], [z])
                            if diag:
                                P.op('pe', lambda e, z=z, off=off: e.matmul(z[:, off:off + 128], lhsT=ident[:], rhs=negtri[:],
                                                                            start=False, stop=True), [ident, negtri], [z])
                            P.op('act', lambda e, z=z, ee=ee, rng=rng: e.activation(out=ee[:, rng], in_=z[:, rng], func=AF.Exp), [z], [ee])
                            P.op('act', lambda e, ss=ss, ee=ee, rng=rng: e.activation(out=ss[:, rng], in_=ee[:, rng], func=AF.Ln,
                                                                                      bias=onet[:], scale=1.0), [ee, onet], [ss])

                        def cmm(i):
                            kb, off, rng = geom(i)
                            cps = pc[i % 2]; ss = sp16[i % 2]; Rr = R16[i % 2]
                            P.op('pe', lambda e, cps=cps, ss=ss, rng=rng, i=i: e.matmul(
                                cps[:, rng], lhsT=triGE[:], rhs=ss[:, rng], start=True, stop=(i == 0)), [triGE, ss], [cps])
                            if i > 0:
                                P.op('pe', lambda e, cps=cps, Rr=Rr, rng=rng: e.matmul(
                                    cps[:, rng], lhsT=onesb[:], rhs=Rr[:, rng], start=False, stop=True), [onesb, Rr], [cps])

                        def av(i):
                            kb, off, rng = geom(i)
                            aa = A16[i % 2]
                            P.op('pe', lambda e, aa=aa, rng=rng, kb=kb, i=i, pob=pob, nst=nst: e.matmul(
                                pob[:, rng], lhsT=vhp[:, kb, :], rhs=aa[:, rng], start=False, stop=(i == nst - 1)), [vhp, aa], [pob])

                        def back(i):
                            kb, off, rng = geom(i)
                            cps = pc[i % 2]; ee = e32[i % 2]; ss = sp16[i % 2]; gg = g32[i % 2]; aa = A16[i % 2]
                            Rw = R16[(i + 1) % 2]
                            P.op('act', lambda e, cps=cps, gg=gg, rng=rng: e.activation(out=gg[:, rng], in_=cps[:, rng], func=AF.Exp,
                                                                                        scale=-1.0), [cps], [gg])
                            P.op('dve', lambda e, aa=aa, ee=ee, gg=gg, rng=rng: e.tensor_tensor(out=aa[:, rng], in0=ee[:, rng],
                                                                                               in1=gg[:, rng], op=ALU.mult),
                                 [ee, gg], [aa])
                            if i < nst - 1:
                                P.op('pool', lambda e, ss=ss, rng=rng: e.tensor_tensor(out=R32[:, rng], in0=R32[:, rng], in1=ss[:, rng],
                                                                                      op=ALU.add), [R32, ss], [R32])
                                P.op('pool', lambda e, Rw=Rw: e.tensor_copy(out=Rw[:], in_=R32[:]), [R32], [Rw])

                        front(0)
                        for i in range(nst):
                            if i + 1 < nst:
                                front(i + 1)
                            cmm(i)
                            if i >= 1:
                                av(i - 1)
                            back(i)
                        av(nst - 1)
                        prow = slice(64 * hq, 64 * hq + 64)
                        P.op('act', lambda e, pob=pob, ob_=ob_, prow=prow: e.activation(out=ob_[prow, :], in_=pob[prow, :], func=AF.Copy),
                             [pob], [ob_])
                        P.dma('sp', lambda e, ob_=ob_, prow=prow, hp=hp, G=G: e.dma_start(
                            out=OSBT[hp, prow, G * 512:(G + 1) * 512], in_=ob_[prow, :]), [ob_], [])
                P.barrier()
            with ExitStack() as ph:
                _phase1(ph)
            if stop_after == 'p3':
                P.emit()
                return nc

            P.serialize = SER_P4A
            def _phase2(ph):
                wG = P.sb("p4_wG", [128, 8, 2048], BF16, ph)
                for pi in (4, 5):
                    c0, c1 = WPIECES[pi]
                    for c in range(8):
                        P.dma('pool', lambda e, c=c, c0=c0, c1=c1, pi=pi: e.dma_start(
                            out=wG[:, c, c0 - 3592:c1 - 3592], in_=w_in_p[pi][c * 128:(c + 1) * 128, :], max_dma_last_dim=4096), [], [wG])
                wsb = P.sb("p4_wsb", [128, 4, D], BF16, ph); wml = P.sb("p4_wml", [128, 4, D], BF16, ph)
                wo = P.sb("p4_wo", [128, 8, D], BF16, ph)
                load_w(wsb, w_bsb, 4); load_w(wml, w_bml, 4); load_w(wo, w_out, 8)
                L1 = ln_params(1, ph, "p4")
                st = P.sb("p4_st", [128, 12], F32, ph); mv = P.sb("p4_mv", [128, 2], F32, ph)
                sd = P.sb("p4_sd", [128, 1], F32, ph); rstd = P.sb("p4_rstd", [128, 1], F32, ph)
                xn = P.sb("p4_xn", [128, D], F32, ph)
                hn = [P.sb(f"p4_hn{i}", [128, D], F32, ph) for i in range(2)]
                hnb = P.sb("p4_hnb", [128, D], BF16, ph)
                hnT = P.sb("p4_hnT", [128, 8, 128], BF16, ph)
                gT = P.sb("p4_gT", [128, 16, 128], BF16, ph)
                osbT = [P.sb(f"p4_osbT{i}", [128, 4, 128], BF16, ph) for i in range(2)]
                omlb = [P.sb(f"p4_oml{i}", [128, 512], BF16, ph) for i in range(2)]
                omlT = P.sb("p4_omlT", [128, 4, 128], BF16, ph)
                t1 = P.sb("p4_t1", [128, D], F32, ph); t2 = P.sb("p4_t2", [128, D], F32, ph)
                yT = P.sb("p4_yT", [128, 8, 128], BF16, ph)
                r1 = P.sb("p4_r1", [128, D], F32, ph)
                h1o = [P.sb(f"p4_h1{i}", [128, D], F32, ph) for i in range(2)]
                L0 = ln_params(0, ph, "p4")
                xt = [P.sb(f"p4_xt{i}", [128, D], F32, ph) for i in range(2)]
                osbB = [P.sb(f"p4_osbB{i}", [128, 4, 128], BF16, ph) for i in range(2)]
                omlB = [P.sb(f"p4_omlB{i}", [128, 512], BF16, ph) for i in range(2)]
                selt = P.sb("p4_sel", [128, 2], F32, ph)
                P.dma('sp', lambda e: e.dma_start(out=selt[:], in_=sel_in), [], [selt])
                for s in range(NO):
                    rows = slice(s * 128, (s + 1) * 128)
                    rowsA = rows; rowsB = slice((NO + s) * 128, (NO + s + 1) * 128)
                    hb = hn[s % 2]; ob_ = osbT[s % 2]; omb = omlb[s % 2]; h1b_ = h1o[s % 2]
                    xb = xt[s % 2]; obB = osbB[s % 2]; omB = omlB[s % 2]
                    P.dma('sp', lambda e, xb=xb, rows=rows: e.dma_start(out=xb[:], in_=x_own[rows, :]), [], [xb])
                    layer_norm(xb, hb, L0, st, mv, sd, rstd, xn)
                    P.dma('sp', lambda e, ob_=ob_, rowsA=rowsA: e.dma_start(out=ob_[:], in_=OSBT[:, :, rowsA].rearrange("hp p t -> p hp t")),
                          [], [ob_])
                    P.dma('sp', lambda e, obB=obB, rowsB=rowsB: e.dma_start(out=obB[:], in_=OSBT[:, :, rowsB].rearrange("hp p t -> p hp t")),
                          [], [obB])
                    P.dma('sp', lambda e, omb=omb, rowsA=rowsA: e.dma_start(out=omb[:], in_=OMLS[rowsA, :]), [], [omb])
                    P.dma('sp', lambda e, omB=omB, rowsB=rowsB: e.dma_start(out=omB[:], in_=OMLS[rowsB, :]), [], [omB])
                    for (ta, tb_, pat) in ((ob_, obB, "p c t -> p (c t)"), (omb, omB, None)):
                        va = ta[:].rearrange(pat) if pat else ta[:]
                        vb_ = tb_[:].rearrange(pat) if pat else tb_[:]
                        P.op('dve', lambda e, va=va: e.tensor_scalar(out=va, in0=va, scalar1=selt[:, 0:1], scalar2=None, op0=ALU.mult),
                             [ta, selt], [ta])
                        P.op('dve', lambda e, va=va, vb_=vb_: e.scalar_tensor_tensor(out=va, in0=vb_, scalar=selt[:, 1:2], in1=va,
                                                                                  op0=ALU.mult, op1=ALU.add), [tb_, selt, ta], [ta])
                    P.op('act', lambda e, hb=hb: e.activation(out=hnb[:], in_=hb[:], func=AF.Copy), [hb], [hnb])
                    transpose_to(hnb, 8, pb[0], hnT)
                    transpose_to(omb, 4, pb[1], omlT)
                    for ch in range(16):
                        bk = pb[ch // 4]; cs = slice((ch % 4) * 128, (ch % 4 + 1) * 128)
                        for c in range(8):
                            P.op('pe', lambda e, bk=bk, cs=cs, c=c, ch=ch: e.matmul(
                                bk[:, cs], lhsT=wG[:, c, ch * 128:(ch + 1) * 128], rhs=hnT[:, c, :],
                                start=(c == 0), stop=(c == 7)), [wG, hnT], [bk])
                    for q4 in range(4):
                        P.op('act', lambda e, q4=q4: e.activation(out=gT[:, q4 * 4:(q4 + 1) * 4, :].rearrange("p c t -> p (c t)"),
                                                                  in_=pb[q4][:], func=AF.Sigmoid), [pb[q4]], [gT])
                    for dc in range(8):
                        cs = slice((dc % 4) * 128, (dc % 4 + 1) * 128)
                        for hp in range(4):
                            P.op('pe', lambda e, dc=dc, cs=cs, hp=hp, ob_=ob_: e.matmul(
                                pb[dc // 4][:, cs], lhsT=wsb[:, hp, dc * 128:(dc + 1) * 128], rhs=ob_[:, hp, :],
                                start=(hp == 0), stop=(hp == 3)), [wsb, ob_], [pb[dc // 4]])
                        for fc in range(4):
                            P.op('pe', lambda e, dc=dc, cs=cs, fc=fc: e.matmul(
                                pb[2 + dc // 4][:, cs], lhsT=wml[:, fc, dc * 128:(dc + 1) * 128], rhs=omlT[:, fc, :],
                                start=(fc == 0), stop=(fc == 3)), [wml, omlT], [pb[2 + dc // 4]])
                    for hf in range(2):
                        cs = slice(hf * 512, (hf + 1) * 512)
                        gsb_ = gT[:, hf * 4:(hf + 1) * 4, :].rearrange("p c t -> p (c t)")
                        gml_ = gT[:, 8 + hf * 4:8 + (hf + 1) * 4, :].rearrange("p c t -> p (c t)")
                        P.op('dve', lambda e, cs=cs, hf=hf, gsb_=gsb_: e.tensor_tensor(out=t1[:, cs], in0=gsb_, in1=pb[hf][:], op=ALU.mult),
                             [gT, pb[hf]], [t1])
                        P.op('dve', lambda e, cs=cs, hf=hf, gml_=gml_: e.tensor_tensor(out=t2[:, cs], in0=gml_, in1=pb[2 + hf][:], op=ALU.mult),
                             [gT, pb[2 + hf]], [t2])
                    P.op('dve', lambda e: e.tensor_tensor(out=yT[:].rearrange("p c t -> p (c t)"), in0=t1[:], in1=t2[:], op=ALU.add),
                         [t1, t2], [yT])
                    for hf in range(2):
                        cs = slice(hf * 512, (hf + 1) * 512)
                        for dc in range(8):
                            P.op('pe', lambda e, hf=hf, cs=cs, dc=dc: e.matmul(pb[hf][:], lhsT=yT[:, dc, :], rhs=wo[:, dc, cs],
                                                                             start=(dc == 0), stop=(dc == 7)), [yT, wo], [pb[hf]])
                        P.op('dve', lambda e, hf=hf, cs=cs, hb=hb: e.scalar_tensor_tensor(out=r1[:, cs], in0=hb[:, cs], scalar=ALPHA,
                                                                                        in1=pb[hf][:], op0=ALU.mult, op1=ALU.add),
                             [hb, pb[hf]], [r1])
                    layer_norm(r1, h1b_, L1, st, mv, sd, rstd, xn)
                    P.dma('sp', lambda e, h1b_=h1b_, rows=rows: e.dma_start(out=H1S[rows, :], in_=h1b_[:]), [h1b_], [])
                P.barrier()
            with ExitStack() as ph:
                _phase2(ph)
            if stop_after == 'p4a':
                P.emit()
                nc._plog = P.log
                return nc

        if not with_mix:
            def _phase3(ph):
                xt = [P.sb(f"a_xt{i}", [128, D], F32, ph) for i in range(2)]
                st = P.sb("a_st", [128, 12], F32, ph); mv = P.sb("a_mv", [128, 2], F32, ph)
                sd = P.sb("a_sd", [128, 1], F32, ph); rstd = P.sb("a_rstd", [128, 1], F32, ph)
                xn = P.sb("a_xn", [128, D], F32, ph); hn = P.sb("a_hn", [128, D], F32, ph)
                r1 = P.sb("a_r1", [128, D], F32, ph)
                h1o = [P.sb(f"a_h1{i}", [128, D], F32, ph) for i in range(2)]
                L0 = ln_params(0, ph, "a"); L1 = ln_params(1, ph, "a")
                for s in range(NO):
                    rows = slice(s * 128, (s + 1) * 128)
                    xb = xt[s % 2]; hb = h1o[s % 2]
                    P.dma('sp', lambda e, xb=xb, rows=rows: e.dma_start(out=xb[:], in_=x_own[rows, :]), [], [xb])
                    layer_norm(xb, hn, L0, st, mv, sd, rstd, xn)
                    P.op('act', lambda e: e.activation(out=r1[:], in_=hn[:], func=AF.Copy, scale=ALPHA), [hn], [r1])
                    layer_norm(r1, hb, L1, st, mv, sd, rstd, xn)
                    P.dma('sp', lambda e, hb=hb, rows=rows: e.dma_start(out=H1S[rows, :], in_=hb[:]), [hb], [])
                P.barrier()

            with ExitStack() as ph:
                _phase3(ph)
        P.serialize = SER_P4B
        def _phase4(ph):
            wpg = P.sb("b_wpg", [128, 8, D], BF16, ph)
            wple = P.sb("b_wple", [128, 2, D], BF16, ph)
            wq = P.sb("b_wq", [128, 8, 2048], BF16, ph)
            wql = P.sb("b_wql", [128, 8, 2048], BF16, ph)
            L2 = ln_params(2, ph, "b")
            kT = [P.sb(f"b_kT{i}", [128, 128], F32, ph) for i in range(2)]
            ktmp = P.sb("b_ktmp", [128, 128], F32, ph)
            kTh = [P.sb(f"b_kTh{i}", [128, 128], BF16, ph) for i in range(2)]
            kTl = [P.sb(f"b_kTl{i}", [128, 128], BF16, ph) for i in range(2)]
            iota16 = P.sb("b_iota16", [128, 16], F32, ph)
            load_w(wpg, w_pg, 8)
            load_w(wple, w_ple, 2)
            load_w(wq, peer_wq, 8)
            P.op('pool', lambda e: e.iota(iota16[:], pattern=[[1, 16]], base=0, channel_multiplier=0,
                                          allow_small_or_imprecise_dtypes=True), [], [iota16])
            for i, kk in enumerate((peer_k1, peer_k2)):
                P.dma('sp', lambda e, kk=kk: e.dma_start(out=ktmp[:], in_=kk), [], [ktmp])
                P.op('pe', lambda e: e.transpose(out=pb[0][:, 0:128], in_=ktmp[:], identity=identf[:]),
                     [ktmp, identf], [pb[0]])
                P.op('dve', lambda e, i=i: e.tensor_copy(out=kT[i][:], in_=pb[0][:, 0:128]), [pb[0]], [kT[i]])
                P.op('dve', lambda e, i=i: e.tensor_copy(out=kTh[i][:], in_=kT[i][:]), [kT[i]], [kTh[i]])
                P.op('dve', lambda e, i=i: e.tensor_tensor(out=kTl[i][:], in0=kT[i][:], in1=kTh[i][:], op=ALU.subtract),
                     [kT[i], kTh[i]], [kTl[i]])

            st = P.sb("b_st", [128, 12], F32, ph); mv = P.sb("b_mv", [128, 2], F32, ph)
            sd = P.sb("b_sd", [128, 1], F32, ph); rstd = P.sb("b_rstd", [128, 1], F32, ph)
            xn = P.sb("b_xn", [128, D], F32, ph)
            h1t = [P.sb(f"b_h1{i}", [128, D], F32, ph) for i in range(2)]
            ptl = [P.sb(f"b_pt{i}", [128, 256], F32, ph) for i in range(2)]
            ptb = P.sb("b_ptb", [128, 256], BF16, ph)
            h1b = P.sb("b_h1b", [128, D], BF16, ph)
            h1T = P.sb("b_h1T", [128, 8, 128], BF16, ph)
            h1l = P.sb("b_h1l", [128, D], BF16, ph)
            h1Tl = P.sb("b_h1Tl", [128, 8, 128], BF16, ph)
            qhi = P.sb("b_qhi", [128, 2048], BF16, ph)
            qlo = P.sb("b_qlo", [128, 2048], BF16, ph)
            pT = P.sb("b_pT", [128, 2, 128], BF16, ph)
            ple = P.sb("b_ple", [128, D], F32, ph)
            r2 = P.sb("b_r2", [128, D], F32, ph)
            ot = [P.sb(f"b_ot{i}", [128, D], F32, ph) for i in range(2)]
            bigA = P.sb("b_bigA", [128, 2048], F32, ph)
            bigB = P.sb("b_bigB", [128, 2048], F32, ph)
            bigC = P.sb("b_bigC", [128, 2048], F32, ph)
            bigD = P.sb("b_bigD", [128, 2048], F32, ph)
            V16 = P.sb("b_V16", [128, 16, 16], F32, ph)
            I16 = P.sb("b_I16", [128, 16, 16], U32, ph)
            I16f = P.sb("b_I16f", [128, 16, 16], F32, ph)
            i1s = P.sb("b_i1s", [128, 8, 16], F32, ph)
            tops = P.sb("b_tops", [128, 8, 16], F32, ph)
            posu = P.sb("b_posu", [128, 8, 16], U32, ph)
            pau = P.sb("b_pau", [128, 8, 16], U32, ph)
            pbu = P.sb("b_pbu", [128, 8, 16], U32, ph)
            paf = P.sb("b_paf", [128, 8, 16], F32, ph)
            pbf = P.sb("b_pbf", [128, 8, 16], F32, ph)
            e1 = P.sb("b_e1", [128, 8, 16], F32, ph)
            e2 = P.sb("b_e2", [128, 8, 16], F32, ph)
            idxf = P.sb("b_idxf", [128, 128], F32, ph)
            idxi = [P.sb(f"b_idxi{i}", [128, 128], I32, ph) for i in range(2)]
            gex = P.sb("b_gex", [128, 8, 16], F32, ph)
            gz = P.sb("b_gz", [128, 8], F32, ph)
            gates = P.sb("b_gates", [128, 8, 16], F32, ph)
            apre = P.sb("b_apre", [128, 128], F32, ph)
            wgt = P.sb("b_wgt", [128, 128], F32, ph)
            junk = P.sb("b_junk", [128, D], BF16, ph)
            NG = 4
            gb = [P.sb(f"b_gb{i}", [128, D], F32, ph) for i in range(NG)]
            gcount = [0]
            acc = P.ps("b_acc", [128, D], F32, ph)
            h1ps = P.ps("b_h1ps", [128, D], F32, ph)

            for c in range(8):
                P.dma('sp', lambda e, c=c: e.dma_start(out=bigA[:], in_=peer_wq[c * 128:(c + 1) * 128, :]), [], [bigA])
                P.op('dve', lambda e, c=c: e.tensor_tensor(out=wql[:, c, :], in0=bigA[:], in1=wq[:, c, :], op=ALU.subtract),
                     [bigA, wq], [wql])
            for s in range(NO):
                rows = slice(s * 128, (s + 1) * 128)
                h1 = h1t[s % 2]; pl = ptl[s % 2]; ob = ot[s % 2]; ixi = idxi[s % 2]
                P.dma('sp', lambda e, h1=h1, rows=rows: e.dma_start(out=h1[:], in_=H1S[rows, :]), [], [h1])
                P.dma('sp', lambda e, pl=pl, rows=rows: e.dma_start(out=pl[:], in_=p_own[rows, :]), [], [pl])
                P.op('act', lambda e, h1=h1: e.activation(out=h1b[:], in_=h1[:], func=AF.Copy), [h1], [h1b])
                transpose_to(h1b, 8, pb[0], h1T)
                if with_peer:
                    P.op('dve', lambda e, h1=h1: e.tensor_tensor(out=h1l[:], in0=h1[:], in1=h1b[:], op=ALU.subtract),
                         [h1, h1b], [h1l])
                    transpose_to(h1l, 8, pb[1], h1Tl)
                P.op('dve', lambda e, pl=pl: e.tensor_copy(out=ptb[:], in_=pl[:]), [pl], [ptb])
                transpose_to(ptb, 2, pb[1], pT)
                if with_peer:
                    P.op('act', lambda e, h1=h1: e.activation(out=h1ps[:], in_=h1[:], func=AF.Copy), [h1], [h1ps])
                    for ch in range(16):
                        bk = pb[ch // 4]; cs = slice((ch % 4) * 128, (ch % 4 + 1) * 128)
                        for pi, (wt, ht) in enumerate(((wq, h1T), (wql, h1T), (wq, h1Tl))):
                            for c in range(8):
                                P.op('pe', lambda e, bk=bk, cs=cs, c=c, ch=ch, wt=wt, ht=ht, pi=pi: e.matmul(
                                    bk[:, cs], lhsT=wt[:, c, ch * 128:(ch + 1) * 128], rhs=ht[:, c, :],
                                    start=(c == 0 and pi == 0), stop=(c == 7 and pi == 2)), [wt, ht], [bk])
                    for q4 in range(4):
                        P.op('act', lambda e, q4=q4: e.activation(out=qhi[:, q4 * 512:(q4 + 1) * 512], in_=pb[q4][:],
                                                                  func=AF.Copy), [pb[q4]], [qhi])
                        P.op('dve', lambda e, q4=q4: e.tensor_tensor(out=qlo[:, q4 * 512:(q4 + 1) * 512], in0=pb[q4][:],
                                                                     in1=qhi[:, q4 * 512:(q4 + 1) * 512], op=ALU.subtract),
                             [pb[q4], qhi], [qlo])
                    for ch in range(16):
                        bk = pb[ch // 4]; cs = slice((ch % 4) * 128, (ch % 4 + 1) * 128)
                        for pi, (qt, kt) in enumerate(((qhi, kTh), (qlo, kTh), (qhi, kTl))):
                            P.op('pe', lambda e, bk=bk, cs=cs, ch=ch, qt=qt, kt=kt, pi=pi: e.matmul(
                                bk[:, cs], lhsT=qt[:, ch * 128:(ch + 1) * 128], rhs=kt[ch % 2][:],
                                start=(pi == 0), stop=(pi == 2)), [qt, kt[ch % 2]], [bk])
                    for q4 in range(4):
                        P.op('act', lambda e, q4=q4: e.activation(out=bigB[:, q4 * 512:(q4 + 1) * 512], in_=pb[q4][:],
                                                                  func=AF.Copy), [pb[q4]], [bigB])
                    for ch in range(16):
                        seg = slice(ch * 128, (ch + 1) * 128)
                        P.op('dve', lambda e, ch=ch, seg=seg: e.max(out=V16[:, ch, 0:8], in_=bigB[:, seg]), [bigB], [V16])
                        P.op('dve', lambda e, ch=ch, seg=seg: e.match_replace(out=bigC[:, seg], in_to_replace=V16[:, ch, 0:8],
                                                                              in_values=bigB[:, seg], imm_value=-1e30),
                             [bigB, V16], [bigC])
                        P.op('dve', lambda e, ch=ch, seg=seg: e.max(out=V16[:, ch, 8:16], in_=bigC[:, seg]), [bigC], [V16])
                        P.op('dve', lambda e, ch=ch, seg=seg: e.max_index(out=I16[:, ch, 0:8], in_max=V16[:, ch, 0:8],
                                                                          in_values=bigB[:, seg]), [bigB, V16], [I16])
                        P.op('dve', lambda e, ch=ch, seg=seg: e.max_index(out=I16[:, ch, 8:16], in_max=V16[:, ch, 8:16],
                                                                          in_values=bigB[:, seg]), [bigB, V16], [I16])
                    P.op('dve', lambda e: e.tensor_copy(out=I16f[:], in_=I16[:]), [I16], [I16f])
                    Vv = V16[:].rearrange("p (h two) k -> p h two k", two=2)
                    Iv = I16f[:].rearrange("p (h two) k -> p h two k", two=2)
                    P.op('dve', lambda e: e.tensor_scalar(out=i1s[:], in0=Iv[:, :, 0, :], scalar1=128.0, scalar2=None,
                                                          op0=ALU.mult), [I16f], [i1s])
                    c4 = lambda t: t[:].rearrange("p (h a b) -> p h a b", h=8, a=16)
                    bc_a = lambda ap: ap.unsqueeze(3).to_broadcast([128, 8, 16, 16])
                    bc_b = lambda ap: ap.unsqueeze(2).to_broadcast([128, 8, 16, 16])
                    P.op('dve', lambda e: e.tensor_tensor(out=c4(bigA), in0=bc_a(Vv[:, :, 0, :]), in1=bc_b(Vv[:, :, 1, :]),
                                                          op=ALU.add), [V16], [bigA])
                    P.op('dve', lambda e: e.tensor_tensor(out=c4(bigD), in0=bc_a(i1s[:]), in1=bc_b(Iv[:, :, 1, :]),
                                                          op=ALU.add), [i1s, I16f], [bigD])
                    for h in range(8):
                        seg = slice(h * 256, (h + 1) * 256)
                        P.op('dve', lambda e, h=h, seg=seg: e.max(out=tops[:, h, 0:8], in_=bigA[:, seg]), [bigA], [tops])
                        P.op('dve', lambda e, h=h, seg=seg: e.match_replace(out=bigC[:, seg], in_to_replace=tops[:, h, 0:8],
                                                                            in_values=bigA[:, seg], imm_value=-1e30),
                             [bigA, tops], [bigC])
                        P.op('dve', lambda e, h=h, seg=seg: e.max(out=tops[:, h, 8:16], in_=bigC[:, seg]), [bigC], [tops])
                        P.op('dve', lambda e, h=h, seg=seg: e.max_index(out=posu[:, h, 0:8], in_max=tops[:, h, 0:8],
                                                                        in_values=bigA[:, seg]), [bigA, tops], [posu])
                        P.op('dve', lambda e, h=h, seg=seg: e.max_index(out=posu[:, h, 8:16], in_max=tops[:, h, 8:16],
                                                                        in_values=bigA[:, seg]), [bigA, tops], [posu])
                    P.op('dve', lambda e: e.tensor_single_scalar(out=pau[:], in_=posu[:], scalar=4,
                                                                 op=ALU.logical_shift_right), [posu], [pau])
                    P.op('dve', lambda e: e.tensor_single_scalar(out=pbu[:], in_=posu[:], scalar=15,
                                                                 op=ALU.bitwise_and), [posu], [pbu])
                    P.op('dve', lambda e: e.tensor_copy(out=paf[:], in_=pau[:]), [pau], [paf])
                    P.op('dve', lambda e: e.tensor_copy(out=pbf[:], in_=pbu[:]), [pbu], [pbf])
                    io4 = iota16[:].unsqueeze(1).unsqueeze(1).to_broadcast([128, 8, 16, 16])
                    for (pf, src_ap, res_t, rd) in ((paf, i1s[:], e1, [i1s]), (pbf, Iv[:, :, 1, :], e2, [I16f])):
                        P.op('dve', lambda e, pf=pf: e.tensor_tensor(out=c4(bigB), in0=bc_a(pf[:]), in1=io4, op=ALU.is_equal),
                             [pf, iota16], [bigB])
                        P.op('dve', lambda e, src_ap=src_ap: e.tensor_tensor(out=c4(bigC), in0=c4(bigB), in1=bc_b(src_ap),
                                                                             op=ALU.mult), [bigB] + rd, [bigC])
                        P.op('dve', lambda e, res_t=res_t: e.tensor_reduce(out=res_t[:], in_=c4(bigC), axis=AX.X, op=ALU.add),
                             [bigC], [res_t])
                    P.op('dve', lambda e: e.tensor_tensor(out=idxf[:].rearrange("p (h k) -> p h k", h=8), in0=e1[:], in1=e2[:],
                                                          op=ALU.add), [e1, e2], [idxf])
                    P.op('dve', lambda e, ixi=ixi: e.tensor_copy(out=ixi[:], in_=idxf[:]), [idxf], [ixi])
                    P.op('dve', lambda e: e.tensor_tensor(out=gex[:], in0=tops[:],
                                                          in1=tops[:, :, 0:1].to_broadcast([128, 8, 16]), op=ALU.subtract),
                         [tops], [gex])
                    P.op('act', lambda e: e.activation(out=gex[:], in_=gex[:], func=AF.Exp), [gex], [gex])
                    P.op('dve', lambda e: e.tensor_reduce(out=gz[:], in_=gex[:], axis=AX.X, op=ALU.add), [gex], [gz])
                    P.op('dve', lambda e: e.reciprocal(out=gz[:], in_=gz[:]), [gz], [gz])
                    P.op('dve', lambda e: e.tensor_tensor(out=gates[:], in0=gex[:],
                                                          in1=gz[:].unsqueeze(2).to_broadcast([128, 8, 16]), op=ALU.mult),
                         [gex, gz], [gates])
                    for hk in range(128):
                        g = gb[gcount[0] % NG]; gcount[0] += 1
                        P.dma('pool', lambda e, g=g, hk=hk, ixi=ixi: e.indirect_dma_start(
                            out=g[:], out_offset=None, in_=peer_u,
                            in_offset=bass.IndirectOffsetOnAxis(ap=ixi[:, hk:hk + 1], axis=0)), [ixi], [g])
                        P.op('dve', lambda e, g=g, hk=hk: e.scalar_tensor_tensor(
                            out=junk[:], in0=g[:], scalar=1.0, in1=h1ps[:], op0=ALU.mult, op1=ALU.mult,
                            accum_out=apre[:, hk:hk + 1]), [g, h1ps], [junk, apre])
                    if debug:
                        P.dma('sp', lambda e, rows=rows: e.dma_start(out=DBG[rows, 0, :], in_=idxf[:]), [idxf], [])
                        P.dma('sp', lambda e, rows=rows: e.dma_start(out=DBG[rows, 1, :], in_=apre[:]), [apre], [])
                        P.dma('sp', lambda e, rows=rows: e.dma_start(out=DBG[rows, 2, :], in_=gates[:].rearrange("p h k -> p (h k)")), [gates], [])
                    P.op('act', lambda e: e.activation(out=apre[:], in_=apre[:], func=AF.Gelu), [apre], [apre])
                    if debug:
                        P.dma('sp', lambda e, rows=rows: e.dma_start(out=DBG[rows, 3, :], in_=apre[:]), [apre], [])
                    P.op('dve', lambda e: e.tensor_tensor(out=wgt[:], in0=apre[:],
                                                          in1=gates[:].rearrange("p h k -> p (h k)"), op=ALU.mult),
                         [apre, gates], [wgt])
                    for hk in range(128):
                        g = gb[gcount[0] % NG]; gcount[0] += 1
                        P.dma('pool', lambda e, g=g, hk=hk, ixi=ixi: e.indirect_dma_start(
                            out=g[:], out_offset=None, in_=peer_v,
                            in_offset=bass.IndirectOffsetOnAxis(ap=ixi[:, hk:hk + 1], axis=0)), [ixi], [g])
                        if hk == 0:
                            P.op('dve', lambda e, g=g: e.tensor_scalar(
                                out=acc[:], in0=g[:], scalar1=wgt[:, 0:1], scalar2=None, op0=ALU.mult),
                                [g, wgt], [acc])
                        else:
                            P.op('dve', lambda e, g=g, hk=hk: e.scalar_tensor_tensor(
                                out=acc[:], in0=g[:], scalar=wgt[:, hk:hk + 1], in1=acc[:],
                                op0=ALU.mult, op1=ALU.add), [g, wgt, acc], [acc])
                for hf in range(2):
                    cs = slice(hf * 512, (hf + 1) * 512)
                    for c in range(8):
                        P.op('pe', lambda e, c=c, cs=cs, hf=hf: e.matmul(pb[hf][:], lhsT=h1T[:, c, :], rhs=wpg[:, c, cs],
                                                                         start=(c == 0), stop=(c == 7)),
                             [h1T, wpg], [pb[hf]])
                    for c in range(2):
                        P.op('pe', lambda e, c=c, cs=cs, hf=hf: e.matmul(pb[2 + hf][:], lhsT=pT[:, c, :], rhs=wple[:, c, cs],
                                                                         start=(c == 0), stop=(c == 1)),
                             [pT, wple], [pb[2 + hf]])
                    P.op('act', lambda e, cs=cs, hf=hf: e.activation(out=xn[:, cs], in_=pb[hf][:], func=AF.Sigmoid),
                         [pb[hf]], [xn])
                    P.op('dve', lambda e, cs=cs, hf=hf: e.tensor_tensor(out=ple[:, cs], in0=xn[:, cs], in1=pb[2 + hf][:],
                                                                        op=ALU.mult), [xn, pb[2 + hf]], [ple])
                P.op('dve', lambda e, h1=h1: e.scalar_tensor_tensor(out=r2[:], in0=h1[:], scalar=ALPHA, in1=ple[:],
                                                                    op0=ALU.mult, op1=ALU.add), [h1, ple], [r2])
                if with_peer:
                    P.op('dve', lambda e: e.tensor_tensor(out=r2[:], in0=r2[:], in1=acc[:], op=ALU.add), [r2, acc], [r2])
                layer_norm(r2, ob, L2, st, mv, sd, rstd, xn)
                P.dma('sp', lambda e, ob=ob, rows=rows: e.dma_start(out=out[rows, :], in_=ob[:]), [ob], [])
        with ExitStack() as ph:
            _phase4(ph)
        P.emit()
    return nc


_NC = {}
W_NAMES = ['w_ple_gate', 'w_ple', 'peer_wq', 'peer_k1', 'peer_k2', 'peer_u', 'peer_v',
           'w_branch_sb', 'w_branch_ml', 'w_out']


def kernel(_nblk=NB, _with_mix=True, _with_peer=True, _debug=False, _stop=None, **inp):
    key = (_nblk, _with_mix, _with_peer, _debug, _stop)
    if key not in _NC:
        _NC[key] = build_nc(_nblk, _with_mix, _with_peer, _debug, _stop)
    nc = _NC[key]
    T = _nblk * 128
    x = np.asarray(inp['x'], dtype=np.float32)
    p = np.asarray(inp['p'], dtype=np.float32)[0]
    shared = {n: np.ascontiguousarray(np.asarray(inp[n], dtype=np.float32)[0]) for n in W_NAMES}
    w_in_h = np.asarray(inp['w_in'], dtype=np.float32)[0]
    for i, (c0, c1) in enumerate(((0, 1024), (1024, 2048), (2048, 3072), (3072, 3592), (3592, 4616), (4616, 5640))):
        shared[f'w_in_p{i}'] = np.ascontiguousarray(w_in_h[:, c0:c1])
    shared['conv_wb'] = np.ascontiguousarray(np.concatenate([np.asarray(inp['conv_w'], dtype=np.float32)[0],
                                                             np.asarray(inp['conv_b'], dtype=np.float32)], axis=0))
    shared['gate_b'] = np.ascontiguousarray(np.concatenate([np.asarray(inp['b_igate'], dtype=np.float32)[0],
                                                            np.asarray(inp['b_fgate'], dtype=np.float32)[0]])[None, :])
    shared['ln0_g'] = np.ascontiguousarray(inp['ln0_g'], dtype=np.float32)
    shared['ln0_b'] = np.ascontiguousarray(inp['ln0_b'], dtype=np.float32)
    for i in (1, 2):
        shared[f'ln{i}_g'] = np.ascontiguousarray(inp[f'ln{i}_g'][0], dtype=np.float32)
        shared[f'ln{i}_b'] = np.ascontiguousarray(inp[f'ln{i}_b'][0], dtype=np.float32)
    in_maps = []
    TO = T // 2
    for core in range(8):
        b, j = core // 2, core % 2
        m = dict(shared)
        m["x_all"] = np.ascontiguousarray(x[b, :T])
        m["x_own"] = np.ascontiguousarray(x[b, j * TO:(j + 1) * TO])
        m["p_own"] = np.ascontiguousarray(p[b, j * TO:(j + 1) * TO])
        sel = np.zeros((128, 2), dtype=np.float32); sel[:, j] = 1.0
        m["sel"] = sel
        in_maps.append(m)
    res = run_bass_kernel_spmd(nc, in_maps, core_ids=list(range(8)))
    outp = np.empty((4, T, D), dtype=np.float32)
    for core in range(8):
        b, j = core // 2, core % 2
        outp[b, j * TO:(j + 1) * TO] = res.results[core]["out"]
    if _debug:
        return outp, [res.results[2 * b] for b in range(4)]
    return outp
```

```python
import numpy as np
from contextlib import ExitStack
import concourse.bass as bass
import concourse.mybir as mybir
from concourse.bass_utils import run_bass_kernel_spmd

F32 = mybir.dt.float32
BF16 = mybir.dt.bfloat16
I32 = mybir.dt.int32
U32 = mybir.dt.uint32
AF = mybir.ActivationFunctionType
ALU = mybir.AluOpType
AX = mybir.AxisListType

D = 1024
SEQ = 8192
NB = 64
NOWN = 32
ALPHA = 2.0 ** 0.25
EPS = 1e-5
ENGS = ['pe', 'act', 'dve', 'pool', 'sp']
import os
SERIALIZE = False
SER_P1, SER_P3, SER_P4A, SER_P4B = False, False, False, False


class Res:
    __slots__ = ('w', 'r')

    def __init__(self):
        self.w = None
        self.r = {}


class Tile:
    def __init__(self, t):
        self.t = t
        self.res = Res()
        self._sub = {}
        self.dkey = None
        self.dcount = 0

    def __getitem__(self, k):
        return self.t[k]

    def sub(self, k):
        if k not in self._sub:
            self._sub[k] = Res()
        return self._sub[k]


class Prog:
    def __init__(self, nc, es):
        self.nc = nc
        self.es = es
        self.ops = {e: [] for e in ENGS}
        self.cnt = {e: 0 for e in ENGS}
        self.dcnt = {e: 0 for e in ENGS}
        self.seen = {e: {} for e in ENGS}
        self.sem = {}
        for e in ENGS:
            self.sem[('c', e)] = es.enter_context(nc.semaphore('c_' + e))
        self.dtiles = []
        self.log = []
        self.serialize = SERIALIZE

    def sb(self, name, shape, dt, es=None):
        return Tile((es or self.es).enter_context(self.nc.sbuf_tensor(name, shape, dt)))

    def ps(self, name, shape, dt, es=None):
        return Tile((es or self.es).enter_context(self.nc.psum_tensor(name, shape, dt)))

    def barrier(self):
        cur = []
        for e in ENGS:
            if self.cnt[e]:
                cur.append((('c', e), self.cnt[e]))
        for t in self.dtiles:
            cur.append((t.dkey, 16 * t.dcount))
        for e in ENGS:
            waits = [(k, v) for k, v in cur if self.seen[e].get(k, 0) < v and not (e == 'pe' and k == ('c', 'pe'))]
            for k, v in waits:
                self.seen[e][k] = v
            self.ops[e].append((waits, None, None, 0))
            self.log.append((e, 'barrier', waits, None))

    def _deps(self, eng, reads, writes):
        waits = {}
        seen = self.seen[eng]

        def add(k, v):
            if seen.get(k, 0) >= v:
                return
            if waits.get(k, 0) < v:
                waits[k] = v

        for r in reads:
            if r.w is not None:
                add(*r.w)
        for w in writes:
            if w.w is not None:
                add(*w.w)
            for k, v in w.r.items():
                add(k, v)
        for k, v in waits.items():
            seen[k] = v
        return list(waits.items())

    def _record(self, ev, reads, writes):
        k, v = ev
        for r in reads:
            if r.r.get(k, 0) < v:
                r.r[k] = v
        for w in writes:
            w.w = ev
            w.r = {}

    def op(self, eng, fn, reads=(), writes=()):
        reads = [x.res if isinstance(x, Tile) else x for x in reads]
        writes = [x.res if isinstance(x, Tile) else x for x in writes]
        waits = self._deps(eng, reads, writes)
        self.cnt[eng] += 1
        ev = (('c', eng), self.cnt[eng])
        if eng == 'pe':
            self.seen[eng][ev[0]] = ev[1]
        self._record(ev, reads, writes)
        self.ops[eng].append((waits, fn, ev[0], 1))
        self.log.append((eng, 'op', waits, ev))
        if self.serialize:
            self.barrier()

    def dma(self, eng, fn, reads=(), writes=()):
        tiles = [x for x in list(writes) + list(reads) if isinstance(x, Tile)]
        owner = tiles[0]
        if owner.dkey is None:
            owner.dkey = ('t', len(self.dtiles))
            self.sem[owner.dkey] = self.es.enter_context(self.nc.semaphore('t%d' % len(self.dtiles)))
            self.dtiles.append(owner)
        reads = [x.res if isinstance(x, Tile) else x for x in reads]
        writes = [x.res if isinstance(x, Tile) else x for x in writes]
        waits = self._deps(eng, reads, writes)
        owner.dcount += 1
        ev = (owner.dkey, 16 * owner.dcount)
        self._record(ev, reads, writes)
        self.ops[eng].append((waits, fn, ev[0], 16))
        self.log.append((eng, 'dma', waits, ev))
        if self.serialize:
            self.barrier()

    def emit(self):
        nc = self.nc
        handles = {'pe': 'tensor', 'act': 'scalar', 'dve': 'vector', 'pool': 'gpsimd', 'sp': 'sync'}
        with nc.Block() as blk:
            for e in ENGS:
                ops = self.ops[e]
                final = None
                if e == 'sp':
                    final = [(t.dkey, 16 * t.dcount) for t in self.dtiles]

                def body(eng, ops=ops, final=final):
                    for waits, fn, key, inc in ops:
                        for k, v in waits:
                            eng.wait_ge(self.sem[k], v)
                        if fn is not None:
                            fn(eng).then_inc(self.sem[key], inc)
                    if final:
                        for k, v in final:
                            eng.wait_ge(self.sem[k], v)

                getattr(blk, handles[e])(body)


def _r(x):
    return [x] if not isinstance(x, (list, tuple)) else list(x)


def build_nc(NBLK=NB, with_mix=True, with_peer=True, debug=False, stop_after=None):
    T = NBLK * 128
    nc = bass.Bass("TRN2", target_bir_lowering=False)
    dr = lambda name, shape, dt=F32, kind="ExternalInput": nc.dram_tensor(name, shape, dt, kind=kind).ap()
    SCR = "ExternalOutput" if debug else "Internal"
    NO = NBLK // 2
    TO = NO * 128
    x_all = dr("x_all", [T, D])
    x_own = dr("x_own", [TO, D])
    p_own = dr("p_own", [TO, 256])
    sel_in = dr("sel", [128, 2])
    ln_g = [dr(f"ln{i}_g", [D]) for i in range(3)]
    ln_b = [dr(f"ln{i}_b", [D]) for i in range(3)]
    WPIECES = ((0, 1024), (1024, 2048), (2048, 3072), (3072, 3592), (3592, 4616), (4616, 5640))
    w_in_p = [dr(f"w_in_p{i}", [D, c1 - c0]) for i, (c0, c1) in enumerate(WPIECES)]
    conv_wb = dr("conv_wb", [5, D])
    gate_b = dr("gate_b", [1, 8])
    w_bsb = dr("w_branch_sb", [512, D]); w_bml = dr("w_branch_ml", [512, D]); w_out = dr("w_out", [D, D])
    QTS = dr("qts", [8, 64, T], BF16, SCR)
    KTS = dr("kts", [8, 64, T], BF16, SCR)
    VS = dr("vs", [T, 512], BF16, SCR)
    OMLS = dr("omls", [T, 512], BF16, SCR)
    OSBT = dr("osbt", [4, 128, T], BF16, SCR)
    UB = dr("peer_u_bf", [16384, D], BF16, "Internal")
    VB = dr("peer_v_bf", [16384, D], BF16, "Internal")
    w_pg = dr("w_ple_gate", [D, D])
    w_ple = dr("w_ple", [256, D])
    peer_wq = dr("peer_wq", [D, 2048])
    peer_k1 = dr("peer_k1", [128, 128])
    peer_k2 = dr("peer_k2", [128, 128])
    peer_u = dr("peer_u", [16384, D])
    peer_v = dr("peer_v", [16384, D])
    out = dr("out", [TO, D], F32, "ExternalOutput")
    H1S = dr("h1s", [TO, D], F32, SCR)
    DBG = dr("dbg", [TO, 4, 128], F32, "ExternalOutput") if debug else None

    with ExitStack() as es:
        P = Prog(nc, es)
        ident = P.sb("ident", [128, 128], BF16)
        identf = P.sb("identf", [128, 128], F32)
        epst = P.sb("epst", [128, 1], F32)
        for idt in (ident, identf):
            P.op('pool', lambda e, idt=idt: e.memset(idt[:], 1.0), [], [idt])
            P.op('pool', lambda e, idt=idt: e.affine_select(out=idt[:], in_=idt[:], pattern=[[-1, 128]],
                                                            compare_op=ALU.is_equal, fill=0.0, base=0,
                                                            channel_multiplier=1), [idt], [idt])
        P.op('pool', lambda e: e.memset(epst[:], EPS), [], [epst])
        onet = P.sb("onet", [128, 1], F32)
        P.op('pool', lambda e: e.memset(onet[:], 1.0), [], [onet])
        def ln_params(i, ph, tag):
            g = P.sb(f"{tag}_lng{i}", [128, D], F32, ph); b = P.sb(f"{tag}_lnb{i}", [128, D], F32, ph)
            P.dma('sp', lambda e: e.dma_start(out=g[:], in_=ln_g[i].partition_broadcast(128)), [], [g])
            P.dma('sp', lambda e: e.dma_start(out=b[:], in_=ln_b[i].partition_broadcast(128)), [], [b])
            return (g, b)
        pb = [P.ps(f"pb{i}", [128, 512], F32) for i in range(4)]

        def layer_norm(src, dst, k, st, mv, sd, rstd, xn):
            P.op('dve', lambda e: e.bn_stats(out=st[:, 0:6], in_=src[:, 0:512]), [src], [st])
            P.op('dve', lambda e: e.bn_stats(out=st[:, 6:12], in_=src[:, 512:1024]), [src, st], [st])
            P.op('dve', lambda e: e.bn_aggr(out=mv[:], in_=st[:]), [st], [mv])
            P.op('act', lambda e: e.activation(out=sd[:], in_=mv[:, 1:2], func=AF.Sqrt, bias=epst[:], scale=1.0),
                 [mv, epst], [sd])
            P.op('dve', lambda e: e.reciprocal(out=rstd[:], in_=sd[:]), [sd], [rstd])
            P.op('dve', lambda e: e.tensor_scalar(out=xn[:], in0=src[:], scalar1=mv[:, 0:1], scalar2=rstd[:],
                                                  op0=ALU.subtract, op1=ALU.mult), [src, mv, rstd], [xn])
            P.op('pool', lambda e: e.tensor_tensor(out=xn[:], in0=xn[:], in1=k[0][:], op=ALU.mult),
                 [xn, k[0]], [xn])
            P.op('pool', lambda e: e.tensor_tensor(out=dst[:], in0=xn[:], in1=k[1][:], op=ALU.add),
                 [xn, k[1]], [dst])

        def load_w(dst, src_ap, nchunk, c0=0, c1=None):
            for c in range(nchunk):
                sa = src_ap[c * 128:(c + 1) * 128, :] if c1 is None else src_ap[c * 128:(c + 1) * 128, c0:c1]
                P.dma('pool', lambda e, c=c, sa=sa: e.dma_start(out=dst[:, c, :], in_=sa, max_dma_last_dim=4096),
                      [], [dst])

        def transpose_to(src_b, nchunk, psb, dstT):
            pv = psb[:].bitcast(BF16)
            for c in range(nchunk):
                P.op('pe', lambda e, c=c: e.transpose(out=pv[:, c * 128:(c + 1) * 128],
                                                      in_=src_b[:, c * 128:(c + 1) * 128], identity=ident[:]),
                     [src_b, ident], [psb])
            P.op('dve', lambda e: e.tensor_copy(out=dstT[:].rearrange("p c t -> p (c t)"),
                                                in_=pv[:, 0:nchunk * 128]), [psb], [dstT])

        if with_mix:
            P.serialize = SER_P1
            def _phase0(ph):
                wA = P.sb("p1_wA", [128, 8, 3592], BF16, ph)
                for pi in range(4):
                    c0, c1 = WPIECES[pi]
                    for c in range(8):
                        P.dma('pool', lambda e, c=c, c0=c0, c1=c1, pi=pi: e.dma_start(
                            out=wA[:, c, c0:c1], in_=w_in_p[pi][c * 128:(c + 1) * 128, :], max_dma_last_dim=4096), [], [wA])
                L0 = ln_params(0, ph, "p1")
                cvt = P.sb("p1_cvt", [128, 2], F32, ph)
                for tab_src, tab_dst in ((peer_u, UB), (peer_v, VB)):
                    for r0 in range(0, 16384, 1024):
                        P.dma('pool', lambda e, tab_src=tab_src, tab_dst=tab_dst, r0=r0: e.dma_start(
                            out=tab_dst[r0:r0 + 1024, :], in_=tab_src[r0:r0 + 1024, :], max_dma_last_dim=4096), [cvt], [])
                cw = P.sb("p1_cw", [128, 8, 5], F32, ph)
                cw5 = P.sb("p1_cw5", [5, D], F32, ph)
                P.dma('sp', lambda e: e.dma_start(out=cw5[:], in_=conv_wb), [], [cw5])
                for c in range(8):
                    P.op('pe', lambda e, c=c: e.transpose(out=pb[0][:, c * 5:(c + 1) * 5], in_=cw5[0:5, c * 128:(c + 1) * 128],
                                                          identity=identf[0:5, 0:5]), [cw5, identf], [pb[0]])
                P.op('dve', lambda e: e.tensor_copy(out=cw[:].rearrange("p c j -> p (c j)"), in_=pb[0][:, 0:40]), [pb[0]], [cw])
                onesF = P.sb("p1_onesF", [128, 128], F32, ph)
                P.op('pool', lambda e: e.memset(onesF[:], 1.0), [], [onesF])
                gb1 = P.sb("p1_gb1", [1, 8], F32, ph)
                P.dma('sp', lambda e: e.dma_start(out=gb1[:], in_=gate_b), [], [gb1])
                gbb = P.sb("p1_gbb", [128, 8], F32, ph)
                P.op('pe', lambda e: e.matmul(pb[1][:, 0:8], lhsT=onesF[0:1, :], rhs=gb1[:], start=True, stop=True), [onesF, gb1], [pb[1]])
                P.op('dve', lambda e: e.tensor_copy(out=gbb[:], in_=pb[1][:, 0:8]), [pb[1]], [gbb])
                triLE = P.sb("p1_triLE", [128, 128], F32, ph)
                P.op('pool', lambda e: e.memset(triLE[:], 1.0), [], [triLE])
                P.op('pool', lambda e: e.affine_select(out=triLE[:], in_=triLE[:], pattern=[[1, 128]], compare_op=ALU.is_ge,
                                                       fill=0.0, base=0, channel_multiplier=-1), [triLE], [triLE])
                xt = [P.sb(f"p1_xt{i}", [128, D], F32, ph) for i in range(2)]
                st = P.sb("p1_st", [128, 12], F32, ph); mv = P.sb("p1_mv", [128, 2], F32, ph)
                sd = P.sb("p1_sd", [128, 1], F32, ph); rstd = P.sb("p1_rstd", [128, 1], F32, ph)
                xn = P.sb("p1_xn", [128, D], F32, ph)
                hn = [P.sb(f"p1_hn{i}", [128, D], F32, ph) for i in range(2)]
                hnb = P.sb("p1_hnb", [128, D], BF16, ph)
                hnT = P.sb("p1_hnT", [128, 8, 128], BF16, ph)
                qTb = [P.sb(f"p1_qTb{i}", [64, 8, 128], BF16, ph) for i in range(2)]
                kTb = [P.sb(f"p1_kTb{i}", [64, 8, 128], BF16, ph) for i in range(2)]
                xraw = P.sb("p1_xraw", [128, 8, 131], F32, ph)
                cacc = P.sb("p1_cacc", [128, 8, 128], F32, ph)
                ctmp = P.sb("p1_ctmp", [128, 8, 128], F32, ph)
                mqk = P.sb("p1_mqk", [128, 8, 128], BF16, ph)
                vb = [P.sb(f"p1_vb{i}", [128, 512], BF16, ph) for i in range(2)]
                mva = P.sb("p1_mva", [128, 4, 129], BF16, ph)
                sgo = P.sb("p1_sgo", [128, 512], F32, ph)
                ifr = P.sb("p1_ifr", [128, 8], F32, ph)
                li = P.sb("p1_li", [128, 4], F32, ph); fz = P.sb("p1_fz", [128, 4], F32, ph)
                l1 = P.sb("p1_l1", [128, 4], F32, ph); gtmp = P.sb("p1_gtmp", [128, 4], F32, ph)
                gs = P.sb("p1_gs", [128, 4], F32, ph); eq = P.sb("p1_eq", [128, 4], F32, ph); eb = P.sb("p1_eb", [128, 4], F32, ph)
                STt = P.sb("p1_ST", [128, 128], BF16, ph)
                ktil = P.sb("p1_ktil", [128, 128], BF16, ph)
                C32 = [P.sb(f"p1_C32_{h}", [128, 129], F32, ph) for h in range(4)]
                C16 = [P.sb(f"p1_C16_{h}", [128, 129], BF16, ph) for h in range(4)]
                tmpC = P.sb("p1_tmpC", [128, 129], F32, ph)
                dn = P.sb("p1_dn", [128, 1], F32, ph); scl = P.sb("p1_scl", [128, 1], F32, ph)
                oml = [P.sb(f"p1_oml{i}", [128, 512], BF16, ph) for i in range(2)]
                P.op('pool', lambda e: e.memset(xraw[:], 0.0), [], [xraw])
                P.op('pool', lambda e: e.memset(mva[:], 1.0), [], [mva])
                for h in range(4):
                    P.op('pool', lambda e, h=h: e.memset(C32[h][:], 0.0), [], [C32[h]])
                    P.op('pool', lambda e, h=h: e.memset(C16[h][:], 0.0), [], [C16[h]])
                LNSC = float(np.log(128.0 ** -0.5))
                lnsc = P.sb("p1_lnsc", [128, 1], F32, ph); onec = P.sb("p1_onec", [128, 1], F32, ph)
                P.op('pool', lambda e: e.memset(lnsc[:], LNSC), [], [lnsc])
                P.op('pool', lambda e: e.memset(onec[:], 1.0), [], [onec])

                for s in range(NBLK):
                    rows = slice(s * 128, (s + 1) * 128)
                    tcol = slice(s * 128, (s + 1) * 128)
                    xb = xt[s % 2]; hb = hn[s % 2]; qb = qTb[s % 2]; kb_ = kTb[s % 2]; vbb = vb[s % 2]; omb = oml[s % 2]
                    P.dma('sp', lambda e, xb=xb, rows=rows: e.dma_start(out=xb[:], in_=x_all[rows, :]), [], [xb])
                    layer_norm(xb, hb, L0, st, mv, sd, rstd, xn)
                    P.op('act', lambda e, hb=hb: e.activation(out=hnb[:], in_=hb[:], func=AF.Copy), [hb], [hnb])
                    transpose_to(hnb, 8, pb[0], hnT)
                    for which, col0, dstb, scale, banks in ((0, 0, qb, 1.0, (0, 1)), (1, 512, kb_, 0.125, (2, 3))):
                        for h in range(8):
                            bk = pb[banks[h // 4]]; cs = slice((h % 4) * 128, (h % 4 + 1) * 128)
                            for c in range(8):
                                P.op('pe', lambda e, bk=bk, cs=cs, c=c, h=h, col0=col0: e.matmul(
                                    bk[0:64, cs], lhsT=wA[:, c, col0 + h * 64:col0 + (h + 1) * 64], rhs=hnT[:, c, :],
                                    start=(c == 0), stop=(c == 7)), [wA, hnT], [bk])
                        for hh in range(2):
                            P.op('act', lambda e, hh=hh, dstb=dstb, scale=scale, banks=banks: e.activation(
                                out=dstb[:, hh * 4:(hh + 1) * 4, :].rearrange("p h t -> p (h t)"), in_=pb[banks[hh]][0:64, :],
                                func=AF.Copy, scale=scale), [pb[banks[hh]]], [dstb])
                    P.dma('sp', lambda e, qb=qb, tcol=tcol: e.dma_start(out=QTS[:, :, tcol].rearrange("h d t -> d h t"), in_=qb[:]), [qb], [])
                    P.dma('sp', lambda e, kb_=kb_, tcol=tcol: e.dma_start(out=KTS[:, :, tcol].rearrange("h d t -> d h t"), in_=kb_[:]), [kb_], [])
                    for cc in range(8):
                        bk = pb[cc // 4]; cs = slice((cc % 4) * 128, (cc % 4 + 1) * 128)
                        for c in range(8):
                            P.op('pe', lambda e, bk=bk, cs=cs, c=c, cc=cc: e.matmul(
                                bk[:, cs], lhsT=wA[:, c, 1536 + cc * 128:1536 + (cc + 1) * 128], rhs=hnT[:, c, :],
                                start=(c == 0), stop=(c == 7)), [wA, hnT], [bk])
                    for hh in range(2):
                        P.op('act', lambda e, hh=hh: e.activation(out=xraw[:, hh * 4:(hh + 1) * 4, 3:131],
                                                                  in_=pb[hh][:].rearrange("p (c t) -> p c t", c=4),
                                                                  func=AF.Copy), [pb[hh]], [xraw])
                    for col0, bk, n in ((1024, pb[2], 512), (2560, pb[3], 512), (3072, pb[0], 512), (3584, pb[1], 8)):
                        for c in range(8):
                            P.op('pe', lambda e, bk=bk, c=c, col0=col0, n=n: e.matmul(
                                bk[:, 0:n], lhsT=hnT[:, c, :], rhs=wA[:, c, col0:col0 + n],
                                start=(c == 0), stop=(c == 7)), [wA, hnT], [bk])
                    P.op('act', lambda e, vbb=vbb: e.activation(out=vbb[:], in_=pb[2][:], func=AF.Copy), [pb[2]], [vbb])
                    P.dma('sp', lambda e, vbb=vbb, rows=rows: e.dma_start(out=VS[rows, :], in_=vbb[:]), [vbb], [])
                    P.op('act', lambda e: e.activation(out=mva[:, :, 0:128], in_=pb[3][:].rearrange("p (h d) -> p h d", h=4),
                                                       func=AF.Copy), [pb[3]], [mva])
                    P.op('act', lambda e: e.activation(out=sgo[:], in_=pb[0][:], func=AF.Sigmoid), [pb[0]], [sgo])
                    P.op('dve', lambda e: e.tensor_copy(out=ifr[:], in_=pb[1][:, 0:8]), [pb[1]], [ifr])
                    wb = lambda j: cw[:, :, j:j + 1].to_broadcast([128, 8, 128])
                    P.op('dve', lambda e: e.tensor_tensor(out=cacc[:], in0=xraw[:, :, 3:131], in1=wb(0), op=ALU.mult), [xraw, cw], [cacc])
                    P.op('dve', lambda e: e.tensor_tensor(out=cacc[:], in0=cacc[:], in1=cw[:, :, 4:5].to_broadcast([128, 8, 128]),
                                                          op=ALU.add), [cacc, cw], [cacc])
                    for j in range(1, 4):
                        P.op('dve', lambda e, j=j: e.tensor_tensor(out=ctmp[:], in0=xraw[:, :, 3 - j:131 - j], in1=wb(j), op=ALU.mult),
                             [xraw, cw], [ctmp])
                        P.op('dve', lambda e: e.tensor_tensor(out=cacc[:], in0=cacc[:], in1=ctmp[:], op=ALU.add), [cacc, ctmp], [cacc])
                    P.op('act', lambda e: e.activation(out=mqk[:], in_=cacc[:], func=AF.Silu), [cacc], [mqk])
                    P.op('dve', lambda e: e.tensor_copy(out=ctmp[:, :, 0:3], in_=xraw[:, :, 128:131]), [xraw], [ctmp])
                    P.op('dve', lambda e: e.tensor_copy(out=xraw[:, :, 0:3], in_=ctmp[:, :, 0:3]), [ctmp], [xraw])
                    P.op('dve', lambda e: e.tensor_tensor(out=li[:], in0=ifr[:, 0:4], in1=gbb[:, 0:4], op=ALU.add), [ifr, gbb], [li])
                    P.op('dve', lambda e: e.tensor_tensor(out=fz[:], in0=ifr[:, 4:8], in1=gbb[:, 4:8], op=ALU.add), [ifr, gbb], [fz])
                    P.op('act', lambda e: e.activation(out=fz[:], in_=fz[:], func=AF.Exp, scale=-1.0), [fz], [fz])
                    P.op('act', lambda e: e.activation(out=l1[:], in_=fz[:], func=AF.Ln, bias=onec[:], scale=1.0), [fz, onec], [l1])
                    P.op('pe', lambda e: e.matmul(pb[1][:, 16:20], lhsT=triLE[:], rhs=l1[:], start=True, stop=True), [triLE, l1], [pb[1]])
                    P.op('pe', lambda e: e.matmul(pb[1][:, 32:36], lhsT=onesF[:], rhs=l1[:], start=True, stop=True), [onesF, l1], [pb[1]])
                    P.op('dve', lambda e: e.tensor_tensor(out=gtmp[:], in0=li[:], in1=pb[1][:, 16:20], op=ALU.add), [li, pb[1]], [gtmp])
                    P.op('act', lambda e: e.activation(out=gs[:], in_=gtmp[:], func=AF.Exp, bias=lnsc[:], scale=1.0), [gtmp, lnsc], [gs])
                    P.op('act', lambda e: e.activation(out=eq[:], in_=pb[1][:, 16:20], func=AF.Exp, scale=-1.0), [pb[1]], [eq])
                    P.op('act', lambda e: e.activation(out=eb[:], in_=pb[1][:, 32:36], func=AF.Exp, scale=-1.0), [pb[1]], [eb])
                    for h in range(4):
                        qTh_ = mqk[:, h, :]; kTh_ = mqk[:, 4 + h, :]
                        P.op('pe', lambda e, h=h: e.matmul(pb[2][:, 0:128], lhsT=mqk[:, 4 + h, :], rhs=mqk[:, h, :], start=True, stop=True),
                             [mqk], [pb[2]])
                        P.op('dve', lambda e, h=h: e.scalar_tensor_tensor(out=STt[:], in0=pb[2][:, 0:128], scalar=gs[:, h:h + 1],
                                                                          in1=triLE[:], op0=ALU.mult, op1=ALU.mult),
                             [pb[2], gs, triLE], [STt])
                        pv3 = pb[3][:].bitcast(BF16)
                        P.op('pe', lambda e, h=h, pv3=pv3: e.transpose(out=pv3[:, 0:128], in_=mqk[:, 4 + h, :], identity=ident[:]),
                             [mqk, ident], [pb[3]])
                        P.op('act', lambda e, h=h, pv3=pv3: e.activation(out=ktil[:], in_=pv3[:, 0:128], func=AF.Copy, scale=gs[:, h:h + 1]),
                             [pb[3], gs], [ktil])
                        P.op('pe', lambda e, h=h: e.matmul(pb[0][:, 0:129], lhsT=STt[:], rhs=mva[:, h, :], start=True, stop=False),
                             [STt, mva], [pb[0]])
                        P.op('pe', lambda e, h=h: e.matmul(pb[0][:, 0:129], lhsT=mqk[:, h, :], rhs=C16[h][:], start=False, stop=True),
                             [mqk, C16[h]], [pb[0]])
                        P.op('pe', lambda e, h=h: e.matmul(pb[2][:, 256:385], lhsT=ktil[:], rhs=mva[:, h, :], start=True, stop=True),
                             [ktil, mva], [pb[2]])
                        P.op('act', lambda e, h=h: e.activation(out=tmpC[:], in_=C32[h][:], func=AF.Copy, scale=eb[:, h:h + 1]),
                             [C32[h], eb], [tmpC])
                        P.op('dve', lambda e, h=h: e.scalar_tensor_tensor(out=C32[h][:], in0=pb[2][:, 256:385], scalar=eb[:, h:h + 1],
                                                                          in1=tmpC[:], op0=ALU.mult, op1=ALU.add),
                             [pb[2], eb, tmpC], [C32[h]])
                        P.op('act', lambda e, h=h: e.activation(out=C16[h][:], in_=C32[h][:], func=AF.Copy), [C32[h]], [C16[h]])
                        P.op('act', lambda e, h=h: e.activation(out=dn[:], in_=pb[0][:, 128:129], func=AF.Abs, scale=eq[:, h:h + 1]),
                             [pb[0], eq], [dn])
                        P.op('dve', lambda e: e.tensor_single_scalar(out=dn[:], in_=dn[:], scalar=1.0, op=ALU.max), [dn], [dn])
                        P.op('dve', lambda e: e.reciprocal(out=dn[:], in_=dn[:]), [dn], [dn])
                        P.op('dve', lambda e, h=h: e.tensor_tensor(out=scl[:], in0=dn[:], in1=eq[:, h:h + 1], op=ALU.mult), [dn, eq], [scl])
                        P.op('dve', lambda e, h=h, omb=omb: e.scalar_tensor_tensor(
                            out=omb[:, h * 128:(h + 1) * 128], in0=pb[0][:, 0:128], scalar=scl[:], in1=sgo[:, h * 128:(h + 1) * 128],
                            op0=ALU.mult, op1=ALU.mult), [pb[0], scl, sgo], [omb])
                    P.dma('sp', lambda e, omb=omb, rows=rows: e.dma_start(out=OMLS[rows, :], in_=omb[:]), [omb], [])
                P.barrier()
            with ExitStack() as ph:
                _phase0(ph)
            if stop_after == 'p1':
                P.emit()
                return nc

            P.serialize = SER_P3
            def _phase1(ph):
                negtri = P.sb("p3_negtri", [128, 128], BF16, ph)
                triGE = P.sb("p3_triGE", [128, 128], BF16, ph)
                onesb = P.sb("p3_onesb", [128, 128], BF16, ph)
                zerob = P.sb("p3_zerob", [128, 512], BF16, ph)
                P.op('pool', lambda e: e.memset(onesb[:], 1.0), [], [onesb])
                P.op('pool', lambda e: e.memset(zerob[:], 0.0), [], [zerob])
                for tt, val in ((negtri, -30000.0), (triGE, 1.0)):
                    P.op('pool', lambda e, tt=tt, val=val: e.memset(tt[:], val), [], [tt])
                    P.op('pool', lambda e, tt=tt: e.affine_select(out=tt[:], in_=tt[:], pattern=[[-1, 128]], compare_op=ALU.is_ge,
                                                                  fill=0.0, base=0, channel_multiplier=1), [tt], [tt])
                kTh_t = [P.sb(f"p3_kT{i}", [64, T], BF16, ph) for i in range(2)]
                qTh_t = [P.sb(f"p3_qT{i}", [64, T], BF16, ph) for i in range(2)]
                vhp = P.sb("p3_vhp", [128, NBLK, 128], BF16, ph)
                e32 = [P.sb(f"p3_e32_{i}", [128, 512], F32, ph) for i in range(2)]
                sp16 = [P.sb(f"p3_sp16_{i}", [128, 512], BF16, ph) for i in range(2)]
                g32 = [P.sb(f"p3_g32_{i}", [128, 512], F32, ph) for i in range(2)]
                A16 = [P.sb(f"p3_A16_{i}", [128, 512], BF16, ph) for i in range(2)]
                R32 = P.sb("p3_R32", [128, 512], F32, ph)
                R16 = [P.sb(f"p3_R16_{i}", [128, 512], BF16, ph) for i in range(2)]
                obuf = [P.sb(f"p3_obuf{i}", [128, 512], BF16, ph) for i in range(2)]
                pz = [pb[0], pb[1]]; pc = [pb[2], pb[3]]
                po = [P.ps(f"p3_po{i}", [128, 512], F32, ph) for i in range(2)]
                stepn = 0; gcnt = 0
                for h in range(8):
                    hp, hq = h // 2, h % 2
                    kt = kTh_t[h % 2]; qt = qTh_t[h % 2]
                    P.dma('sp', lambda e, kt=kt, h=h: e.dma_start(out=kt[:], in_=KTS[h]), [], [kt])
                    P.dma('sp', lambda e, qt=qt, h=h: e.dma_start(out=qt[:], in_=QTS[h]), [], [qt])
                    if hq == 0:
                        P.dma('sp', lambda e, hp=hp: e.dma_start(
                            out=vhp[:], in_=VS[:, hp * 128:(hp + 1) * 128].rearrange("(kb s) c -> s kb c", s=128)), [], [vhp])
                    for G in range(NBLK // 4):
                        pob = po[gcnt % 2]; ob_ = obuf[gcnt % 2]; gcnt += 1
                        P.op('pool', lambda e: e.memset(R32[:], 0.0), [], [R32])
                        P.op('pe', lambda e, pob=pob: e.matmul(pob[:], lhsT=onesb[:], rhs=zerob[:], start=True, stop=False),
                             [onesb, zerob], [pob])
                        kbs = list(range(4 * G + 3, -1, -1))
                        nst = len(kbs)

                        def geom(i):
                            kb = kbs[i]
                            off = max(0, kb - 4 * G) * 128
                            return kb, off, slice(off, 512)

                        def front(i):
                            kb, off, rng = geom(i)
                            z = pz[i % 2]; ee = e32[i % 2]; ss = sp16[i % 2]
                            diag = kb >= 4 * G
                            P.op('pe', lambda e, z=z, rng=rng, kb=kb, off=off, diag=diag, kt=kt, qt=qt, G=G: e.matmul(
                                z[:, rng], lhsT=kt[:, kb * 128:(kb + 1) * 128], rhs=qt[:, G * 512 + off:(G + 1) * 512],
                                start=True, stop=(not diag)), [kt, qt], [z])
                            if diag:
                                P.op('pe', lambda e, z=z, off=off: e.matmul(z[:, off:off + 128], lhsT=ident[:], rhs=negtri[:],
                                                                            start=False, stop=True), [ident, negtri], [z])
                            P.op('act', lambda e, z=z, ee=ee, rng=rng: e.activation(out=ee[:, rng], in_=z[:, rng], func=AF.Exp), [z], [ee])
                            P.op('act', lambda e, ss=ss, ee=ee, rng=rng: e.activation(out=ss[:, rng], in_=ee[:, rng], func=AF.Ln,
                                                                                      bias=onet[:], scale=1.0), [ee, onet], [ss])

                        def cmm(i):
                            kb, off, rng = geom(i)
                            cps = pc[i % 2]; ss = sp16[i % 2]; Rr = R16[i % 2]
                            P.op('pe', lambda e, cps=cps, ss=ss, rng=rng, i=i: e.matmul(
                                cps[:, rng], lhsT=triGE[:], rhs=ss[:, rng], start=True, stop=(i == 0)), [triGE, ss], [cps])
                            if i > 0:
                                P.op('pe', lambda e, cps=cps, Rr=Rr, rng=rng: e.matmul(
                                    cps[:, rng], lhsT=onesb[:], rhs=Rr[:, rng], start=False, stop=True), [onesb, Rr], [cps])

                        def av(i):
                            kb, off, rng = geom(i)
                            aa = A16[i % 2]
                            P.op('pe', lambda e, aa=aa, rng=rng, kb=kb, i=i, pob=pob, nst=nst: e.matmul(
                                pob[:, rng], lhsT=vhp[:, kb, :], rhs=aa[:, rng], start=False, stop=(i == nst - 1)), [vhp, aa], [pob])

                        def back(i):
                            kb, off, rng = geom(i)
                            cps = pc[i % 2]; ee = e32[i % 2]; ss = sp16[i % 2]; gg = g32[i % 2]; aa = A16[i % 2]
                            Rw = R16[(i + 1) % 2]
                            P.op('act', lambda e, cps=cps, gg=gg, rng=rng: e.activation(out=gg[:, rng], in_=cps[:, rng], func=AF.Exp,
                                                                                        scale=-1.0), [cps], [gg])
                            P.op('dve', lambda e, aa=aa, ee=ee, gg=gg, rng=rng: e.tensor_tensor(out=aa[:, rng], in0=ee[:, rng],
                                                                                               in1=gg[:, rng], op=ALU.mult),
                                 [ee, gg], [aa])
                            if i < nst - 1:
                                P.op('pool', lambda e, ss=ss, rng=rng: e.tensor_tensor(out=R32[:, rng], in0=R32[:, rng], in1=ss[:, rng],
                                                                                      op=ALU.add), [R32, ss], [R32])
                                P.op('pool', lambda e, Rw=Rw: e.tensor_copy(out=Rw[:], in_=R32[:]), [R32], [Rw])

                        front(0)
                        for i in range(nst):
                            if i + 1 < nst:
                                front(i + 1)
                            cmm(i)
                            if i >= 1:
                                av(i - 1)
                            back(i)
                        av(nst - 1)
                        prow = slice(64 * hq, 64 * hq + 64)
                        P.op('act', lambda e, pob=pob, ob_=ob_, prow=prow: e.activation(out=ob_[prow, :], in_=pob[prow, :], func=AF.Copy),
                             [pob], [ob_])
                        P.dma('sp', lambda e, ob_=ob_, prow=prow, hp=hp, G=G: e.dma_start(
                            out=OSBT[hp, prow, G * 512:(G + 1) * 512], in_=ob_[prow, :]), [ob_], [])
                P.barrier()
            with ExitStack() as ph:
                _phase1(ph)
            if stop_after == 'p3':
                P.emit()
                return nc

            P.serialize = SER_P4A
            def _phase2(ph):
                wG = P.sb("p4_wG", [128, 8, 2048], BF16, ph)
                for pi in (4, 5):
                    c0, c1 = WPIECES[pi]
                    for c in range(8):
                        P.dma('pool', lambda e, c=c, c0=c0, c1=c1, pi=pi: e.dma_start(
                            out=wG[:, c, c0 - 3592:c1 - 3592], in_=w_in_p[pi][c * 128:(c + 1) * 128, :], max_dma_last_dim=4096), [], [wG])
                wsb = P.sb("p4_wsb", [128, 4, D], BF16, ph); wml = P.sb("p4_wml", [128, 4, D], BF16, ph)
                wo = P.sb("p4_wo", [128, 8, D], BF16, ph)
                load_w(wsb, w_bsb, 4); load_w(wml, w_bml, 4); load_w(wo, w_out, 8)
                L1 = ln_params(1, ph, "p4")
                st = P.sb("p4_st", [128, 12], F32, ph); mv = P.sb("p4_mv", [128, 2], F32, ph)
                sd = P.sb("p4_sd", [128, 1], F32, ph); rstd = P.sb("p4_rstd", [128, 1], F32, ph)
                xn = P.sb("p4_xn", [128, D], F32, ph)
                hn = [P.sb(f"p4_hn{i}", [128, D], F32, ph) for i in range(2)]
                hnb = P.sb("p4_hnb", [128, D], BF16, ph)
                hnT = P.sb("p4_hnT", [128, 8, 128], BF16, ph)
                gT = P.sb("p4_gT", [128, 16, 128], BF16, ph)
                osbT = [P.sb(f"p4_osbT{i}", [128, 4, 128], BF16, ph) for i in range(2)]
                omlb = [P.sb(f"p4_oml{i}", [128, 512], BF16, ph) for i in range(2)]
                omlT = P.sb("p4_omlT", [128, 4, 128], BF16, ph)
                t1 = P.sb("p4_t1", [128, D], F32, ph); t2 = P.sb("p4_t2", [128, D], F32, ph)
                yT = P.sb("p4_yT", [128, 8, 128], BF16, ph)
                r1 = P.sb("p4_r1", [128, D], F32, ph)
                h1o = [P.sb(f"p4_h1{i}", [128, D], F32, ph) for i in range(2)]
                L0 = ln_params(0, ph, "p4")
                xt = [P.sb(f"p4_xt{i}", [128, D], F32, ph) for i in range(2)]
                osbB = [P.sb(f"p4_osbB{i}", [128, 4, 128], BF16, ph) for i in range(2)]
                omlB = [P.sb(f"p4_omlB{i}", [128, 512], BF16, ph) for i in range(2)]
                selt = P.sb("p4_sel", [128, 2], F32, ph)
                P.dma('sp', lambda e: e.dma_start(out=selt[:], in_=sel_in), [], [selt])
                for s in range(NO):
                    rows = slice(s * 128, (s + 1) * 128)
                    rowsA = rows; rowsB = slice((NO + s) * 128, (NO + s + 1) * 128)
                    hb = hn[s % 2]; ob_ = osbT[s % 2]; omb = omlb[s % 2]; h1b_ = h1o[s % 2]
                    xb = xt[s % 2]; obB = osbB[s % 2]; omB = omlB[s % 2]
                    P.dma('sp', lambda e, xb=xb, rows=rows: e.dma_start(out=xb[:], in_=x_own[rows, :]), [], [xb])
                    layer_norm(xb, hb, L0, st, mv, sd, rstd, xn)
                    P.dma('sp', lambda e, ob_=ob_, rowsA=rowsA: e.dma_start(out=ob_[:], in_=OSBT[:, :, rowsA].rearrange("hp p t -> p hp t")),
                          [], [ob_])
                    P.dma('sp', lambda e, obB=obB, rowsB=rowsB: e.dma_start(out=obB[:], in_=OSBT[:, :, rowsB].rearrange("hp p t -> p hp t")),
                          [], [obB])
                    P.dma('sp', lambda e, omb=omb, rowsA=rowsA: e.dma_start(out=omb[:], in_=OMLS[rowsA, :]), [], [omb])
                    P.dma('sp', lambda e, omB=omB, rowsB=rowsB: e.dma_start(out=omB[:], in_=OMLS[rowsB, :]), [], [omB])
                    for (ta, tb_, pat) in ((ob_, obB, "p c t -> p (c t)"), (omb, omB, None)):
                        va = ta[:].rearrange(pat) if pat else ta[:]
                        vb_ = tb_[:].rearrange(pat) if pat else tb_[:]
                        P.op('dve', lambda e, va=va: e.tensor_scalar(out=va, in0=va, scalar1=selt[:, 0:1], scalar2=None, op0=ALU.mult),
                             [ta, selt], [ta])
                        P.op('dve', lambda e, va=va, vb_=vb_: e.scalar_tensor_tensor(out=va, in0=vb_, scalar=selt[:, 1:2], in1=va,
                                                                                  op0=ALU.mult, op1=ALU.add), [tb_, selt, ta], [ta])
                    P.op('act', lambda e, hb=hb: e.activation(out=hnb[:], in_=hb[:], func=AF.Copy), [hb], [hnb])
                    transpose_to(hnb, 8, pb[0], hnT)
                    transpose_to(omb, 4, pb[1], omlT)
                    for ch in range(16):
                        bk = pb[ch // 4]; cs = slice((ch % 4) * 128, (ch % 4 + 1) * 128)
                        for c in range(8):
                            P.op('pe', lambda e, bk=bk, cs=cs, c=c, ch=ch: e.matmul(
                                bk[:, cs], lhsT=wG[:, c, ch * 128:(ch + 1) * 128], rhs=hnT[:, c, :],
                                start=(c == 0), stop=(c == 7)), [wG, hnT], [bk])
                    for q4 in range(4):
                        P.op('act', lambda e, q4=q4: e.activation(out=gT[:, q4 * 4:(q4 + 1) * 4, :].rearrange("p c t -> p (c t)"),
                                                                  in_=pb[q4][:], func=AF.Sigmoid), [pb[q4]], [gT])
                    for dc in range(8):
                        cs = slice((dc % 4) * 128, (dc % 4 + 1) * 128)
                        for hp in range(4):
                            P.op('pe', lambda e, dc=dc, cs=cs, hp=hp, ob_=ob_: e.matmul(
                                pb[dc // 4][:, cs], lhsT=wsb[:, hp, dc * 128:(dc + 1) * 128], rhs=ob_[:, hp, :],
                                start=(hp == 0), stop=(hp == 3)), [wsb, ob_], [pb[dc // 4]])
                        for fc in range(4):
                            P.op('pe', lambda e, dc=dc, cs=cs, fc=fc: e.matmul(
                                pb[2 + dc // 4][:, cs], lhsT=wml[:, fc, dc * 128:(dc + 1) * 128], rhs=omlT[:, fc, :],
                                start=(fc == 0), stop=(fc == 3)), [wml, omlT], [pb[2 + dc // 4]])
                    for hf in range(2):
                        cs = slice(hf * 512, (hf + 1) * 512)
                        gsb_ = gT[:, hf * 4:(hf + 1) * 4, :].rearrange("p c t -> p (c t)")
                        gml_ = gT[:, 8 + hf * 4:8 + (hf + 1) * 4, :].rearrange("p c t -> p (c t)")
                        P.op('dve', lambda e, cs=cs, hf=hf, gsb_=gsb_: e.tensor_tensor(out=t1[:, cs], in0=gsb_, in1=pb[hf][:], op=ALU.mult),
                             [gT, pb[hf]], [t1])
                        P.op('dve', lambda e, cs=cs, hf=hf, gml_=gml_: e.tensor_tensor(out=t2[:, cs], in0=gml_, in1=pb[2 + hf][:], op=ALU.mult),
                             [gT, pb[2 + hf]], [t2])
                    P.op('dve', lambda e: e.tensor_tensor(out=yT[:].rearrange("p c t -> p (c t)"), in0=t1[:], in1=t2[:], op=ALU.add),
                         [t1, t2], [yT])
                    for hf in range(2):
                        cs = slice(hf * 512, (hf + 1) * 512)
                        for dc in range(8):
                            P.op('pe', lambda e, hf=hf, cs=cs, dc=dc: e.matmul(pb[hf][:], lhsT=yT[:, dc, :], rhs=wo[:, dc, cs],
                                                                             start=(dc == 0), stop=(dc == 7)), [yT, wo], [pb[hf]])
                        P.op('dve', lambda e, hf=hf, cs=cs, hb=hb: e.scalar_tensor_tensor(out=r1[:, cs], in0=hb[:, cs], scalar=ALPHA,
                                                                                        in1=pb[hf][:], op0=ALU.mult, op1=ALU.add),
                             [hb, pb[hf]], [r1])
                    layer_norm(r1, h1b_, L1, st, mv, sd, rstd, xn)
                    P.dma('sp', lambda e, h1b_=h1b_, rows=rows: e.dma_start(out=H1S[rows, :], in_=h1b_[:]), [h1b_], [])
                P.barrier()
            with ExitStack() as ph:
                _phase2(ph)
            if stop_after == 'p4a':
                P.emit()
                nc._plog = P.log
                return nc

        if not with_mix:
            def _phase3(ph):
                xt = [P.sb(f"a_xt{i}", [128, D], F32, ph) for i in range(2)]
                st = P.sb("a_st", [128, 12], F32, ph); mv = P.sb("a_mv", [128, 2], F32, ph)
                sd = P.sb("a_sd", [128, 1], F32, ph); rstd = P.sb("a_rstd", [128, 1], F32, ph)
                xn = P.sb("a_xn", [128, D], F32, ph); hn = P.sb("a_hn", [128, D], F32, ph)
                r1 = P.sb("a_r1", [128, D], F32, ph)
                h1o = [P.sb(f"a_h1{i}", [128, D], F32, ph) for i in range(2)]
                L0 = ln_params(0, ph, "a"); L1 = ln_params(1, ph, "a")
                for s in range(NO):
                    rows = slice(s * 128, (s + 1) * 128)
                    xb = xt[s % 2]; hb = h1o[s % 2]
                    P.dma('sp', lambda e, xb=xb, rows=rows: e.dma_start(out=xb[:], in_=x_own[rows, :]), [], [xb])
                    layer_norm(xb, hn, L0, st, mv, sd, rstd, xn)
                    P.op('act', lambda e: e.activation(out=r1[:], in_=hn[:], func=AF.Copy, scale=ALPHA), [hn], [r1])
                    layer_norm(r1, hb, L1, st, mv, sd, rstd, xn)
                    P.dma('sp', lambda e, hb=hb, rows=rows: e.dma_start(out=H1S[rows, :], in_=hb[:]), [hb], [])
                P.barrier()

            with ExitStack() as ph:
                _phase3(ph)
        P.serialize = SER_P4B
        def _phase4(ph):
            wpg = P.sb("b_wpg", [128, 8, D], BF16, ph)
            wple = P.sb("b_wple", [128, 2, D], BF16, ph)
            wq = P.sb("b_wq", [128, 8, 2048], BF16, ph)
            wql = P.sb("b_wql", [128, 8, 2048], BF16, ph)
            L2 = ln_params(2, ph, "b")
            kT = [P.sb(f"b_kT{i}", [128, 128], F32, ph) for i in range(2)]
            ktmp = P.sb("b_ktmp", [128, 128], F32, ph)
            kTh = [P.sb(f"b_kTh{i}", [128, 128], BF16, ph) for i in range(2)]
            kTl = [P.sb(f"b_kTl{i}", [128, 128], BF16, ph) for i in range(2)]
            iota16 = P.sb("b_iota16", [128, 16], F32, ph)
            load_w(wpg, w_pg, 8)
            load_w(wple, w_ple, 2)
            load_w(wq, peer_wq, 8)
            P.op('pool', lambda e: e.iota(iota16[:], pattern=[[1, 16]], base=0, channel_multiplier=0,
                                          allow_small_or_imprecise_dtypes=True), [], [iota16])
            for i, kk in enumerate((peer_k1, peer_k2)):
                P.dma('sp', lambda e, kk=kk: e.dma_start(out=ktmp[:], in_=kk), [], [ktmp])
                P.op('pe', lambda e: e.transpose(out=pb[0][:, 0:128], in_=ktmp[:], identity=identf[:]),
                     [ktmp, identf], [pb[0]])
                P.op('dve', lambda e, i=i: e.tensor_copy(out=kT[i][:], in_=pb[0][:, 0:128]), [pb[0]], [kT[i]])
                P.op('dve', lambda e, i=i: e.tensor_copy(out=kTh[i][:], in_=kT[i][:]), [kT[i]], [kTh[i]])
                P.op('dve', lambda e, i=i: e.tensor_tensor(out=kTl[i][:], in0=kT[i][:], in1=kTh[i][:], op=ALU.subtract),
                     [kT[i], kTh[i]], [kTl[i]])

            st = P.sb("b_st", [128, 12], F32, ph); mv = P.sb("b_mv", [128, 2], F32, ph)
            sd = P.sb("b_sd", [128, 1], F32, ph); rstd = P.sb("b_rstd", [128, 1], F32, ph)
            xn = P.sb("b_xn", [128, D], F32, ph)
            h1t = [P.sb(f"b_h1{i}", [128, D], F32, ph) for i in range(2)]
            ptl = [P.sb(f"b_pt{i}", [128, 256], F32, ph) for i in range(2)]
            ptb = P.sb("b_ptb", [128, 256], BF16, ph)
            h1b = P.sb("b_h1b", [128, D], BF16, ph)
            h1T = P.sb("b_h1T", [128, 8, 128], BF16, ph)
            h1l = P.sb("b_h1l", [128, D], BF16, ph)
            h1Tl = P.sb("b_h1Tl", [128, 8, 128], BF16, ph)
            qhi = P.sb("b_qhi", [128, 2048], BF16, ph)
            qlo = P.sb("b_qlo", [128, 2048], BF16, ph)
            pT = P.sb("b_pT", [128, 2, 128], BF16, ph)
            ple = P.sb("b_ple", [128, D], F32, ph)
            r2 = P.sb("b_r2", [128, D], F32, ph)
            ot = [P.sb(f"b_ot{i}", [128, D], F32, ph) for i in range(2)]
            bigA = P.sb("b_bigA", [128, 2048], F32, ph)
            bigB = P.sb("b_bigB", [128, 2048], F32, ph)
            bigC = P.sb("b_bigC", [128, 2048], F32, ph)
            bigD = P.sb("b_bigD", [128, 2048], F32, ph)
            V16 = P.sb("b_V16", [128, 16, 16], F32, ph)
            I16 = P.sb("b_I16", [128, 16, 16], U32, ph)
            I16f = P.sb("b_I16f", [128, 16, 16], F32, ph)
            i1s = P.sb("b_i1s", [128, 8, 16], F32, ph)
            tops = P.sb("b_tops", [128, 8, 16], F32, ph)
            posu = P.sb("b_posu", [128, 8, 16], U32, ph)
            pau = P.sb("b_pau", [128, 8, 16], U32, ph)
            pbu = P.sb("b_pbu", [128, 8, 16], U32, ph)
            paf = P.sb("b_paf", [128, 8, 16], F32, ph)
            pbf = P.sb("b_pbf", [128, 8, 16], F32, ph)
            e1 = P.sb("b_e1", [128, 8, 16], F32, ph)
            e2 = P.sb("b_e2", [128, 8, 16], F32, ph)
            idxf = P.sb("b_idxf", [128, 128], F32, ph)
            idxi = [P.sb(f"b_idxi{i}", [128, 128], I32, ph) for i in range(2)]
            gex = P.sb("b_gex", [128, 8, 16], F32, ph)
            gz = P.sb("b_gz", [128, 8], F32, ph)
            gates = P.sb("b_gates", [128, 8, 16], F32, ph)
            apre = P.sb("b_apre", [128, 128], F32, ph)
            wgt = P.sb("b_wgt", [128, 128], F32, ph)
            junk = P.sb("b_junk", [128, D], BF16, ph)
            NG = 8
            gb = [P.sb(f"b_gb{i}", [128, D], BF16, ph) for i in range(NG)]
            gcount = [0]
            acc = P.ps("b_acc", [128, D], F32, ph)
            h1ps = P.ps("b_h1ps", [128, D], F32, ph)

            for c in range(8):
                P.dma('sp', lambda e, c=c: e.dma_start(out=bigA[:], in_=peer_wq[c * 128:(c + 1) * 128, :]), [], [bigA])
                P.op('dve', lambda e, c=c: e.tensor_tensor(out=wql[:, c, :], in0=bigA[:], in1=wq[:, c, :], op=ALU.subtract),
                     [bigA, wq], [wql])
            for s in range(NO):
                rows = slice(s * 128, (s + 1) * 128)
                h1 = h1t[s % 2]; pl = ptl[s % 2]; ob = ot[s % 2]; ixi = idxi[s % 2]
                P.dma('sp', lambda e, h1=h1, rows=rows: e.dma_start(out=h1[:], in_=H1S[rows, :]), [], [h1])
                P.dma('sp', lambda e, pl=pl, rows=rows: e.dma_start(out=pl[:], in_=p_own[rows, :]), [], [pl])
                P.op('act', lambda e, h1=h1: e.activation(out=h1b[:], in_=h1[:], func=AF.Copy), [h1], [h1b])
                transpose_to(h1b, 8, pb[0], h1T)
                if with_peer:
                    P.op('dve', lambda e, h1=h1: e.tensor_tensor(out=h1l[:], in0=h1[:], in1=h1b[:], op=ALU.subtract),
                         [h1, h1b], [h1l])
                    transpose_to(h1l, 8, pb[1], h1Tl)
                P.op('dve', lambda e, pl=pl: e.tensor_copy(out=ptb[:], in_=pl[:]), [pl], [ptb])
                transpose_to(ptb, 2, pb[1], pT)
                if with_peer:
                    P.op('act', lambda e, h1=h1: e.activation(out=h1ps[:], in_=h1[:], func=AF.Copy), [h1], [h1ps])
                    for ch in range(16):
                        bk = pb[ch // 4]; cs = slice((ch % 4) * 128, (ch % 4 + 1) * 128)
                        for pi, (wt, ht) in enumerate(((wq, h1T), (wql, h1T), (wq, h1Tl))):
                            for c in range(8):
                                P.op('pe', lambda e, bk=bk, cs=cs, c=c, ch=ch, wt=wt, ht=ht, pi=pi: e.matmul(
                                    bk[:, cs], lhsT=wt[:, c, ch * 128:(ch + 1) * 128], rhs=ht[:, c, :],
                                    start=(c == 0 and pi == 0), stop=(c == 7 and pi == 2)), [wt, ht], [bk])
                    for q4 in range(4):
                        P.op('act', lambda e, q4=q4: e.activation(out=qhi[:, q4 * 512:(q4 + 1) * 512], in_=pb[q4][:],
                                                                  func=AF.Copy), [pb[q4]], [qhi])
                        P.op('dve', lambda e, q4=q4: e.tensor_tensor(out=qlo[:, q4 * 512:(q4 + 1) * 512], in0=pb[q4][:],
                                                                     in1=qhi[:, q4 * 512:(q4 + 1) * 512], op=ALU.subtract),
                             [pb[q4], qhi], [qlo])
                    for ch in range(16):
                        bk = pb[ch // 4]; cs = slice((ch % 4) * 128, (ch % 4 + 1) * 128)
                        for pi, (qt, kt) in enumerate(((qhi, kTh), (qlo, kTh), (qhi, kTl))):
                            P.op('pe', lambda e, bk=bk, cs=cs, ch=ch, qt=qt, kt=kt, pi=pi: e.matmul(
                                bk[:, cs], lhsT=qt[:, ch * 128:(ch + 1) * 128], rhs=kt[ch % 2][:],
                                start=(pi == 0), stop=(pi == 2)), [qt, kt[ch % 2]], [bk])
                    for q4 in range(4):
                        P.op('act', lambda e, q4=q4: e.activation(out=bigB[:, q4 * 512:(q4 + 1) * 512], in_=pb[q4][:],
                                                                  func=AF.Copy), [pb[q4]], [bigB])
                    for ch in range(16):
                        seg = slice(ch * 128, (ch + 1) * 128)
                        P.op('dve', lambda e, ch=ch, seg=seg: e.max(out=V16[:, ch, 0:8], in_=bigB[:, seg]), [bigB], [V16])
                        P.op('dve', lambda e, ch=ch, seg=seg: e.match_replace(out=bigC[:, seg], in_to_replace=V16[:, ch, 0:8],
                                                                              in_values=bigB[:, seg], imm_value=-1e30),
                             [bigB, V16], [bigC])
                        P.op('dve', lambda e, ch=ch, seg=seg: e.max(out=V16[:, ch, 8:16], in_=bigC[:, seg]), [bigC], [V16])
                        P.op('dve', lambda e, ch=ch, seg=seg: e.max_index(out=I16[:, ch, 0:8], in_max=V16[:, ch, 0:8],
                                                                          in_values=bigB[:, seg]), [bigB, V16], [I16])
                        P.op('dve', lambda e, ch=ch, seg=seg: e.max_index(out=I16[:, ch, 8:16], in_max=V16[:, ch, 8:16],
                                                                          in_values=bigB[:, seg]), [bigB, V16], [I16])
                    P.op('dve', lambda e: e.tensor_copy(out=I16f[:], in_=I16[:]), [I16], [I16f])
                    Vv = V16[:].rearrange("p (h two) k -> p h two k", two=2)
                    Iv = I16f[:].rearrange("p (h two) k -> p h two k", two=2)
                    P.op('dve', lambda e: e.tensor_scalar(out=i1s[:], in0=Iv[:, :, 0, :], scalar1=128.0, scalar2=None,
                                                          op0=ALU.mult), [I16f], [i1s])
                    c4 = lambda t: t[:].rearrange("p (h a b) -> p h a b", h=8, a=16)
                    bc_a = lambda ap: ap.unsqueeze(3).to_broadcast([128, 8, 16, 16])
                    bc_b = lambda ap: ap.unsqueeze(2).to_broadcast([128, 8, 16, 16])
                    P.op('dve', lambda e: e.tensor_tensor(out=c4(bigA), in0=bc_a(Vv[:, :, 0, :]), in1=bc_b(Vv[:, :, 1, :]),
                                                          op=ALU.add), [V16], [bigA])
                    P.op('dve', lambda e: e.tensor_tensor(out=c4(bigD), in0=bc_a(i1s[:]), in1=bc_b(Iv[:, :, 1, :]),
                                                          op=ALU.add), [i1s, I16f], [bigD])
                    for h in range(8):
                        seg = slice(h * 256, (h + 1) * 256)
                        P.op('dve', lambda e, h=h, seg=seg: e.max(out=tops[:, h, 0:8], in_=bigA[:, seg]), [bigA], [tops])
                        P.op('dve', lambda e, h=h, seg=seg: e.match_replace(out=bigC[:, seg], in_to_replace=tops[:, h, 0:8],
                                                                            in_values=bigA[:, seg], imm_value=-1e30),
                             [bigA, tops], [bigC])
                        P.op('dve', lambda e, h=h, seg=seg: e.max(out=tops[:, h, 8:16], in_=bigC[:, seg]), [bigC], [tops])
                        P.op('dve', lambda e, h=h, seg=seg: e.max_index(out=posu[:, h, 0:8], in_max=tops[:, h, 0:8],
                                                                        in_values=bigA[:, seg]), [bigA, tops], [posu])
                        P.op('dve', lambda e, h=h, seg=seg: e.max_index(out=posu[:, h, 8:16], in_max=tops[:, h, 8:16],
                                                                        in_values=bigA[:, seg]), [bigA, tops], [posu])
                    P.op('dve', lambda e: e.tensor_single_scalar(out=pau[:], in_=posu[:], scalar=4,
                                                                 op=ALU.logical_shift_right), [posu], [pau])
                    P.op('dve', lambda e: e.tensor_single_scalar(out=pbu[:], in_=posu[:], scalar=15,
                                                                 op=ALU.bitwise_and), [posu], [pbu])
                    P.op('dve', lambda e: e.tensor_copy(out=paf[:], in_=pau[:]), [pau], [paf])
                    P.op('dve', lambda e: e.tensor_copy(out=pbf[:], in_=pbu[:]), [pbu], [pbf])
                    io4 = iota16[:].unsqueeze(1).unsqueeze(1).to_broadcast([128, 8, 16, 16])
                    for (pf, src_ap, res_t, rd) in ((paf, i1s[:], e1, [i1s]), (pbf, Iv[:, :, 1, :], e2, [I16f])):
                        P.op('dve', lambda e, pf=pf: e.tensor_tensor(out=c4(bigB), in0=bc_a(pf[:]), in1=io4, op=ALU.is_equal),
                             [pf, iota16], [bigB])
                        P.op('dve', lambda e, src_ap=src_ap: e.tensor_tensor(out=c4(bigC), in0=c4(bigB), in1=bc_b(src_ap),
                                                                             op=ALU.mult), [bigB] + rd, [bigC])
                        P.op('dve', lambda e, res_t=res_t: e.tensor_reduce(out=res_t[:], in_=c4(bigC), axis=AX.X, op=ALU.add),
                             [bigC], [res_t])
                    P.op('dve', lambda e: e.tensor_tensor(out=idxf[:].rearrange("p (h k) -> p h k", h=8), in0=e1[:], in1=e2[:],
                                                          op=ALU.add), [e1, e2], [idxf])
                    P.op('dve', lambda e, ixi=ixi: e.tensor_copy(out=ixi[:], in_=idxf[:]), [idxf], [ixi])
                    P.op('dve', lambda e: e.tensor_tensor(out=gex[:], in0=tops[:],
                                                          in1=tops[:, :, 0:1].to_broadcast([128, 8, 16]), op=ALU.subtract),
                         [tops], [gex])
                    P.op('act', lambda e: e.activation(out=gex[:], in_=gex[:], func=AF.Exp), [gex], [gex])
                    P.op('dve', lambda e: e.tensor_reduce(out=gz[:], in_=gex[:], axis=AX.X, op=ALU.add), [gex], [gz])
                    P.op('dve', lambda e: e.reciprocal(out=gz[:], in_=gz[:]), [gz], [gz])
                    P.op('dve', lambda e: e.tensor_tensor(out=gates[:], in0=gex[:],
                                                          in1=gz[:].unsqueeze(2).to_broadcast([128, 8, 16]), op=ALU.mult),
                         [gex, gz], [gates])
                    for hk in range(128):
                        g = gb[gcount[0] % NG]; gcount[0] += 1
                        P.dma('pool', lambda e, g=g, hk=hk, ixi=ixi: e.indirect_dma_start(
                            out=g[:], out_offset=None, in_=UB,
                            in_offset=bass.IndirectOffsetOnAxis(ap=ixi[:, hk:hk + 1], axis=0)), [ixi], [g])
                        P.op('dve', lambda e, g=g, hk=hk: e.scalar_tensor_tensor(
                            out=junk[:], in0=g[:], scalar=1.0, in1=h1ps[:], op0=ALU.mult, op1=ALU.mult,
                            accum_out=apre[:, hk:hk + 1]), [g, h1ps], [junk, apre])
                    if debug:
                        P.dma('sp', lambda e, rows=rows: e.dma_start(out=DBG[rows, 0, :], in_=idxf[:]), [idxf], [])
                        P.dma('sp', lambda e, rows=rows: e.dma_start(out=DBG[rows, 1, :], in_=apre[:]), [apre], [])
                        P.dma('sp', lambda e, rows=rows: e.dma_start(out=DBG[rows, 2, :], in_=gates[:].rearrange("p h k -> p (h k)")), [gates], [])
                    P.op('act', lambda e: e.activation(out=apre[:], in_=apre[:], func=AF.Gelu), [apre], [apre])
                    if debug:
                        P.dma('sp', lambda e, rows=rows: e.dma_start(out=DBG[rows, 3, :], in_=apre[:]), [apre], [])
                    P.op('dve', lambda e: e.tensor_tensor(out=wgt[:], in0=apre[:],
                                                          in1=gates[:].rearrange("p h k -> p (h k)"), op=ALU.mult),
                         [apre, gates], [wgt])
                    for hk in range(128):
                        g = gb[gcount[0] % NG]; gcount[0] += 1
                        P.dma('pool', lambda e, g=g, hk=hk, ixi=ixi: e.indirect_dma_start(
                            out=g[:], out_offset=None, in_=VB,
                            in_offset=bass.IndirectOffsetOnAxis(ap=ixi[:, hk:hk + 1], axis=0)), [ixi], [g])
                        if hk == 0:
                            P.op('dve', lambda e, g=g: e.tensor_scalar(
                                out=acc[:], in0=g[:], scalar1=wgt[:, 0:1], scalar2=None, op0=ALU.mult),
                                [g, wgt], [acc])
                        else:
                            P.op('dve', lambda e, g=g, hk=hk: e.scalar_tensor_tensor(
                                out=acc[:], in0=g[:], scalar=wgt[:, hk:hk + 1], in1=acc[:],
                                op0=ALU.mult, op1=ALU.add), [g, wgt, acc], [acc])
                for hf in range(2):
                    cs = slice(hf * 512, (hf + 1) * 512)
                    for c in range(8):
                        P.op('pe', lambda e, c=c, cs=cs, hf=hf: e.matmul(pb[hf][:], lhsT=h1T[:, c, :], rhs=wpg[:, c, cs],
                                                                         start=(c == 0), stop=(c == 7)),
                             [h1T, wpg], [pb[hf]])
                    for c in range(2):
                        P.op('pe', lambda e, c=c, cs=cs, hf=hf: e.matmul(pb[2 + hf][:], lhsT=pT[:, c, :], rhs=wple[:, c, cs],
                                                                         start=(c == 0), stop=(c == 1)),
                             [pT, wple], [pb[2 + hf]])
                    P.op('act', lambda e, cs=cs, hf=hf: e.activation(out=xn[:, cs], in_=pb[hf][:], func=AF.Sigmoid),
                         [pb[hf]], [xn])
                    P.op('dve', lambda e, cs=cs, hf=hf: e.tensor_tensor(out=ple[:, cs], in0=xn[:, cs], in1=pb[2 + hf][:],
                                                                        op=ALU.mult), [xn, pb[2 + hf]], [ple])
                P.op('dve', lambda e, h1=h1: e.scalar_tensor_tensor(out=r2[:], in0=h1[:], scalar=ALPHA, in1=ple[:],
                                                                    op0=ALU.mult, op1=ALU.add), [h1, ple], [r2])
                if with_peer:
                    P.op('dve', lambda e: e.tensor_tensor(out=r2[:], in0=r2[:], in1=acc[:], op=ALU.add), [r2, acc], [r2])
                layer_norm(r2, ob, L2, st, mv, sd, rstd, xn)
                P.dma('sp', lambda e, ob=ob, rows=rows: e.dma_start(out=out[rows, :], in_=ob[:]), [ob], [])
        with ExitStack() as ph:
            _phase4(ph)
        P.emit()
    return nc


_NC = {}
W_NAMES = ['w_ple_gate', 'w_ple', 'peer_wq', 'peer_k1', 'peer_k2', 'peer_u', 'peer_v',
           'w_branch_sb', 'w_branch_ml', 'w_out']


def kernel(_nblk=NB, _with_mix=True, _with_peer=True, _debug=False, _stop=None, **inp):
    key = (_nblk, _with_mix, _with_peer, _debug, _stop)
    if key not in _NC:
        _NC[key] = build_nc(_nblk, _with_mix, _with_peer, _debug, _stop)
    nc = _NC[key]
    T = _nblk * 128
    x = np.asarray(inp['x'], dtype=np.float32)
    p = np.asarray(inp['p'], dtype=np.float32)[0]
    shared = {n: np.ascontiguousarray(np.asarray(inp[n], dtype=np.float32)[0]) for n in W_NAMES}
    w_in_h = np.asarray(inp['w_in'], dtype=np.float32)[0]
    for i, (c0, c1) in enumerate(((0, 1024), (1024, 2048), (2048, 3072), (3072, 3592), (3592, 4616), (4616, 5640))):
        shared[f'w_in_p{i}'] = np.ascontiguousarray(w_in_h[:, c0:c1])
    shared['conv_wb'] = np.ascontiguousarray(np.concatenate([np.asarray(inp['conv_w'], dtype=np.float32)[0],
                                                             np.asarray(inp['conv_b'], dtype=np.float32)], axis=0))
    shared['gate_b'] = np.ascontiguousarray(np.concatenate([np.asarray(inp['b_igate'], dtype=np.float32)[0],
                                                            np.asarray(inp['b_fgate'], dtype=np.float32)[0]])[None, :])
    shared['ln0_g'] = np.ascontiguousarray(inp['ln0_g'], dtype=np.float32)
    shared['ln0_b'] = np.ascontiguousarray(inp['ln0_b'], dtype=np.float32)
    for i in (1, 2):
        shared[f'ln{i}_g'] = np.ascontiguousarray(inp[f'ln{i}_g'][0], dtype=np.float32)
        shared[f'ln{i}_b'] = np.ascontiguousarray(inp[f'ln{i}_b'][0], dtype=np.float32)
    in_maps = []
    TO = T // 2
    for core in range(8):
        b, j = core // 2, core % 2
        m = dict(shared)
        m["x_all"] = np.ascontiguousarray(x[b, :T])
        m["x_own"] = np.ascontiguousarray(x[b, j * TO:(j + 1) * TO])
        m["p_own"] = np.ascontiguousarray(p[b, j * TO:(j + 1) * TO])
        sel = np.zeros((128, 2), dtype=np.float32); sel[:, j] = 1.0
        m["sel"] = sel
        in_maps.append(m)
    res = run_bass_kernel_spmd(nc, in_maps, core_ids=list(range(8)))
    outp = np.empty((4, T, D), dtype=np.float32)
    for core in range(8):
        b, j = core // 2, core % 2
        outp[b, j * TO:(j + 1) * TO] = res.results[core]["out"]
    if _debug:
        return outp, [res.results[2 * b] for b in range(4)]
    return outp
```

```python
import numpy as np
from contextlib import ExitStack
import concourse.bass as bass
import concourse.mybir as mybir
from concourse.bass_utils import run_bass_kernel_spmd

F32 = mybir.dt.float32
BF16 = mybir.dt.bfloat16
I32 = mybir.dt.int32
U32 = mybir.dt.uint32
AF = mybir.ActivationFunctionType
ALU = mybir.AluOpType
AX = mybir.AxisListType

D = 1024
SEQ = 8192
NB = 64
NOWN = 32
ALPHA = 2.0 ** 0.25
EPS = 1e-5
ENGS = ['pe', 'act', 'dve', 'pool', 'sp']
import os
SERIALIZE = False
SER_P1, SER_P3, SER_P4A, SER_P4B = False, False, False, False


class Res:
    __slots__ = ('w', 'r')

    def __init__(self):
        self.w = None
        self.r = {}


class Tile:
    def __init__(self, t):
        self.t = t
        self.res = Res()
        self._sub = {}
        self.dkey = None
        self.dcount = 0

    def __getitem__(self, k):
        return self.t[k]

    def sub(self, k):
        if k not in self._sub:
            self._sub[k] = Res()
        return self._sub[k]


class Prog:
    def __init__(self, nc, es):
        self.nc = nc
        self.es = es
        self.ops = {e: [] for e in ENGS}
        self.cnt = {e: 0 for e in ENGS}
        self.dcnt = {e: 0 for e in ENGS}
        self.seen = {e: {} for e in ENGS}
        self.sem = {}
        for e in ENGS:
            self.sem[('c', e)] = es.enter_context(nc.semaphore('c_' + e))
        self.dtiles = []
        self.log = []
        self.serialize = SERIALIZE

    def sb(self, name, shape, dt, es=None):
        return Tile((es or self.es).enter_context(self.nc.sbuf_tensor(name, shape, dt)))

    def ps(self, name, shape, dt, es=None):
        return Tile((es or self.es).enter_context(self.nc.psum_tensor(name, shape, dt)))

    def barrier(self):
        cur = []
        for e in ENGS:
            if self.cnt[e]:
                cur.append((('c', e), self.cnt[e]))
        for t in self.dtiles:
            cur.append((t.dkey, 16 * t.dcount))
        for e in ENGS:
            waits = [(k, v) for k, v in cur if self.seen[e].get(k, 0) < v and not (e == 'pe' and k == ('c', 'pe'))]
            for k, v in waits:
                self.seen[e][k] = v
            self.ops[e].append((waits, None, None, 0))
            self.log.append((e, 'barrier', waits, None))

    def _deps(self, eng, reads, writes):
        waits = {}
        seen = self.seen[eng]

        def add(k, v):
            if seen.get(k, 0) >= v:
                return
            if waits.get(k, 0) < v:
                waits[k] = v

        for r in reads:
            if r.w is not None:
                add(*r.w)
        for w in writes:
            if w.w is not None:
                add(*w.w)
            for k, v in w.r.items():
                add(k, v)
        for k, v in waits.items():
            seen[k] = v
        return list(waits.items())

    def _record(self, ev, reads, writes):
        k, v = ev
        for r in reads:
            if r.r.get(k, 0) < v:
                r.r[k] = v
        for w in writes:
            w.w = ev
            w.r = {}

    def op(self, eng, fn, reads=(), writes=()):
        reads = [x.res if isinstance(x, Tile) else x for x in reads]
        writes = [x.res if isinstance(x, Tile) else x for x in writes]
        waits = self._deps(eng, reads, writes)
        self.cnt[eng] += 1
        ev = (('c', eng), self.cnt[eng])
        if eng == 'pe':
            self.seen[eng][ev[0]] = ev[1]
        self._record(ev, reads, writes)
        self.ops[eng].append((waits, fn, ev[0], 1))
        self.log.append((eng, 'op', waits, ev))
        if self.serialize:
            self.barrier()

    def dma(self, eng, fn, reads=(), writes=()):
        tiles = [x for x in list(writes) + list(reads) if isinstance(x, Tile)]
        owner = tiles[0]
        if owner.dkey is None:
            owner.dkey = ('t', len(self.dtiles))
            self.sem[owner.dkey] = self.es.enter_context(self.nc.semaphore('t%d' % len(self.dtiles)))
            self.dtiles.append(owner)
        reads = [x.res if isinstance(x, Tile) else x for x in reads]
        writes = [x.res if isinstance(x, Tile) else x for x in writes]
        waits = self._deps(eng, reads, writes)
        owner.dcount += 1
        ev = (owner.dkey, 16 * owner.dcount)
        self._record(ev, reads, writes)
        self.ops[eng].append((waits, fn, ev[0], 16))
        self.log.append((eng, 'dma', waits, ev))
        if self.serialize:
            self.barrier()

    def emit(self):
        nc = self.nc
        handles = {'pe': 'tensor', 'act': 'scalar', 'dve': 'vector', 'pool': 'gpsimd', 'sp': 'sync'}
        with nc.Block() as blk:
            for e in ENGS:
                ops = self.ops[e]
                final = None
                if e == 'sp':
                    final = [(t.dkey, 16 * t.dcount) for t in self.dtiles]

                def body(eng, ops=ops, final=final):
                    for waits, fn, key, inc in ops:
                        for k, v in waits:
                            eng.wait_ge(self.sem[k], v)
                        if fn is not None:
                            fn(eng).then_inc(self.sem[key], inc)
                    if final:
                        for k, v in final:
                            eng.wait_ge(self.sem[k], v)

                getattr(blk, handles[e])(body)


def _r(x):
    return [x] if not isinstance(x, (list, tuple)) else list(x)


def build_nc(NBLK=NB, with_mix=True, with_peer=True, debug=False, stop_after=None):
    T = NBLK * 128
    nc = bass.Bass("TRN2", target_bir_lowering=False)
    dr = lambda name, shape, dt=F32, kind="ExternalInput": nc.dram_tensor(name, shape, dt, kind=kind).ap()
    SCR = "ExternalOutput" if debug else "Internal"
    NO = NBLK // 2
    TO = NO * 128
    x_all = dr("x_all", [T, D])
    x_own = dr("x_own", [TO, D])
    p_own = dr("p_own", [TO, 256])
    sel_in = dr("sel", [128, 2])
    ln_g = [dr(f"ln{i}_g", [D]) for i in range(3)]
    ln_b = [dr(f"ln{i}_b", [D]) for i in range(3)]
    WPIECES = ((0, 1024), (1024, 2048), (2048, 3072), (3072, 3592), (3592, 4616), (4616, 5640))
    w_in_p = [dr(f"w_in_p{i}", [D, c1 - c0]) for i, (c0, c1) in enumerate(WPIECES)]
    conv_wb = dr("conv_wb", [5, D])
    gate_b = dr("gate_b", [1, 8])
    w_bsb = dr("w_branch_sb", [512, D]); w_bml = dr("w_branch_ml", [512, D]); w_out = dr("w_out", [D, D])
    QTS = dr("qts", [8, 64, T], BF16, SCR)
    KTS = dr("kts", [8, 64, T], BF16, SCR)
    VS = dr("vs", [T, 512], BF16, SCR)
    OMLS = dr("omls", [T, 512], BF16, SCR)
    OSBT = dr("osbt", [4, 128, T], BF16, SCR)
    UB = dr("peer_u_bf", [16384, D], BF16, "Internal")
    VB = dr("peer_v_bf", [16384, D], BF16, "Internal")
    w_pg = dr("w_ple_gate", [D, D])
    w_ple = dr("w_ple", [256, D])
    peer_wq = dr("peer_wq", [D, 2048])
    peer_k1 = dr("peer_k1", [128, 128])
    peer_k2 = dr("peer_k2", [128, 128])
    peer_u = dr("peer_u", [16384, D])
    peer_v = dr("peer_v", [16384, D])
    out = dr("out", [TO, D], F32, "ExternalOutput")
    H1S = dr("h1s", [TO, D], F32, SCR)
    DBG = dr("dbg", [TO, 4, 128], F32, "ExternalOutput") if debug else None

    with ExitStack() as es:
        P = Prog(nc, es)
        ident = P.sb("ident", [128, 128], BF16)
        identf = P.sb("identf", [128, 128], F32)
        epst = P.sb("epst", [128, 1], F32)
        for idt in (ident, identf):
            P.op('pool', lambda e, idt=idt: e.memset(idt[:], 1.0), [], [idt])
            P.op('pool', lambda e, idt=idt: e.affine_select(out=idt[:], in_=idt[:], pattern=[[-1, 128]],
                                                            compare_op=ALU.is_equal, fill=0.0, base=0,
                                                            channel_multiplier=1), [idt], [idt])
        P.op('pool', lambda e: e.memset(epst[:], EPS), [], [epst])
        onet = P.sb("onet", [128, 1], F32)
        P.op('pool', lambda e: e.memset(onet[:], 1.0), [], [onet])
        def ln_params(i, ph, tag):
            g = P.sb(f"{tag}_lng{i}", [128, D], F32, ph); b = P.sb(f"{tag}_lnb{i}", [128, D], F32, ph)
            P.dma('sp', lambda e: e.dma_start(out=g[:], in_=ln_g[i].partition_broadcast(128)), [], [g])
            P.dma('sp', lambda e: e.dma_start(out=b[:], in_=ln_b[i].partition_broadcast(128)), [], [b])
            return (g, b)
        pb = [P.ps(f"pb{i}", [128, 512], F32) for i in range(4)]

        def layer_norm(src, dst, k, st, mv, sd, rstd, xn):
            P.op('dve', lambda e: e.bn_stats(out=st[:, 0:6], in_=src[:, 0:512]), [src], [st])
            P.op('dve', lambda e: e.bn_stats(out=st[:, 6:12], in_=src[:, 512:1024]), [src, st], [st])
            P.op('dve', lambda e: e.bn_aggr(out=mv[:], in_=st[:]), [st], [mv])
            P.op('act', lambda e: e.activation(out=sd[:], in_=mv[:, 1:2], func=AF.Sqrt, bias=epst[:], scale=1.0),
                 [mv, epst], [sd])
            P.op('dve', lambda e: e.reciprocal(out=rstd[:], in_=sd[:]), [sd], [rstd])
            P.op('dve', lambda e: e.tensor_scalar(out=xn[:], in0=src[:], scalar1=mv[:, 0:1], scalar2=rstd[:],
                                                  op0=ALU.subtract, op1=ALU.mult), [src, mv, rstd], [xn])
            P.op('pool', lambda e: e.tensor_tensor(out=xn[:], in0=xn[:], in1=k[0][:], op=ALU.mult),
                 [xn, k[0]], [xn])
            P.op('pool', lambda e: e.tensor_tensor(out=dst[:], in0=xn[:], in1=k[1][:], op=ALU.add),
                 [xn, k[1]], [dst])

        def load_w(dst, src_ap, nchunk, c0=0, c1=None):
            for c in range(nchunk):
                sa = src_ap[c * 128:(c + 1) * 128, :] if c1 is None else src_ap[c * 128:(c + 1) * 128, c0:c1]
                P.dma('pool', lambda e, c=c, sa=sa: e.dma_start(out=dst[:, c, :], in_=sa, max_dma_last_dim=4096),
                      [], [dst])

        def transpose_to(src_b, nchunk, psb, dstT):
            pv = psb[:].bitcast(BF16)
            for c in range(nchunk):
                P.op('pe', lambda e, c=c: e.transpose(out=pv[:, c * 128:(c + 1) * 128],
                                                      in_=src_b[:, c * 128:(c + 1) * 128], identity=ident[:]),
                     [src_b, ident], [psb])
            P.op('dve', lambda e: e.tensor_copy(out=dstT[:].rearrange("p c t -> p (c t)"),
                                                in_=pv[:, 0:nchunk * 128]), [psb], [dstT])

        if with_mix:
            P.serialize = SER_P1
            def _phase0(ph):
                wA = P.sb("p1_wA", [128, 8, 3592], BF16, ph)
                for pi in range(4):
                    c0, c1 = WPIECES[pi]
                    for c in range(8):
                        P.dma('pool', lambda e, c=c, c0=c0, c1=c1, pi=pi: e.dma_start(
                            out=wA[:, c, c0:c1], in_=w_in_p[pi][c * 128:(c + 1) * 128, :], max_dma_last_dim=4096), [], [wA])
                L0 = ln_params(0, ph, "p1")
                cvt = P.sb("p1_cvt", [128, 2], F32, ph)
                for tab_src, tab_dst in ((peer_u, UB), (peer_v, VB)):
                    for r0 in range(0, 16384, 1024):
                        P.dma('pool', lambda e, tab_src=tab_src, tab_dst=tab_dst, r0=r0: e.dma_start(
                            out=tab_dst[r0:r0 + 1024, :], in_=tab_src[r0:r0 + 1024, :], max_dma_last_dim=4096), [cvt], [])
                cw = P.sb("p1_cw", [128, 8, 5], F32, ph)
                cw5 = P.sb("p1_cw5", [5, D], F32, ph)
                P.dma('sp', lambda e: e.dma_start(out=cw5[:], in_=conv_wb), [], [cw5])
                for c in range(8):
                    P.op('pe', lambda e, c=c: e.transpose(out=pb[0][:, c * 5:(c + 1) * 5], in_=cw5[0:5, c * 128:(c + 1) * 128],
                                                          identity=identf[0:5, 0:5]), [cw5, identf], [pb[0]])
                P.op('dve', lambda e: e.tensor_copy(out=cw[:].rearrange("p c j -> p (c j)"), in_=pb[0][:, 0:40]), [pb[0]], [cw])
                onesF = P.sb("p1_onesF", [128, 128], F32, ph)
                P.op('pool', lambda e: e.memset(onesF[:], 1.0), [], [onesF])
                gb1 = P.sb("p1_gb1", [1, 8], F32, ph)
                P.dma('sp', lambda e: e.dma_start(out=gb1[:], in_=gate_b), [], [gb1])
                gbb = P.sb("p1_gbb", [128, 8], F32, ph)
                P.op('pe', lambda e: e.matmul(pb[1][:, 0:8], lhsT=onesF[0:1, :], rhs=gb1[:], start=True, stop=True), [onesF, gb1], [pb[1]])
                P.op('dve', lambda e: e.tensor_copy(out=gbb[:], in_=pb[1][:, 0:8]), [pb[1]], [gbb])
                triLE = P.sb("p1_triLE", [128, 128], F32, ph)
                P.op('pool', lambda e: e.memset(triLE[:], 1.0), [], [triLE])
                P.op('pool', lambda e: e.affine_select(out=triLE[:], in_=triLE[:], pattern=[[1, 128]], compare_op=ALU.is_ge,
                                                       fill=0.0, base=0, channel_multiplier=-1), [triLE], [triLE])
                xt = [P.sb(f"p1_xt{i}", [128, D], F32, ph) for i in range(2)]
                st = P.sb("p1_st", [128, 12], F32, ph); mv = P.sb("p1_mv", [128, 2], F32, ph)
                sd = P.sb("p1_sd", [128, 1], F32, ph); rstd = P.sb("p1_rstd", [128, 1], F32, ph)
                xn = P.sb("p1_xn", [128, D], F32, ph)
                hn = [P.sb(f"p1_hn{i}", [128, D], F32, ph) for i in range(2)]
                hnb = P.sb("p1_hnb", [128, D], BF16, ph)
                hnT = P.sb("p1_hnT", [128, 8, 128], BF16, ph)
                qTb = [P.sb(f"p1_qTb{i}", [64, 8, 128], BF16, ph) for i in range(2)]
                kTb = [P.sb(f"p1_kTb{i}", [64, 8, 128], BF16, ph) for i in range(2)]
                xraw = P.sb("p1_xraw", [128, 8, 131], F32, ph)
                cacc = P.sb("p1_cacc", [128, 8, 128], F32, ph)
                ctmp = P.sb("p1_ctmp", [128, 8, 128], F32, ph)
                mqk = P.sb("p1_mqk", [128, 8, 128], BF16, ph)
                vb = [P.sb(f"p1_vb{i}", [128, 512], BF16, ph) for i in range(2)]
                mva = P.sb("p1_mva", [128, 4, 129], BF16, ph)
                sgo = P.sb("p1_sgo", [128, 512], F32, ph)
                ifr = P.sb("p1_ifr", [128, 8], F32, ph)
                li = P.sb("p1_li", [128, 4], F32, ph); fz = P.sb("p1_fz", [128, 4], F32, ph)
                l1 = P.sb("p1_l1", [128, 4], F32, ph); gtmp = P.sb("p1_gtmp", [128, 4], F32, ph)
                gs = P.sb("p1_gs", [128, 4], F32, ph); eq = P.sb("p1_eq", [128, 4], F32, ph); eb = P.sb("p1_eb", [128, 4], F32, ph)
                STt = P.sb("p1_ST", [128, 128], BF16, ph)
                ktil = P.sb("p1_ktil", [128, 128], BF16, ph)
                C32 = [P.sb(f"p1_C32_{h}", [128, 129], F32, ph) for h in range(4)]
                C16 = [P.sb(f"p1_C16_{h}", [128, 129], BF16, ph) for h in range(4)]
                tmpC = P.sb("p1_tmpC", [128, 129], F32, ph)
                dn = P.sb("p1_dn", [128, 1], F32, ph); scl = P.sb("p1_scl", [128, 1], F32, ph)
                oml = [P.sb(f"p1_oml{i}", [128, 512], BF16, ph) for i in range(2)]
                P.op('pool', lambda e: e.memset(xraw[:], 0.0), [], [xraw])
                P.op('pool', lambda e: e.memset(mva[:], 1.0), [], [mva])
                for h in range(4):
                    P.op('pool', lambda e, h=h: e.memset(C32[h][:], 0.0), [], [C32[h]])
                    P.op('pool', lambda e, h=h: e.memset(C16[h][:], 0.0), [], [C16[h]])
                LNSC = float(np.log(128.0 ** -0.5))
                lnsc = P.sb("p1_lnsc", [128, 1], F32, ph); onec = P.sb("p1_onec", [128, 1], F32, ph)
                P.op('pool', lambda e: e.memset(lnsc[:], LNSC), [], [lnsc])
                P.op('pool', lambda e: e.memset(onec[:], 1.0), [], [onec])

                for s in range(NBLK):
                    rows = slice(s * 128, (s + 1) * 128)
                    tcol = slice(s * 128, (s + 1) * 128)
                    xb = xt[s % 2]; hb = hn[s % 2]; qb = qTb[s % 2]; kb_ = kTb[s % 2]; vbb = vb[s % 2]; omb = oml[s % 2]
                    P.dma('sp', lambda e, xb=xb, rows=rows: e.dma_start(out=xb[:], in_=x_all[rows, :]), [], [xb])
                    layer_norm(xb, hb, L0, st, mv, sd, rstd, xn)
                    P.op('act', lambda e, hb=hb: e.activation(out=hnb[:], in_=hb[:], func=AF.Copy), [hb], [hnb])
                    transpose_to(hnb, 8, pb[0], hnT)
                    for which, col0, dstb, scale, banks in ((0, 0, qb, 1.0, (0, 1)), (1, 512, kb_, 0.125, (2, 3))):
                        for h in range(8):
                            bk = pb[banks[h // 4]]; cs = slice((h % 4) * 128, (h % 4 + 1) * 128)
                            for c in range(8):
                                P.op('pe', lambda e, bk=bk, cs=cs, c=c, h=h, col0=col0: e.matmul(
                                    bk[0:64, cs], lhsT=wA[:, c, col0 + h * 64:col0 + (h + 1) * 64], rhs=hnT[:, c, :],
                                    start=(c == 0), stop=(c == 7)), [wA, hnT], [bk])
                        for hh in range(2):
                            P.op('act', lambda e, hh=hh, dstb=dstb, scale=scale, banks=banks: e.activation(
                                out=dstb[:, hh * 4:(hh + 1) * 4, :].rearrange("p h t -> p (h t)"), in_=pb[banks[hh]][0:64, :],
                                func=AF.Copy, scale=scale), [pb[banks[hh]]], [dstb])
                    P.dma('sp', lambda e, qb=qb, tcol=tcol: e.dma_start(out=QTS[:, :, tcol].rearrange("h d t -> d h t"), in_=qb[:]), [qb], [])
                    P.dma('sp', lambda e, kb_=kb_, tcol=tcol: e.dma_start(out=KTS[:, :, tcol].rearrange("h d t -> d h t"), in_=kb_[:]), [kb_], [])
                    for cc in range(8):
                        bk = pb[cc // 4]; cs = slice((cc % 4) * 128, (cc % 4 + 1) * 128)
                        for c in range(8):
                            P.op('pe', lambda e, bk=bk, cs=cs, c=c, cc=cc: e.matmul(
                                bk[:, cs], lhsT=wA[:, c, 1536 + cc * 128:1536 + (cc + 1) * 128], rhs=hnT[:, c, :],
                                start=(c == 0), stop=(c == 7)), [wA, hnT], [bk])
                    for hh in range(2):
                        P.op('act', lambda e, hh=hh: e.activation(out=xraw[:, hh * 4:(hh + 1) * 4, 3:131],
                                                                  in_=pb[hh][:].rearrange("p (c t) -> p c t", c=4),
                                                                  func=AF.Copy), [pb[hh]], [xraw])
                    for col0, bk, n in ((1024, pb[2], 512), (2560, pb[3], 512), (3072, pb[0], 512), (3584, pb[1], 8)):
                        for c in range(8):
                            P.op('pe', lambda e, bk=bk, c=c, col0=col0, n=n: e.matmul(
                                bk[:, 0:n], lhsT=hnT[:, c, :], rhs=wA[:, c, col0:col0 + n],
                                start=(c == 0), stop=(c == 7)), [wA, hnT], [bk])
                    P.op('act', lambda e, vbb=vbb: e.activation(out=vbb[:], in_=pb[2][:], func=AF.Copy), [pb[2]], [vbb])
                    P.dma('sp', lambda e, vbb=vbb, rows=rows: e.dma_start(out=VS[rows, :], in_=vbb[:]), [vbb], [])
                    P.op('act', lambda e: e.activation(out=mva[:, :, 0:128], in_=pb[3][:].rearrange("p (h d) -> p h d", h=4),
                                                       func=AF.Copy), [pb[3]], [mva])
                    P.op('act', lambda e: e.activation(out=sgo[:], in_=pb[0][:], func=AF.Sigmoid), [pb[0]], [sgo])
                    P.op('dve', lambda e: e.tensor_copy(out=ifr[:], in_=pb[1][:, 0:8]), [pb[1]], [ifr])
                    wb = lambda j: cw[:, :, j:j + 1].to_broadcast([128, 8, 128])
                    P.op('dve', lambda e: e.tensor_tensor(out=cacc[:], in0=xraw[:, :, 3:131], in1=wb(0), op=ALU.mult), [xraw, cw], [cacc])
                    P.op('dve', lambda e: e.tensor_tensor(out=cacc[:], in0=cacc[:], in1=cw[:, :, 4:5].to_broadcast([128, 8, 128]),
                                                          op=ALU.add), [cacc, cw], [cacc])
                    for j in range(1, 4):
                        P.op('dve', lambda e, j=j: e.tensor_tensor(out=ctmp[:], in0=xraw[:, :, 3 - j:131 - j], in1=wb(j), op=ALU.mult),
                             [xraw, cw], [ctmp])
                        P.op('dve', lambda e: e.tensor_tensor(out=cacc[:], in0=cacc[:], in1=ctmp[:], op=ALU.add), [cacc, ctmp], [cacc])
                    P.op('act', lambda e: e.activation(out=mqk[:], in_=cacc[:], func=AF.Silu), [cacc], [mqk])
                    P.op('dve', lambda e: e.tensor_copy(out=ctmp[:, :, 0:3], in_=xraw[:, :, 128:131]), [xraw], [ctmp])
                    P.op('dve', lambda e: e.tensor_copy(out=xraw[:, :, 0:3], in_=ctmp[:, :, 0:3]), [ctmp], [xraw])
                    P.op('dve', lambda e: e.tensor_tensor(out=li[:], in0=ifr[:, 0:4], in1=gbb[:, 0:4], op=ALU.add), [ifr, gbb], [li])
                    P.op('dve', lambda e: e.tensor_tensor(out=fz[:], in0=ifr[:, 4:8], in1=gbb[:, 4:8], op=ALU.add), [ifr, gbb], [fz])
                    P.op('act', lambda e: e.activation(out=fz[:], in_=fz[:], func=AF.Exp, scale=-1.0), [fz], [fz])
                    P.op('act', lambda e: e.activation(out=l1[:], in_=fz[:], func=AF.Ln, bias=onec[:], scale=1.0), [fz, onec], [l1])
                    P.op('pe', lambda e: e.matmul(pb[1][:, 16:20], lhsT=triLE[:], rhs=l1[:], start=True, stop=True), [triLE, l1], [pb[1]])
                    P.op('pe', lambda e: e.matmul(pb[1][:, 32:36], lhsT=onesF[:], rhs=l1[:], start=True, stop=True), [onesF, l1], [pb[1]])
                    P.op('dve', lambda e: e.tensor_tensor(out=gtmp[:], in0=li[:], in1=pb[1][:, 16:20], op=ALU.add), [li, pb[1]], [gtmp])
                    P.op('act', lambda e: e.activation(out=gs[:], in_=gtmp[:], func=AF.Exp, bias=lnsc[:], scale=1.0), [gtmp, lnsc], [gs])
                    P.op('act', lambda e: e.activation(out=eq[:], in_=pb[1][:, 16:20], func=AF.Exp, scale=-1.0), [pb[1]], [eq])
                    P.op('act', lambda e: e.activation(out=eb[:], in_=pb[1][:, 32:36], func=AF.Exp, scale=-1.0), [pb[1]], [eb])
                    for h in range(4):
                        qTh_ = mqk[:, h, :]; kTh_ = mqk[:, 4 + h, :]
                        P.op('pe', lambda e, h=h: e.matmul(pb[2][:, 0:128], lhsT=mqk[:, 4 + h, :], rhs=mqk[:, h, :], start=True, stop=True),
                             [mqk], [pb[2]])
                        P.op('dve', lambda e, h=h: e.scalar_tensor_tensor(out=STt[:], in0=pb[2][:, 0:128], scalar=gs[:, h:h + 1],
                                                                          in1=triLE[:], op0=ALU.mult, op1=ALU.mult),
                             [pb[2], gs, triLE], [STt])
                        pv3 = pb[3][:].bitcast(BF16)
                        P.op('pe', lambda e, h=h, pv3=pv3: e.transpose(out=pv3[:, 0:128], in_=mqk[:, 4 + h, :], identity=ident[:]),
                             [mqk, ident], [pb[3]])
                        P.op('act', lambda e, h=h, pv3=pv3: e.activation(out=ktil[:], in_=pv3[:, 0:128], func=AF.Copy, scale=gs[:, h:h + 1]),
                             [pb[3], gs], [ktil])
                        P.op('pe', lambda e, h=h: e.matmul(pb[0][:, 0:129], lhsT=STt[:], rhs=mva[:, h, :], start=True, stop=False),
                             [STt, mva], [pb[0]])
                        P.op('pe', lambda e, h=h: e.matmul(pb[0][:, 0:129], lhsT=mqk[:, h, :], rhs=C16[h][:], start=False, stop=True),
                             [mqk, C16[h]], [pb[0]])
                        P.op('pe', lambda e, h=h: e.matmul(pb[2][:, 256:385], lhsT=ktil[:], rhs=mva[:, h, :], start=True, stop=True),
                             [ktil, mva], [pb[2]])
                        P.op('act', lambda e, h=h: e.activation(out=tmpC[:], in_=C32[h][:], func=AF.Copy, scale=eb[:, h:h + 1]),
                             [C32[h], eb], [tmpC])
                        P.op('dve', lambda e, h=h: e.scalar_tensor_tensor(out=C32[h][:], in0=pb[2][:, 256:385], scalar=eb[:, h:h + 1],
                                                                          in1=tmpC[:], op0=ALU.mult, op1=ALU.add),
                             [pb[2], eb, tmpC], [C32[h]])
                        P.op('act', lambda e, h=h: e.activation(out=C16[h][:], in_=C32[h][:], func=AF.Copy), [C32[h]], [C16[h]])
                        P.op('act', lambda e, h=h: e.activation(out=dn[:], in_=pb[0][:, 128:129], func=AF.Abs, scale=eq[:, h:h + 1]),
                             [pb[0], eq], [dn])
                        P.op('dve', lambda e: e.tensor_single_scalar(out=dn[:], in_=dn[:], scalar=1.0, op=ALU.max), [dn], [dn])
                        P.op('dve', lambda e: e.reciprocal(out=dn[:], in_=dn[:]), [dn], [dn])
                        P.op('dve', lambda e, h=h: e.tensor_tensor(out=scl[:], in0=dn[:], in1=eq[:, h:h + 1], op=ALU.mult), [dn, eq], [scl])
                        P.op('dve', lambda e, h=h, omb=omb: e.scalar_tensor_tensor(
                            out=omb[:, h * 128:(h + 1) * 128], in0=pb[0][:, 0:128], scalar=scl[:], in1=sgo[:, h * 128:(h + 1) * 128],
                            op0=ALU.mult, op1=ALU.mult), [pb[0], scl, sgo], [omb])
                    P.dma('sp', lambda e, omb=omb, rows=rows: e.dma_start(out=OMLS[rows, :], in_=omb[:]), [omb], [])
                P.barrier()
            with ExitStack() as ph:
                _phase0(ph)
            if stop_after == 'p1':
                P.emit()
                return nc

            P.serialize = SER_P3
            def _phase1(ph):
                negtri = P.sb("p3_negtri", [128, 128], BF16, ph)
                triGE = P.sb("p3_triGE", [128, 128], BF16, ph)
                onesb = P.sb("p3_onesb", [128, 128], BF16, ph)
                zerob = P.sb("p3_zerob", [128, 512], BF16, ph)
                P.op('pool', lambda e: e.memset(onesb[:], 1.0), [], [onesb])
                P.op('pool', lambda e: e.memset(zerob[:], 0.0), [], [zerob])
                for tt, val in ((negtri, -30000.0), (triGE, 1.0)):
                    P.op('pool', lambda e, tt=tt, val=val: e.memset(tt[:], val), [], [tt])
                    P.op('pool', lambda e, tt=tt: e.affine_select(out=tt[:], in_=tt[:], pattern=[[-1, 128]], compare_op=ALU.is_ge,
                                                                  fill=0.0, base=0, channel_multiplier=1), [tt], [tt])
                kTh_t = [P.sb(f"p3_kT{i}", [64, T], BF16, ph) for i in range(2)]
                qTh_t = [P.sb(f"p3_qT{i}", [64, T], BF16, ph) for i in range(2)]
                vhp = P.sb("p3_vhp", [128, NBLK, 128], BF16, ph)
                e32 = [P.sb(f"p3_e32_{i}", [128, 512], F32, ph) for i in range(2)]
                sp16 = [P.sb(f"p3_sp16_{i}", [128, 512], BF16, ph) for i in range(2)]
                g32 = [P.sb(f"p3_g32_{i}", [128, 512], F32, ph) for i in range(2)]
                A16 = [P.sb(f"p3_A16_{i}", [128, 512], BF16, ph) for i in range(2)]
                R32 = P.sb("p3_R32", [128, 512], F32, ph)
                R16 = [P.sb(f"p3_R16_{i}", [128, 512], BF16, ph) for i in range(2)]
                obuf = [P.sb(f"p3_obuf{i}", [128, 512], BF16, ph) for i in range(2)]
                pz = [pb[0], pb[1]]; pc = [pb[2], pb[3]]
                po = [P.ps(f"p3_po{i}", [128, 512], F32, ph) for i in range(2)]
                stepn = 0; gcnt = 0
                for h in range(8):
                    hp, hq = h // 2, h % 2
                    kt = kTh_t[h % 2]; qt = qTh_t[h % 2]
                    P.dma('sp', lambda e, kt=kt, h=h: e.dma_start(out=kt[:], in_=KTS[h]), [], [kt])
                    P.dma('sp', lambda e, qt=qt, h=h: e.dma_start(out=qt[:], in_=QTS[h]), [], [qt])
                    if hq == 0:
                        P.dma('sp', lambda e, hp=hp: e.dma_start(
                            out=vhp[:], in_=VS[:, hp * 128:(hp + 1) * 128].rearrange("(kb s) c -> s kb c", s=128)), [], [vhp])
                    for G in range(NBLK // 4):
                        pob = po[gcnt % 2]; ob_ = obuf[gcnt % 2]; gcnt += 1
                        P.op('pool', lambda e: e.memset(R32[:], 0.0), [], [R32])
                        P.op('pe', lambda e, pob=pob: e.matmul(pob[:], lhsT=onesb[:], rhs=zerob[:], start=True, stop=False),
                             [onesb, zerob], [pob])
                        kbs = list(range(4 * G + 3, -1, -1))
                        nst = len(kbs)

                        def geom(i):
                            kb = kbs[i]
                            off = max(0, kb - 4 * G) * 128
                            return kb, off, slice(off, 512)

                        def front(i):
                            kb, off, rng = geom(i)
                            z = pz[i % 2]; ee = e32[i % 2]; ss = sp16[i % 2]
                            diag = kb >= 4 * G
                            P.op('pe', lambda e, z=z, rng=rng, kb=kb, off=off, diag=diag, kt=kt, qt=qt, G=G: e.matmul(
                                z[:, rng], lhsT=kt[:, kb * 128:(kb + 1) * 128], rhs=qt[:, G * 512 + off:(G + 1) * 512],
                                start=True, stop=(not diag)), [kt, qt], [z])
                            if diag:
                                P.op('pe', lambda e, z=z, off=off: e.matmul(z[:, off:off + 128], lhsT=ident[:], rhs=negtri[:],
                                                                            start=False, stop=True), [ident, negtri], [z])
                            P.op('act', lambda e, z=z, ee=ee, rng=rng: e.activation(out=ee[:, rng], in_=z[:, rng], func=AF.Exp), [z], [ee])
                            P.op('act', lambda e, ss=ss, ee=ee, rng=rng: e.activation(out=ss[:, rng], in_=ee[:, rng], func=AF.Ln,
                                                                                      bias=onet[:], scale=1.0), [ee, onet], [ss])

                        def cmm(i):
                            kb, off, rng = geom(i)
                            cps = pc[i % 2]; ss = sp16[i % 2]; Rr = R16[i % 2]
                            P.op('pe', lambda e, cps=cps, ss=ss, rng=rng, i=i: e.matmul(
                                cps[:, rng], lhsT=triGE[:], rhs=ss[:, rng], start=True, stop=(i == 0)), [triGE, ss], [cps])
                            if i > 0:
                                P.op('pe', lambda e, cps=cps, Rr=Rr, rng=rng: e.matmul(
                                    cps[:, rng], lhsT=onesb[:], rhs=Rr[:, rng], start=False, stop=True), [onesb, Rr], [cps])

                        def av(i):
                            kb, off, rng = geom(i)
                            aa = A16[i % 2]
                            P.op('pe', lambda e, aa=aa, rng=rng, kb=kb, i=i, pob=pob, nst=nst: e.matmul(
                                pob[:, rng], lhsT=vhp[:, kb, :], rhs=aa[:, rng], start=False, stop=(i == nst - 1)), [vhp, aa], [pob])

                        def back(i):
                            kb, off, rng = geom(i)
                            cps = pc[i % 2]; ee = e32[i % 2]; ss = sp16[i % 2]; gg = g32[i % 2]; aa = A16[i % 2]
                            Rw = R16[(i + 1) % 2]
                            P.op('act', lambda e, cps=cps, gg=gg, rng=rng: e.activation(out=gg[:, rng], in_=cps[:, rng], func=AF.Exp,
                                                                                        scale=-1.0), [cps], [gg])
                            P.op('dve', lambda e, aa=aa, ee=ee, gg=gg, rng=rng: e.tensor_tensor(out=aa[:, rng], in0=ee[:, rng],
                                                                                               in1=gg[:, rng], op=ALU.mult),
                                 [ee, gg], [aa])
                            if i < nst - 1:
                                P.op('pool', lambda e, ss=ss, rng=rng: e.tensor_tensor(out=R32[:, rng], in0=R32[:, rng], in1=ss[:, rng],
                                                                                      op=ALU.add), [R32, ss], [R32])
                                P.op('pool', lambda e, Rw=Rw: e.tensor_copy(out=Rw[:], in_=R32[:]), [R32], [Rw])

                        front(0)
                        for i in range(nst):
                            if i + 1 < nst:
                                front(i + 1)
                            cmm(i)
                            if i >= 1:
                                av(i - 1)
                            back(i)
                        av(nst - 1)
                        prow = slice(64 * hq, 64 * hq + 64)
                        P.op('act', lambda e, pob=pob, ob_=ob_, prow=prow: e.activation(out=ob_[prow, :], in_=pob[prow, :], func=AF.Copy),
                             [pob], [ob_])
                        P.dma('sp', lambda e, ob_=ob_, prow=prow, hp=hp, G=G: e.dma_start(
                            out=OSBT[hp, prow, G * 512:(G + 1) * 512], in_=ob_[prow, :]), [ob_], [])
                P.barrier()
            with ExitStack() as ph:
                _phase1(ph)
            if stop_after == 'p3':
                P.emit()
                return nc

            P.serialize = SER_P4A
            def _phase2(ph):
                wG = P.sb("p4_wG", [128, 8, 2048], BF16, ph)
                for pi in (4, 5):
                    c0, c1 = WPIECES[pi]
                    for c in range(8):
                        P.dma('pool', lambda e, c=c, c0=c0, c1=c1, pi=pi: e.dma_start(
                            out=wG[:, c, c0 - 3592:c1 - 3592], in_=w_in_p[pi][c * 128:(c + 1) * 128, :], max_dma_last_dim=4096), [], [wG])
                wsb = P.sb("p4_wsb", [128, 4, D], BF16, ph); wml = P.sb("p4_wml", [128, 4, D], BF16, ph)
                wo = P.sb("p4_wo", [128, 8, D], BF16, ph)
                load_w(wsb, w_bsb, 4); load_w(wml, w_bml, 4); load_w(wo, w_out, 8)
                L1 = ln_params(1, ph, "p4")
                st = P.sb("p4_st", [128, 12], F32, ph); mv = P.sb("p4_mv", [128, 2], F32, ph)
                sd = P.sb("p4_sd", [128, 1], F32, ph); rstd = P.sb("p4_rstd", [128, 1], F32, ph)
                xn = P.sb("p4_xn", [128, D], F32, ph)
                hn = [P.sb(f"p4_hn{i}", [128, D], F32, ph) for i in range(2)]
                hnb = P.sb("p4_hnb", [128, D], BF16, ph)
                hnT = P.sb("p4_hnT", [128, 8, 128], BF16, ph)
                gT = P.sb("p4_gT", [128, 16, 128], BF16, ph)
                osbT = [P.sb(f"p4_osbT{i}", [128, 4, 128], BF16, ph) for i in range(2)]
                omlb = [P.sb(f"p4_oml{i}", [128, 512], BF16, ph) for i in range(2)]
                omlT = P.sb("p4_omlT", [128, 4, 128], BF16, ph)
                t1 = P.sb("p4_t1", [128, D], F32, ph); t2 = P.sb("p4_t2", [128, D], F32, ph)
                yT = P.sb("p4_yT", [128, 8, 128], BF16, ph)
                r1 = P.sb("p4_r1", [128, D], F32, ph)
                h1o = [P.sb(f"p4_h1{i}", [128, D], F32, ph) for i in range(2)]
                L0 = ln_params(0, ph, "p4")
                xt = [P.sb(f"p4_xt{i}", [128, D], F32, ph) for i in range(2)]
                osbB = [P.sb(f"p4_osbB{i}", [128, 4, 128], BF16, ph) for i in range(2)]
                omlB = [P.sb(f"p4_omlB{i}", [128, 512], BF16, ph) for i in range(2)]
                selt = P.sb("p4_sel", [128, 2], F32, ph)
                P.dma('sp', lambda e: e.dma_start(out=selt[:], in_=sel_in), [], [selt])
                for s in range(NO):
                    rows = slice(s * 128, (s + 1) * 128)
                    rowsA = rows; rowsB = slice((NO + s) * 128, (NO + s + 1) * 128)
                    hb = hn[s % 2]; ob_ = osbT[s % 2]; omb = omlb[s % 2]; h1b_ = h1o[s % 2]
                    xb = xt[s % 2]; obB = osbB[s % 2]; omB = omlB[s % 2]
                    P.dma('sp', lambda e, xb=xb, rows=rows: e.dma_start(out=xb[:], in_=x_own[rows, :]), [], [xb])
                    layer_norm(xb, hb, L0, st, mv, sd, rstd, xn)
                    P.dma('sp', lambda e, ob_=ob_, rowsA=rowsA: e.dma_start(out=ob_[:], in_=OSBT[:, :, rowsA].rearrange("hp p t -> p hp t")),
                          [], [ob_])
                    P.dma('sp', lambda e, obB=obB, rowsB=rowsB: e.dma_start(out=obB[:], in_=OSBT[:, :, rowsB].rearrange("hp p t -> p hp t")),
                          [], [obB])
                    P.dma('sp', lambda e, omb=omb, rowsA=rowsA: e.dma_start(out=omb[:], in_=OMLS[rowsA, :]), [], [omb])
                    P.dma('sp', lambda e, omB=omB, rowsB=rowsB: e.dma_start(out=omB[:], in_=OMLS[rowsB, :]), [], [omB])
                    for (ta, tb_, pat) in ((ob_, obB, "p c t -> p (c t)"), (omb, omB, None)):
                        va = ta[:].rearrange(pat) if pat else ta[:]
                        vb_ = tb_[:].rearrange(pat) if pat else tb_[:]
                        P.op('dve', lambda e, va=va: e.tensor_scalar(out=va, in0=va, scalar1=selt[:, 0:1], scalar2=None, op0=ALU.mult),
                             [ta, selt], [ta])
                        P.op('dve', lambda e, va=va, vb_=vb_: e.scalar_tensor_tensor(out=va, in0=vb_, scalar=selt[:, 1:2], in1=va,
                                                                                  op0=ALU.mult, op1=ALU.add), [tb_, selt, ta], [ta])
                    P.op('act', lambda e, hb=hb: e.activation(out=hnb[:], in_=hb[:], func=AF.Copy), [hb], [hnb])
                    transpose_to(hnb, 8, pb[0], hnT)
                    transpose_to(omb, 4, pb[1], omlT)
                    for ch in range(16):
                        bk = pb[ch // 4]; cs = slice((ch % 4) * 128, (ch % 4 + 1) * 128)
                        for c in range(8):
                            P.op('pe', lambda e, bk=bk, cs=cs, c=c, ch=ch: e.matmul(
                                bk[:, cs], lhsT=wG[:, c, ch * 128:(ch + 1) * 128], rhs=hnT[:, c, :],
                                start=(c == 0), stop=(c == 7)), [wG, hnT], [bk])
                    for q4 in range(4):
                        P.op('act', lambda e, q4=q4: e.activation(out=gT[:, q4 * 4:(q4 + 1) * 4, :].rearrange("p c t -> p (c t)"),
                                                                  in_=pb[q4][:], func=AF.Sigmoid), [pb[q4]], [gT])
                    for dc in range(8):
                        cs = slice((dc % 4) * 128, (dc % 4 + 1) * 128)
                        for hp in range(4):
                            P.op('pe', lambda e, dc=dc, cs=cs, hp=hp, ob_=ob_: e.matmul(
                                pb[dc // 4][:, cs], lhsT=wsb[:, hp, dc * 128:(dc + 1) * 128], rhs=ob_[:, hp, :],
                                start=(hp == 0), stop=(hp == 3)), [wsb, ob_], [pb[dc // 4]])
                        for fc in range(4):
                            P.op('pe', lambda e, dc=dc, cs=cs, fc=fc: e.matmul(
                                pb[2 + dc // 4][:, cs], lhsT=wml[:, fc, dc * 128:(dc + 1) * 128], rhs=omlT[:, fc, :],
                                start=(fc == 0), stop=(fc == 3)), [wml, omlT], [pb[2 + dc // 4]])
                    for hf in range(2):
                        cs = slice(hf * 512, (hf + 1) * 512)
                        gsb_ = gT[:, hf * 4:(hf + 1) * 4, :].rearrange("p c t -> p (c t)")
                        gml_ = gT[:, 8 + hf * 4:8 + (hf + 1) * 4, :].rearrange("p c t -> p (c t)")
                        P.op('dve', lambda e, cs=cs, hf=hf, gsb_=gsb_: e.tensor_tensor(out=t1[:, cs], in0=gsb_, in1=pb[hf][:], op=ALU.mult),
                             [gT, pb[hf]], [t1])
                        P.op('dve', lambda e, cs=cs, hf=hf, gml_=gml_: e.tensor_tensor(out=t2[:, cs], in0=gml_, in1=pb[2 + hf][:], op=ALU.mult),
                             [gT, pb[2 + hf]], [t2])
                    P.op('dve', lambda e: e.tensor_tensor(out=yT[:].rearrange("p c t -> p (c t)"), in0=t1[:], in1=t2[:], op=ALU.add),
                         [t1, t2], [yT])
                    for hf in range(2):
                        cs = slice(hf * 512, (hf + 1) * 512)
                        for dc in range(8):
                            P.op('pe', lambda e, hf=hf, cs=cs, dc=dc: e.matmul(pb[hf][:], lhsT=yT[:, dc, :], rhs=wo[:, dc, cs],
                                                                             start=(dc == 0), stop=(dc == 7)), [yT, wo], [pb[hf]])
                        P.op('dve', lambda e, hf=hf, cs=cs, hb=hb: e.scalar_tensor_tensor(out=r1[:, cs], in0=hb[:, cs], scalar=ALPHA,
                                                                                        in1=pb[hf][:], op0=ALU.mult, op1=ALU.add),
                             [hb, pb[hf]], [r1])
                    layer_norm(r1, h1b_, L1, st, mv, sd, rstd, xn)
                    P.dma('sp', lambda e, h1b_=h1b_, rows=rows: e.dma_start(out=H1S[rows, :], in_=h1b_[:]), [h1b_], [])
                P.barrier()
            with ExitStack() as ph:
                _phase2(ph)
            if stop_after == 'p4a':
                P.emit()
                nc._plog = P.log
                return nc

        if not with_mix:
            def _phase3(ph):
                xt = [P.sb(f"a_xt{i}", [128, D], F32, ph) for i in range(2)]
                st = P.sb("a_st", [128, 12], F32, ph); mv = P.sb("a_mv", [128, 2], F32, ph)
                sd = P.sb("a_sd", [128, 1], F32, ph); rstd = P.sb("a_rstd", [128, 1], F32, ph)
                xn = P.sb("a_xn", [128, D], F32, ph); hn = P.sb("a_hn", [128, D], F32, ph)
                r1 = P.sb("a_r1", [128, D], F32, ph)
                h1o = [P.sb(f"a_h1{i}", [128, D], F32, ph) for i in range(2)]
                L0 = ln_params(0, ph, "a"); L1 = ln_params(1, ph, "a")
                for s in range(NO):
                    rows = slice(s * 128, (s + 1) * 128)
                    xb = xt[s % 2]; hb = h1o[s % 2]
                    P.dma('sp', lambda e, xb=xb, rows=rows: e.dma_start(out=xb[:], in_=x_own[rows, :]), [], [xb])
                    layer_norm(xb, hn, L0, st, mv, sd, rstd, xn)
                    P.op('act', lambda e: e.activation(out=r1[:], in_=hn[:], func=AF.Copy, scale=ALPHA), [hn], [r1])
                    layer_norm(r1, hb, L1, st, mv, sd, rstd, xn)
                    P.dma('sp', lambda e, hb=hb, rows=rows: e.dma_start(out=H1S[rows, :], in_=hb[:]), [hb], [])
                P.barrier()

            with ExitStack() as ph:
                _phase3(ph)
        P.serialize = SER_P4B
        def _phase4(ph):
            wpg = P.sb("b_wpg", [128, 8, D], BF16, ph)
            wple = P.sb("b_wple", [128, 2, D], BF16, ph)
            wq = P.sb("b_wq", [128, 8, 2048], BF16, ph)
            wql = P.sb("b_wql", [128, 8, 2048], BF16, ph)
            L2 = ln_params(2, ph, "b")
            kT = [P.sb(f"b_kT{i}", [128, 128], F32, ph) for i in range(2)]
            ktmp = P.sb("b_ktmp", [128, 128], F32, ph)
            kTh = [P.sb(f"b_kTh{i}", [128, 128], BF16, ph) for i in range(2)]
            kTl = [P.sb(f"b_kTl{i}", [128, 128], BF16, ph) for i in range(2)]
            iota16 = P.sb("b_iota16", [128, 16], F32, ph)
            load_w(wpg, w_pg, 8)
            load_w(wple, w_ple, 2)
            load_w(wq, peer_wq, 8)
            P.op('pool', lambda e: e.iota(iota16[:], pattern=[[1, 16]], base=0, channel_multiplier=0,
                                          allow_small_or_imprecise_dtypes=True), [], [iota16])
            for i, kk in enumerate((peer_k1, peer_k2)):
                P.dma('sp', lambda e, kk=kk: e.dma_start(out=ktmp[:], in_=kk), [], [ktmp])
                P.op('pe', lambda e: e.transpose(out=pb[0][:, 0:128], in_=ktmp[:], identity=identf[:]),
                     [ktmp, identf], [pb[0]])
                P.op('dve', lambda e, i=i: e.tensor_copy(out=kT[i][:], in_=pb[0][:, 0:128]), [pb[0]], [kT[i]])
                P.op('dve', lambda e, i=i: e.tensor_copy(out=kTh[i][:], in_=kT[i][:]), [kT[i]], [kTh[i]])
                P.op('dve', lambda e, i=i: e.tensor_tensor(out=kTl[i][:], in0=kT[i][:], in1=kTh[i][:], op=ALU.subtract),
                     [kT[i], kTh[i]], [kTl[i]])

            st = P.sb("b_st", [128, 12], F32, ph); mv = P.sb("b_mv", [128, 2], F32, ph)
            sd = P.sb("b_sd", [128, 1], F32, ph); rstd = P.sb("b_rstd", [128, 1], F32, ph)
            xn = P.sb("b_xn", [128, D], F32, ph)
            h1t = [P.sb(f"b_h1{i}", [128, D], F32, ph) for i in range(2)]
            ptl = [P.sb(f"b_pt{i}", [128, 256], F32, ph) for i in range(2)]
            ptb = P.sb("b_ptb", [128, 256], BF16, ph)
            h1b = P.sb("b_h1b", [128, D], BF16, ph)
            h1T = P.sb("b_h1T", [128, 8, 128], BF16, ph)
            h1l = P.sb("b_h1l", [128, D], BF16, ph)
            h1Tl = P.sb("b_h1Tl", [128, 8, 128], BF16, ph)
            qhi = P.sb("b_qhi", [128, 2048], BF16, ph)
            qlo = P.sb("b_qlo", [128, 2048], BF16, ph)
            pT = P.sb("b_pT", [128, 2, 128], BF16, ph)
            ple = P.sb("b_ple", [128, D], F32, ph)
            r2 = P.sb("b_r2", [128, D], F32, ph)
            ot = [P.sb(f"b_ot{i}", [128, D], F32, ph) for i in range(2)]
            bigA = P.sb("b_bigA", [128, 2048], F32, ph)
            bigB = P.sb("b_bigB", [128, 2048], F32, ph)
            bigC = P.sb("b_bigC", [128, 2048], F32, ph)
            bigD = P.sb("b_bigD", [128, 2048], F32, ph)
            V16 = P.sb("b_V16", [128, 16, 16], F32, ph)
            I16 = P.sb("b_I16", [128, 16, 16], U32, ph)
            I16f = P.sb("b_I16f", [128, 16, 16], F32, ph)
            i1s = P.sb("b_i1s", [128, 8, 16], F32, ph)
            tops = P.sb("b_tops", [128, 8, 16], F32, ph)
            posu = P.sb("b_posu", [128, 8, 16], U32, ph)
            pau = P.sb("b_pau", [128, 8, 16], U32, ph)
            pbu = P.sb("b_pbu", [128, 8, 16], U32, ph)
            paf = P.sb("b_paf", [128, 8, 16], F32, ph)
            pbf = P.sb("b_pbf", [128, 8, 16], F32, ph)
            e1 = P.sb("b_e1", [128, 8, 16], F32, ph)
            e2 = P.sb("b_e2", [128, 8, 16], F32, ph)
            idxf = P.sb("b_idxf", [128, 128], F32, ph)
            idxi = [P.sb(f"b_idxi{i}", [128, 128], I32, ph) for i in range(2)]
            gex = P.sb("b_gex", [128, 8, 16], F32, ph)
            gz = P.sb("b_gz", [128, 8], F32, ph)
            gates = P.sb("b_gates", [128, 8, 16], F32, ph)
            apre = P.sb("b_apre", [128, 128], F32, ph)
            wgt = P.sb("b_wgt", [128, 128], F32, ph)
            dg = [P.sb(f"b_dg{i}", [128, 128], BF16, ph) for i in range(4)]
            junk = P.sb("b_junk", [128, D], BF16, ph)
            NG = 8
            gb = [P.sb(f"b_gb{i}", [128, D], BF16, ph) for i in range(NG)]
            gcount = [0]
            acc = P.ps("b_acc", [128, D], F32, ph)
            h1ps = P.ps("b_h1ps", [128, D], F32, ph)

            for c in range(8):
                P.dma('sp', lambda e, c=c: e.dma_start(out=bigA[:], in_=peer_wq[c * 128:(c + 1) * 128, :]), [], [bigA])
                P.op('dve', lambda e, c=c: e.tensor_tensor(out=wql[:, c, :], in0=bigA[:], in1=wq[:, c, :], op=ALU.subtract),
                     [bigA, wq], [wql])
            for s in range(NO):
                rows = slice(s * 128, (s + 1) * 128)
                h1 = h1t[s % 2]; pl = ptl[s % 2]; ob = ot[s % 2]; ixi = idxi[s % 2]
                P.dma('sp', lambda e, h1=h1, rows=rows: e.dma_start(out=h1[:], in_=H1S[rows, :]), [], [h1])
                P.dma('sp', lambda e, pl=pl, rows=rows: e.dma_start(out=pl[:], in_=p_own[rows, :]), [], [pl])
                P.op('act', lambda e, h1=h1: e.activation(out=h1b[:], in_=h1[:], func=AF.Copy), [h1], [h1b])
                transpose_to(h1b, 8, pb[0], h1T)
                if with_peer:
                    P.op('dve', lambda e, h1=h1: e.tensor_tensor(out=h1l[:], in0=h1[:], in1=h1b[:], op=ALU.subtract),
                         [h1, h1b], [h1l])
                    transpose_to(h1l, 8, pb[1], h1Tl)
                P.op('dve', lambda e, pl=pl: e.tensor_copy(out=ptb[:], in_=pl[:]), [pl], [ptb])
                transpose_to(ptb, 2, pb[1], pT)
                if with_peer:
                    P.op('act', lambda e, h1=h1: e.activation(out=h1ps[:], in_=h1[:], func=AF.Copy), [h1], [h1ps])
                    for ch in range(16):
                        bk = pb[ch // 4]; cs = slice((ch % 4) * 128, (ch % 4 + 1) * 128)
                        for pi, (wt, ht) in enumerate(((wq, h1T), (wql, h1T), (wq, h1Tl))):
                            for c in range(8):
                                P.op('pe', lambda e, bk=bk, cs=cs, c=c, ch=ch, wt=wt, ht=ht, pi=pi: e.matmul(
                                    bk[:, cs], lhsT=wt[:, c, ch * 128:(ch + 1) * 128], rhs=ht[:, c, :],
                                    start=(c == 0 and pi == 0), stop=(c == 7 and pi == 2)), [wt, ht], [bk])
                    for q4 in range(4):
                        P.op('act', lambda e, q4=q4: e.activation(out=qhi[:, q4 * 512:(q4 + 1) * 512], in_=pb[q4][:],
                                                                  func=AF.Copy), [pb[q4]], [qhi])
                        P.op('dve', lambda e, q4=q4: e.tensor_tensor(out=qlo[:, q4 * 512:(q4 + 1) * 512], in0=pb[q4][:],
                                                                     in1=qhi[:, q4 * 512:(q4 + 1) * 512], op=ALU.subtract),
                             [pb[q4], qhi], [qlo])
                    for ch in range(16):
                        bk = pb[ch // 4]; cs = slice((ch % 4) * 128, (ch % 4 + 1) * 128)
                        for pi, (qt, kt) in enumerate(((qhi, kTh), (qlo, kTh), (qhi, kTl))):
                            P.op('pe', lambda e, bk=bk, cs=cs, ch=ch, qt=qt, kt=kt, pi=pi: e.matmul(
                                bk[:, cs], lhsT=qt[:, ch * 128:(ch + 1) * 128], rhs=kt[ch % 2][:],
                                start=(pi == 0), stop=(pi == 2)), [qt, kt[ch % 2]], [bk])
                    for q4 in range(4):
                        P.op('act', lambda e, q4=q4: e.activation(out=bigB[:, q4 * 512:(q4 + 1) * 512], in_=pb[q4][:],
                                                                  func=AF.Copy), [pb[q4]], [bigB])
                    for ch in range(16):
                        seg = slice(ch * 128, (ch + 1) * 128)
                        P.op('dve', lambda e, ch=ch, seg=seg: e.max(out=V16[:, ch, 0:8], in_=bigB[:, seg]), [bigB], [V16])
                        P.op('dve', lambda e, ch=ch, seg=seg: e.match_replace(out=bigC[:, seg], in_to_replace=V16[:, ch, 0:8],
                                                                              in_values=bigB[:, seg], imm_value=-1e30),
                             [bigB, V16], [bigC])
                        P.op('dve', lambda e, ch=ch, seg=seg: e.max(out=V16[:, ch, 8:16], in_=bigC[:, seg]), [bigC], [V16])
                        P.op('dve', lambda e, ch=ch, seg=seg: e.max_index(out=I16[:, ch, 0:8], in_max=V16[:, ch, 0:8],
                                                                          in_values=bigB[:, seg]), [bigB, V16], [I16])
                        P.op('dve', lambda e, ch=ch, seg=seg: e.max_index(out=I16[:, ch, 8:16], in_max=V16[:, ch, 8:16],
                                                                          in_values=bigB[:, seg]), [bigB, V16], [I16])
                    P.op('dve', lambda e: e.tensor_copy(out=I16f[:], in_=I16[:]), [I16], [I16f])
                    Vv = V16[:].rearrange("p (h two) k -> p h two k", two=2)
                    Iv = I16f[:].rearrange("p (h two) k -> p h two k", two=2)
                    P.op('dve', lambda e: e.tensor_scalar(out=i1s[:], in0=Iv[:, :, 0, :], scalar1=128.0, scalar2=None,
                                                          op0=ALU.mult), [I16f], [i1s])
                    c4 = lambda t: t[:].rearrange("p (h a b) -> p h a b", h=8, a=16)
                    bc_a = lambda ap: ap.unsqueeze(3).to_broadcast([128, 8, 16, 16])
                    bc_b = lambda ap: ap.unsqueeze(2).to_broadcast([128, 8, 16, 16])
                    P.op('dve', lambda e: e.tensor_tensor(out=c4(bigA), in0=bc_a(Vv[:, :, 0, :]), in1=bc_b(Vv[:, :, 1, :]),
                                                          op=ALU.add), [V16], [bigA])
                    P.op('dve', lambda e: e.tensor_tensor(out=c4(bigD), in0=bc_a(i1s[:]), in1=bc_b(Iv[:, :, 1, :]),
                                                          op=ALU.add), [i1s, I16f], [bigD])
                    for h in range(8):
                        seg = slice(h * 256, (h + 1) * 256)
                        P.op('dve', lambda e, h=h, seg=seg: e.max(out=tops[:, h, 0:8], in_=bigA[:, seg]), [bigA], [tops])
                        P.op('dve', lambda e, h=h, seg=seg: e.match_replace(out=bigC[:, seg], in_to_replace=tops[:, h, 0:8],
                                                                            in_values=bigA[:, seg], imm_value=-1e30),
                             [bigA, tops], [bigC])
                        P.op('dve', lambda e, h=h, seg=seg: e.max(out=tops[:, h, 8:16], in_=bigC[:, seg]), [bigC], [tops])
                        P.op('dve', lambda e, h=h, seg=seg: e.max_index(out=posu[:, h, 0:8], in_max=tops[:, h, 0:8],
                                                                        in_values=bigA[:, seg]), [bigA, tops], [posu])
                        P.op('dve', lambda e, h=h, seg=seg: e.max_index(out=posu[:, h, 8:16], in_max=tops[:, h, 8:16],
                                                                        in_values=bigA[:, seg]), [bigA, tops], [posu])
                    P.op('dve', lambda e: e.tensor_single_scalar(out=pau[:], in_=posu[:], scalar=4,
                                                                 op=ALU.logical_shift_right), [posu], [pau])
                    P.op('dve', lambda e: e.tensor_single_scalar(out=pbu[:], in_=posu[:], scalar=15,
                                                                 op=ALU.bitwise_and), [posu], [pbu])
                    P.op('dve', lambda e: e.tensor_copy(out=paf[:], in_=pau[:]), [pau], [paf])
                    P.op('dve', lambda e: e.tensor_copy(out=pbf[:], in_=pbu[:]), [pbu], [pbf])
                    io4 = iota16[:].unsqueeze(1).unsqueeze(1).to_broadcast([128, 8, 16, 16])
                    for (pf, src_ap, res_t, rd) in ((paf, i1s[:], e1, [i1s]), (pbf, Iv[:, :, 1, :], e2, [I16f])):
                        P.op('dve', lambda e, pf=pf: e.tensor_tensor(out=c4(bigB), in0=bc_a(pf[:]), in1=io4, op=ALU.is_equal),
                             [pf, iota16], [bigB])
                        P.op('dve', lambda e, src_ap=src_ap: e.tensor_tensor(out=c4(bigC), in0=c4(bigB), in1=bc_b(src_ap),
                                                                             op=ALU.mult), [bigB] + rd, [bigC])
                        P.op('dve', lambda e, res_t=res_t: e.tensor_reduce(out=res_t[:], in_=c4(bigC), axis=AX.X, op=ALU.add),
                             [bigC], [res_t])
                    P.op('dve', lambda e: e.tensor_tensor(out=idxf[:].rearrange("p (h k) -> p h k", h=8), in0=e1[:], in1=e2[:],
                                                          op=ALU.add), [e1, e2], [idxf])
                    P.op('dve', lambda e, ixi=ixi: e.tensor_copy(out=ixi[:], in_=idxf[:]), [idxf], [ixi])
                    P.op('dve', lambda e: e.tensor_tensor(out=gex[:], in0=tops[:],
                                                          in1=tops[:, :, 0:1].to_broadcast([128, 8, 16]), op=ALU.subtract),
                         [tops], [gex])
                    P.op('act', lambda e: e.activation(out=gex[:], in_=gex[:], func=AF.Exp), [gex], [gex])
                    P.op('dve', lambda e: e.tensor_reduce(out=gz[:], in_=gex[:], axis=AX.X, op=ALU.add), [gex], [gz])
                    P.op('dve', lambda e: e.reciprocal(out=gz[:], in_=gz[:]), [gz], [gz])
                    P.op('dve', lambda e: e.tensor_tensor(out=gates[:], in0=gex[:],
                                                          in1=gz[:].unsqueeze(2).to_broadcast([128, 8, 16]), op=ALU.mult),
                         [gex, gz], [gates])
                    for hk in range(128):
                        g = gb[gcount[0] % NG]; gcount[0] += 1
                        P.dma('pool', lambda e, g=g, hk=hk, ixi=ixi: e.indirect_dma_start(
                            out=g[:], out_offset=None, in_=UB,
                            in_offset=bass.IndirectOffsetOnAxis(ap=ixi[:, hk:hk + 1], axis=0)), [ixi], [g])
                        P.op('dve', lambda e, g=g, hk=hk: e.scalar_tensor_tensor(
                            out=junk[:], in0=g[:], scalar=1.0, in1=h1ps[:], op0=ALU.mult, op1=ALU.mult,
                            accum_out=apre[:, hk:hk + 1]), [g, h1ps], [junk, apre])
                    if debug:
                        P.dma('sp', lambda e, rows=rows: e.dma_start(out=DBG[rows, 0, :], in_=idxf[:]), [idxf], [])
                        P.dma('sp', lambda e, rows=rows: e.dma_start(out=DBG[rows, 1, :], in_=apre[:]), [apre], [])
                        P.dma('sp', lambda e, rows=rows: e.dma_start(out=DBG[rows, 2, :], in_=gates[:].rearrange("p h k -> p (h k)")), [gates], [])
                    P.op('act', lambda e: e.activation(out=apre[:], in_=apre[:], func=AF.Gelu), [apre], [apre])
                    if debug:
                        P.dma('sp', lambda e, rows=rows: e.dma_start(out=DBG[rows, 3, :], in_=apre[:]), [apre], [])
                    P.op('dve', lambda e: e.tensor_tensor(out=wgt[:], in0=apre[:],
                                                          in1=gates[:].rearrange("p h k -> p (h k)"), op=ALU.mult),
                         [apre, gates], [wgt])
                    for hk in range(128):
                        g = gb[gcount[0] % NG]; gcount[0] += 1
                        P.dma('pool', lambda e, g=g, hk=hk, ixi=ixi: e.indirect_dma_start(
                            out=g[:], out_offset=None, in_=VB,
                            in_offset=bass.IndirectOffsetOnAxis(ap=ixi[:, hk:hk + 1], axis=0)), [ixi], [g])
                        dgt = dg[hk % 4]
                        P.op('act', lambda e, dgt=dgt, hk=hk: e.activation(out=dgt[:], in_=ident[:], func=AF.Copy,
                                                                          scale=wgt[:, hk:hk + 1]), [ident, wgt], [dgt])
                        for hf in range(2):
                            cs = slice(hf * 512, (hf + 1) * 512)
                            P.op('pe', lambda e, dgt=dgt, g=g, cs=cs, hk=hk: e.matmul(
                                acc[:, cs], lhsT=dgt[:], rhs=g[:, cs], start=(hk == 0), stop=(hk == 127)), [dgt, g], [acc])
                for hf in range(2):
                    cs = slice(hf * 512, (hf + 1) * 512)
                    for c in range(8):
                        P.op('pe', lambda e, c=c, cs=cs, hf=hf: e.matmul(pb[hf][:], lhsT=h1T[:, c, :], rhs=wpg[:, c, cs],
                                                                         start=(c == 0), stop=(c == 7)),
                             [h1T, wpg], [pb[hf]])
                    for c in range(2):
                        P.op('pe', lambda e, c=c, cs=cs, hf=hf: e.matmul(pb[2 + hf][:], lhsT=pT[:, c, :], rhs=wple[:, c, cs],
                                                                         start=(c == 0), stop=(c == 1)),
                             [pT, wple], [pb[2 + hf]])
                    P.op('act', lambda e, cs=cs, hf=hf: e.activation(out=xn[:, cs], in_=pb[hf][:], func=AF.Sigmoid),
                         [pb[hf]], [xn])
                    P.op('dve', lambda e, cs=cs, hf=hf: e.tensor_tensor(out=ple[:, cs], in0=xn[:, cs], in1=pb[2 + hf][:],
                                                                        op=ALU.mult), [xn, pb[2 + hf]], [ple])
                P.op('dve', lambda e, h1=h1: e.scalar_tensor_tensor(out=r2[:], in0=h1[:], scalar=ALPHA, in1=ple[:],
                                                                    op0=ALU.mult, op1=ALU.add), [h1, ple], [r2])
                if with_peer:
                    P.op('dve', lambda e: e.tensor_tensor(out=r2[:], in0=r2[:], in1=acc[:], op=ALU.add), [r2, acc], [r2])
                layer_norm(r2, ob, L2, st, mv, sd, rstd, xn)
                P.dma('sp', lambda e, ob=ob, rows=rows: e.dma_start(out=out[rows, :], in_=ob[:]), [ob], [])
        with ExitStack() as ph:
            _phase4(ph)
        P.emit()
    return nc


_NC = {}
W_NAMES = ['w_ple_gate', 'w_ple', 'peer_wq', 'peer_k1', 'peer_k2', 'peer_u', 'peer_v',
           'w_branch_sb', 'w_branch_ml', 'w_out']


def kernel(_nblk=NB, _with_mix=True, _with_peer=True, _debug=False, _stop=None, **inp):
    key = (_nblk, _with_mix, _with_peer, _debug, _stop)
    if key not in _NC:
        _NC[key] = build_nc(_nblk, _with_mix, _with_peer, _debug, _stop)
    nc = _NC[key]
    T = _nblk * 128
    x = np.asarray(inp['x'], dtype=np.float32)
    p = np.asarray(inp['p'], dtype=np.float32)[0]
    shared = {n: np.ascontiguousarray(np.asarray(inp[n], dtype=np.float32)[0]) for n in W_NAMES}
    w_in_h = np.asarray(inp['w_in'], dtype=np.float32)[0]
    for i, (c0, c1) in enumerate(((0, 1024), (1024, 2048), (2048, 3072), (3072, 3592), (3592, 4616), (4616, 5640))):
        shared[f'w_in_p{i}'] = np.ascontiguousarray(w_in_h[:, c0:c1])
    shared['conv_wb'] = np.ascontiguousarray(np.concatenate([np.asarray(inp['conv_w'], dtype=np.float32)[0],
                                                             np.asarray(inp['conv_b'], dtype=np.float32)], axis=0))
    shared['gate_b'] = np.ascontiguousarray(np.concatenate([np.asarray(inp['b_igate'], dtype=np.float32)[0],
                                                            np.asarray(inp['b_fgate'], dtype=np.float32)[0]])[None, :])
    shared['ln0_g'] = np.ascontiguousarray(inp['ln0_g'], dtype=np.float32)
    shared['ln0_b'] = np.ascontiguousarray(inp['ln0_b'], dtype=np.float32)
    for i in (1, 2):
        shared[f'ln{i}_g'] = np.ascontiguousarray(inp[f'ln{i}_g'][0], dtype=np.float32)
        shared[f'ln{i}_b'] = np.ascontiguousarray(inp[f'ln{i}_b'][0], dtype=np.float32)
    in_maps = []
    TO = T // 2
    for core in range(8):
        b, j = core // 2, core % 2
        m = dict(shared)
        m["x_all"] = np.ascontiguousarray(x[b, :T])
        m["x_own"] = np.ascontiguousarray(x[b, j * TO:(j + 1) * TO])
        m["p_own"] = np.ascontiguousarray(p[b, j * TO:(j + 1) * TO])
        sel = np.zeros((128, 2), dtype=np.float32); sel[:, j] = 1.0
        m["sel"] = sel
        in_maps.append(m)
    res = run_bass_kernel_spmd(nc, in_maps, core_ids=list(range(8)))
    outp = np.empty((4, T, D), dtype=np.float32)
    for core in range(8):
        b, j = core // 2, core % 2
        outp[b, j * TO:(j + 1) * TO] = res.results[core]["out"]
    if _debug:
        return outp, [res.results[2 * b] for b in range(4)]
    return outp
```
